# Optimizing a Trainium2 kernel written in Bass

```python
import math
import jax, jax.numpy as jnp
from jax import lax
import numpy as np

D_MODEL = 1024
BATCH = 4
SEQ = 8192
DEPTH = 2

HEAD_DIM = 64
A_Q_HEADS = 8
A_KV_HEADS = 2
A_GROUP = A_Q_HEADS // A_KV_HEADS
WINDOW = 128
BLOCK = 128
ROPE_THETA = 500000.0
ROPE_DIM = HEAD_DIM // 4
A_WIDTH = A_Q_HEADS * HEAD_DIM
KV_WIDTH = A_KV_HEADS * HEAD_DIM
B_GROUPS = 8
B_GROUP_DIM = 64
B_WIDTH = B_GROUPS * B_GROUP_DIM
CHUNK = 128
EVEN_IN_WIDTH = A_WIDTH + 2 * KV_WIDTH + 2 * B_WIDTH
EVEN_MIX_WIDTH = A_WIDTH + B_WIDTH
POOL_WINDOWS = (2, 4, 8, 16)
POOL_GROUPS = len(POOL_WINDOWS)
POOL_WIDTH = D_MODEL
POOL_GROUP_DIM = POOL_WIDTH // POOL_GROUPS
MEM_LEN = 256
X_HEADS = 4
X_HEAD_DIM = D_MODEL // X_HEADS
N_EXPERTS = 16
N_EXPERT_GROUPS = 4
EXPERTS_PER_GROUP = N_EXPERTS // N_EXPERT_GROUPS
TOP_K = 2
D_EXPERT = D_MODEL // 2
ALPHA = (2.0 * DEPTH) ** 0.25
BETA = (8.0 * DEPTH) ** -0.25
LN_EPS = 1e-5

kernel_name = "hybrid_swa_sgu_pool_memxattn_groupmoe"


def layer_norm(x, g, b):
    xf = x.astype(jnp.float32)
    mu = jnp.mean(xf, axis=-1, keepdims=True)
    var = jnp.mean(jnp.square(xf - mu), axis=-1, keepdims=True)
    y = (xf - mu) * lax.rsqrt(var + LN_EPS) * g.astype(jnp.float32) + b.astype(jnp.float32)
    return y.astype(x.dtype)


def partial_rope(t, positions):
    half = ROPE_DIM // 2
    inv_freq = ROPE_THETA ** (-(jnp.arange(half, dtype=jnp.float32) * 2.0 / ROPE_DIM))
    ang = positions.astype(jnp.float32)[..., None] * inv_freq
    cos = jnp.cos(ang)[:, :, None, :]
    sin = jnp.sin(ang)[:, :, None, :]
    tr = t[..., :ROPE_DIM].astype(jnp.float32)
    t1, t2 = tr[..., :half], tr[..., half:]
    rot = jnp.concatenate([t1 * cos - t2 * sin, t2 * cos + t1 * sin], axis=-1).astype(t.dtype)
    return jnp.concatenate([rot, t[..., ROPE_DIM:]], axis=-1)


def sliding_window_sink_attention(q, k, v, sinks):
    bsz, s_len = q.shape[0], q.shape[1]
    nb = s_len // BLOCK
    qb = q.reshape(bsz, nb, BLOCK, A_KV_HEADS, A_GROUP, HEAD_DIM)

    def with_prev(t):
        tb = t.reshape(bsz, nb, BLOCK, A_KV_HEADS, HEAD_DIM)
        prev = jnp.pad(tb[:, :-1], ((0, 0), (1, 0), (0, 0), (0, 0), (0, 0)))
        return jnp.concatenate([prev, tb], axis=2)

    kb, vb = with_prev(k), with_prev(v)
    s = jnp.einsum('bnqhgd,bnkhd->bnhgqk', qb, kb).astype(jnp.float32) * (HEAD_DIM ** -0.5)
    qi = jnp.arange(BLOCK)[:, None]
    kj = jnp.arange(2 * BLOCK)[None, :]
    rel = qi + BLOCK - kj
    band = (rel >= 0) & (rel < WINDOW)
    not_before_start = (jnp.arange(nb)[:, None, None] > 0) | (kj[None] >= BLOCK)
    valid = band[None] & not_before_start
    s = jnp.where(valid[None, :, None, None], s, -jnp.inf)
    sink = sinks.astype(jnp.float32).reshape(A_KV_HEADS, A_GROUP)[None, None, :, :, None, None]
    m = jnp.maximum(jnp.max(s, axis=-1, keepdims=True), sink)
    p = jnp.exp(s - m)
    denom = jnp.sum(p, axis=-1, keepdims=True) + jnp.exp(sink - m)
    p = (p / denom).astype(v.dtype)
    o = jnp.einsum('bnhgqk,bnkhd->bnqhgd', p, vb)
    return o.reshape(bsz, s_len, A_WIDTH)


def chunked_spatial_gating(u, v, ln_g, ln_b, w_s, b_s):
    bsz, s_len = u.shape[0], u.shape[1]
    nc = s_len // CHUNK
    vg = v.reshape(bsz, s_len, B_GROUPS, B_GROUP_DIM)
    vg = layer_norm(vg, ln_g.reshape(B_GROUPS, B_GROUP_DIM), ln_b.reshape(B_GROUPS, B_GROUP_DIM))
    vg = vg.reshape(bsz, nc, CHUNK, B_GROUPS, B_GROUP_DIM)
    causal = jnp.tril(jnp.ones((CHUNK, CHUNK), dtype=bool))
    w = jnp.where(causal[None], w_s, jnp.zeros_like(w_s))
    mixed = jnp.einsum('gts,bnsgc->bntgc', w, vg) + b_s.T[None, None, :, :, None]
    return u * mixed.reshape(bsz, s_len, B_WIDTH)


def even_mixer(x, positions, w_in, sinks, sgu_ln_g, sgu_ln_b, sgu_w, sgu_b, w_out):
    bsz, s_len, _ = x.shape
    h = x @ w_in
    c1 = A_WIDTH
    c2 = c1 + KV_WIDTH
    c3 = c2 + KV_WIDTH
    c4 = c3 + B_WIDTH
    q, k, v, bu, bv = h[..., :c1], h[..., c1:c2], h[..., c2:c3], h[..., c3:c4], h[..., c4:]
    q = partial_rope(q.reshape(bsz, s_len, A_Q_HEADS, HEAD_DIM), positions)
    k = partial_rope(k.reshape(bsz, s_len, A_KV_HEADS, HEAD_DIM), positions)
    v = v.reshape(bsz, s_len, A_KV_HEADS, HEAD_DIM)
    a_out = sliding_window_sink_attention(q, k, v, sinks)
    b_out = chunked_spatial_gating(jax.nn.gelu(bu), jax.nn.gelu(bv), sgu_ln_g, sgu_ln_b, sgu_w, sgu_b)
    return jnp.concatenate([a_out, b_out], axis=-1) @ w_out


def odd_mixer(x, w_in, pool_w, pool_scale, w_out):
    bsz, s_len, _ = x.shape
    h = x @ w_in
    hf = h.astype(jnp.float32)
    csum = jnp.cumsum(hf, axis=1)
    counts_base = jnp.arange(1, s_len + 1, dtype=jnp.float32)
    groups = []
    for g, win in enumerate(POOL_WINDOWS):
        sl = slice(g * POOL_GROUP_DIM, (g + 1) * POOL_GROUP_DIM)
        c = csum[..., sl]
        lagged = jnp.pad(c, ((0, 0), (win, 0), (0, 0)))[:, :s_len]
        count = jnp.minimum(counts_base, float(win))[None, :, None]
        groups.append((c - lagged) / count - hf[..., sl])
    pooled = jnp.stack(groups, axis=2).astype(x.dtype)
    mapped = jnp.einsum('bsgc,gcd->bsgd', pooled, pool_w).reshape(bsz, s_len, POOL_WIDTH)
    return (mapped * pool_scale) @ w_out


def memory_cross_attention(x, mem, wq, wkv, wo):
    bsz, s_len, _ = x.shape
    q = (x @ wq).reshape(bsz, s_len, X_HEADS, X_HEAD_DIM)
    kv = (mem @ wkv).reshape(bsz, mem.shape[1], 2, X_HEADS, X_HEAD_DIM)
    k, v = kv[:, :, 0], kv[:, :, 1]
    s = jnp.einsum('bshd,bmhd->bhsm', q, k).astype(jnp.float32) * (X_HEAD_DIM ** -0.5)
    p = jax.nn.softmax(s, axis=-1).astype(v.dtype)
    o = jnp.einsum('bhsm,bmhd->bshd', p, v).reshape(bsz, s_len, D_MODEL)
    return o @ wo


def grouped_moe(x, router_w, router_bias, w_gate, w_up, w_down):
    bsz, s_len, d = x.shape
    xt = x.reshape(-1, d)
    scores = jax.nn.softmax((xt @ router_w).astype(jnp.float32), axis=-1)
    biased = scores + router_bias.astype(jnp.float32)
    grouped = biased.reshape(-1, N_EXPERT_GROUPS, EXPERTS_PER_GROUP)
    group_score = jnp.sum(lax.top_k(grouped, TOP_K)[0], axis=-1)
    g_sel = jnp.argmax(group_score, axis=-1)
    in_group = jnp.take_along_axis(grouped, g_sel[:, None, None], axis=1)[:, 0]
    _, local_idx = lax.top_k(in_group, TOP_K)
    expert_idx = g_sel[:, None] * EXPERTS_PER_GROUP + local_idx
    sel = jnp.take_along_axis(scores, expert_idx, axis=1)
    weights = sel / jnp.sum(sel, axis=-1, keepdims=True)
    combine = jnp.sum(jax.nn.one_hot(expert_idx, N_EXPERTS, dtype=jnp.float32) * weights[..., None], axis=1)
    y = jnp.zeros(xt.shape, dtype=jnp.float32)
    for e in range(N_EXPERTS):
        he = jax.nn.silu(xt @ w_gate[e]) * (xt @ w_up[e])
        y = y + combine[:, e:e + 1] * (he @ w_down[e]).astype(jnp.float32)
    return y.astype(x.dtype).reshape(bsz, s_len, d)


def setup_inputs(seed: int = 0) -> dict:
    key = jax.random.key(seed)
    keys = iter(jax.random.split(key, 64))
    f32 = jnp.float32

    def nrm(shape, scale):
        return jax.random.normal(next(keys), shape, f32) * scale

    def gain(n):
        return 1.0 + nrm((n,), 0.05)

    def bias(n):
        return nrm((n,), 0.02)

    inp = {}
    inp["x"] = nrm((BATCH, SEQ, D_MODEL), 1.0)
    inp["mem"] = nrm((BATCH, MEM_LEN, D_MODEL), 1.0)
    offset = jax.random.randint(next(keys), (BATCH, 1), 0, 4096, dtype=jnp.int32)
    inp["positions"] = (offset + jnp.arange(SEQ, dtype=jnp.int32)[None, :]).astype(jnp.int32)
    inp["router_w"] = nrm((D_MODEL, N_EXPERTS), D_MODEL ** -0.5)
    inp["router_bias"] = nrm((N_EXPERTS,), 0.01)

    def common(prefix):
        inp[prefix + "ln1_g"] = gain(D_MODEL)
        inp[prefix + "ln1_b"] = bias(D_MODEL)
        inp[prefix + "xq"] = nrm((D_MODEL, D_MODEL), D_MODEL ** -0.5)
        inp[prefix + "xkv"] = nrm((D_MODEL, 2 * D_MODEL), D_MODEL ** -0.5)
        inp[prefix + "xo"] = nrm((D_MODEL, D_MODEL), BETA * D_MODEL ** -0.5)
        inp[prefix + "ln2_g"] = gain(D_MODEL)
        inp[prefix + "ln2_b"] = bias(D_MODEL)
        inp[prefix + "e_gate"] = nrm((N_EXPERTS, D_MODEL, D_EXPERT), D_MODEL ** -0.5)
        inp[prefix + "e_up"] = nrm((N_EXPERTS, D_MODEL, D_EXPERT), D_MODEL ** -0.5)
        inp[prefix + "e_down"] = nrm((N_EXPERTS, D_EXPERT, D_MODEL), BETA * D_EXPERT ** -0.5)
        inp[prefix + "ln3_g"] = gain(D_MODEL)
        inp[prefix + "ln3_b"] = bias(D_MODEL)

    inp["l0_w_in"] = nrm((D_MODEL, EVEN_IN_WIDTH), D_MODEL ** -0.5)
    inp["l0_sinks"] = nrm((A_Q_HEADS,), 0.5)
    inp["l0_sgu_ln_g"] = gain(B_WIDTH)
    inp["l0_sgu_ln_b"] = bias(B_WIDTH)
    inp["l0_sgu_w"] = nrm((B_GROUPS, CHUNK, CHUNK), CHUNK ** -0.5)
    inp["l0_sgu_b"] = 1.0 + nrm((B_GROUPS, CHUNK), 0.1)
    inp["l0_w_out"] = nrm((EVEN_MIX_WIDTH, D_MODEL), BETA * EVEN_MIX_WIDTH ** -0.5)
    common("l0_")
    inp["l1_w_in"] = nrm((D_MODEL, POOL_WIDTH), D_MODEL ** -0.5)
    inp["l1_pool_w"] = nrm((POOL_GROUPS, POOL_GROUP_DIM, POOL_GROUP_DIM), POOL_GROUP_DIM ** -0.5)
    inp["l1_pool_scale"] = 1.0 + nrm((POOL_WIDTH,), 0.1)
    inp["l1_w_out"] = nrm((POOL_WIDTH, D_MODEL), BETA * POOL_WIDTH ** -0.5)
    common("l1_")

    order = ["x", "mem", "positions", "router_w", "router_bias",
             "l0_w_in", "l0_sinks", "l0_sgu_ln_g", "l0_sgu_ln_b", "l0_sgu_w", "l0_sgu_b", "l0_w_out",
             "l0_ln1_g", "l0_ln1_b", "l0_xq", "l0_xkv", "l0_xo", "l0_ln2_g", "l0_ln2_b",
             "l0_e_gate", "l0_e_up", "l0_e_down", "l0_ln3_g", "l0_ln3_b",
             "l1_w_in", "l1_pool_w", "l1_pool_scale", "l1_w_out",
             "l1_ln1_g", "l1_ln1_b", "l1_xq", "l1_xkv", "l1_xo", "l1_ln2_g", "l1_ln2_b",
             "l1_e_gate", "l1_e_up", "l1_e_down", "l1_ln3_g", "l1_ln3_b"]
    return {name: inp[name] for name in order}


def reference(x, mem, positions, router_w, router_bias,
              l0_w_in, l0_sinks, l0_sgu_ln_g, l0_sgu_ln_b, l0_sgu_w, l0_sgu_b, l0_w_out,
              l0_ln1_g, l0_ln1_b, l0_xq, l0_xkv, l0_xo, l0_ln2_g, l0_ln2_b,
              l0_e_gate, l0_e_up, l0_e_down, l0_ln3_g, l0_ln3_b,
              l1_w_in, l1_pool_w, l1_pool_scale, l1_w_out,
              l1_ln1_g, l1_ln1_b, l1_xq, l1_xkv, l1_xo, l1_ln2_g, l1_ln2_b,
              l1_e_gate, l1_e_up, l1_e_down, l1_ln3_g, l1_ln3_b):
    mixer_params = [
        (l0_w_in, l0_sinks, l0_sgu_ln_g, l0_sgu_ln_b, l0_sgu_w, l0_sgu_b, l0_w_out),
        (l1_w_in, l1_pool_w, l1_pool_scale, l1_w_out),
    ]
    layer_params = [
        (l0_ln1_g, l0_ln1_b, l0_xq, l0_xkv, l0_xo, l0_ln2_g, l0_ln2_b,
         l0_e_gate, l0_e_up, l0_e_down, l0_ln3_g, l0_ln3_b),
        (l1_ln1_g, l1_ln1_b, l1_xq, l1_xkv, l1_xo, l1_ln2_g, l1_ln2_b,
         l1_e_gate, l1_e_up, l1_e_down, l1_ln3_g, l1_ln3_b),
    ]
    for layer in range(DEPTH):
        if layer % 2 == 0:
            mix = even_mixer(x, positions, *mixer_params[layer])
        else:
            mix = odd_mixer(x, *mixer_params[layer])
        (ln1_g, ln1_b, xq, xkv, xo, ln2_g, ln2_b,
         e_gate, e_up, e_down, ln3_g, ln3_b) = layer_params[layer]
        x = layer_norm(ALPHA * x + mix, ln1_g, ln1_b)
        x = layer_norm(ALPHA * x + memory_cross_attention(x, mem, xq, xkv, xo), ln2_g, ln2_b)
        x = layer_norm(ALPHA * x + grouped_moe(x, router_w, router_bias, e_gate, e_up, e_down), ln3_g, ln3_b)
    return x
```

```python
import math
import os
from contextlib import ExitStack

import numpy as np
import ml_dtypes

import concourse.bass as bass
import concourse.mybir as mybir
from concourse.bass_utils import run_bass_kernel_spmd

F32 = mybir.dt.float32
BF16 = mybir.dt.bfloat16
I32 = mybir.dt.int32
AF = mybir.ActivationFunctionType
ALU = mybir.AluOpType
AX = mybir.AxisListType

NCORES = 8
D = 1024
NT = 34
TOK = 4096
ALPHA = (2.0 * 2) ** 0.25
EPS = 1e-5
NE = 16
S_MAX = 33
TWO_PI = 2.0 * math.pi


class Sem:
    def __init__(self, h, name):
        self.h = h
        self.name = name
        self.cnt = 0


class Tk:
    __slots__ = ("name", "w", "r", "sem", "psum")

    def __init__(self, name, sem=None):
        self.name = name
        self.w = None
        self.r = {}
        self.sem = sem
        self.psum = name.startswith(("ptr", "pa", "pb", "pc", "pd", "pg", "pu", "py", "plg"))


class Prog:
    def __init__(self, nc, es):
        self.nc = nc
        self.eng = {"pe": nc.tensor, "act": nc.scalar, "dve": nc.vector, "pool": nc.gpsimd, "sp": nc.sync}
        self.esem = {k: Sem(es.enter_context(nc.semaphore("e_" + k)), "e_" + k) for k in self.eng}
        self.seen = {k: {} for k in self.eng}
        self.dsems = []
        self.es = es
        self.cast_out = []
        self.rec = None
        self.gsems = {}
        self.all_dma_tokens = {}

    def gsem(self, name):
        if name not in self.gsems:
            self.gsems[name] = self.dsem(name)
        return self.gsems[name]

    def dsem(self, name):
        s = Sem(self.es.enter_context(self.nc.semaphore("d_" + name)), "d_" + name)
        self.dsems.append(s)
        return s

    def _wait(self, e, toks):
        need = {}
        for tok in toks:
            if tok is None:
                continue
            s, v = tok
            if e == "pe" and s is self.esem["pe"] and not KPESYNC:
                continue
            if need.get(s.name, (None, -1))[1] < v:
                need[s.name] = (s, v)
        seen = self.seen[e]
        for name, (s, v) in need.items():
            if seen.get(name, -1) >= v:
                continue
            self.eng[e].wait_ge(s.h, v)
            seen[name] = v

    def _deps(self, reads, writes, e=None):
        toks = []
        for t in reads:
            toks.append(t.w)
            if t.psum:
                toks.extend(tok for tok in t.r.values() if e is None or tok[0] is not self.esem[e])
        for t in writes:
            toks.append(t.w)
            toks.extend(t.r.values())
        return toks

    def _commit(self, tok, reads, writes):
        for t in writes:
            t.w = tok
            t.r = {}
        for t in reads:
            s, v = tok
            if t.r.get(s.name, (None, -1))[1] < v:
                t.r[s.name] = tok

    def play(self, item):
        kind = item[0]
        if kind == "op":
            self.op(*item[1:])
        else:
            self.dma(*item[1:])

    def op(self, e, fn, reads=(), writes=()):
        if self.rec is not None:
            self.rec.append(("op", e, fn, list(reads), list(writes)))
            return
        self._wait(e, self._deps(reads, writes, e))
        ins = fn(self.eng[e])
        s = self.esem[e]
        s.cnt += 1
        ins.then_inc(s.h, 1)
        self._commit((s, s.cnt), reads, writes)

    def dma(self, e, fn, sem, reads=(), writes=()):
        if self.rec is not None:
            self.rec.append(("dma", e, fn, sem, list(reads), list(writes)))
            return None
        self._wait(e, self._deps(reads, writes))
        if e == "pool":
            if len(self.cast_out) >= 2:
                self._wait(e, [self.cast_out[-2]])
        ins = fn(self.eng[e])
        if not isinstance(ins, (list, tuple)):
            ins = [ins]
        for i in ins:
            i.then_inc(sem.h, 16)
            sem.cnt += 16
        tok = (sem, sem.cnt)
        if e == "pool":
            self.cast_out.append(tok)
        self.all_dma_tokens[sem.name] = tok
        self._commit(tok, reads, writes)
        return tok

    def barrier(self):
        toks = [(s, s.cnt) for s in self.esem.values()] + list(self.all_dma_tokens.values())
        for e in self.eng:
            self._wait(e, toks)


def bview(ap, shape):
    return ap.to_broadcast(shape)


class StopBuild(Exception):
    pass


import os
KSTOP = int(os.environ.get("KSTOP", "0"))
KTILES = int(os.environ.get("KTILES", "0"))
KSEQ = int(os.environ.get("KSEQ", "0"))
SPARSE = int(os.environ.get("SPARSE", "1"))
KSPLIT = int(os.environ.get("KSPLIT", "0"))
KPESYNC = int(os.environ.get("KPESYNC", "0"))


STOPPED = [False]


def chk(n):
    if KSTOP == n:
        STOPPED[0] = True
        return True
    return False


def build_program(stage="B1"):
    nc = bass.Bass("TRN2", target_bir_lowering=False)
    es = ExitStack()
    with es:
        P = Prog(nc, es)
        try:
            _build(nc, es, stage, P)
        except StopBuild:
            pass
        P.barrier()
    return nc


def _build(nc, es, stage, P):
    def din(name, shape, dt=F32):
        return nc.dram_tensor(name, list(shape), dt, kind="ExternalInput").ap()

    xin = din("xin", [NT * 128, D])
    posT = din("posT", [128, NT], I32)
    mem = din("mem", [256, D])
    router_w = din("router_w", [D, NE])
    router_bias = din("router_bias", [NE])
    W = {}
    W["l0_w_in"] = din("l0_w_in", [D, 1792])
    W["l0_sinks"] = din("l0_sinks", [8])
    W["l0_sgu_ln_g"] = din("l0_sgu_ln_g", [512])
    W["l0_sgu_ln_b"] = din("l0_sgu_ln_b", [512])
    W["l0_sgu_wT"] = din("l0_sgu_wT", [128, 8, 128])
    W["l0_sgu_bT"] = din("l0_sgu_bT", [128, 8])
    W["l0_w_out"] = din("l0_w_out", [D, D])
    W["l1_w_in"] = din("l1_w_in", [D, D])
    W["l1_pool_w"] = din("l1_pool_w", [4, 256, 256])
    W["l1_pool_scaleT"] = din("l1_pool_scaleT", [128, 8])
    W["l1_w_out"] = din("l1_w_out", [D, D])
    for l in range(2):
        p = f"l{l}_"
        for nm in ("ln1_g", "ln1_b", "ln2_g", "ln2_b", "ln3_g", "ln3_b"):
            W[p + nm] = din(p + nm, [D])
        W[p + "xq"] = din(p + "xq", [D, D])
        W[p + "xkv"] = din(p + "xkv", [D, 2 * D])
        W[p + "xo"] = din(p + "xo", [D, D])
        W[p + "e_gate"] = din(p + "e_gate", [NE, D, 512])
        W[p + "e_up"] = din(p + "e_up", [NE, D, 512])
        W[p + "e_down"] = din(p + "e_down", [NE, 512, D])
    c_ident = din("c_ident", [128, 128], BF16)
    c_mask = din("c_mask", [128, 3, 128], BF16)
    c_tril = din("c_tril", [128, 128])
    c_poolB = din("c_poolB", [128, 4, 4, 128], BF16)
    c_invf = din("c_invf", [128, 8])
    c_lstrict = din("c_lstrict", [128, 128], BF16)
    c_tabs = din("c_tabs", [128, 17 + S_MAX + 8])

    out = nc.dram_tensor("out", [TOK, D], F32, kind="ExternalOutput").ap()
    xs_a = nc.dram_tensor("xs_a", [(NT - 1) * 128, D], F32, kind="Internal").ap()
    xs_b = nc.dram_tensor("xs_b", [(NT - 1) * 128, D], F32, kind="Internal").ap()
    xsort = nc.dram_tensor("xsort", [S_MAX * 512, D], BF16, kind="Internal").ap()
    ysort = nc.dram_tensor("ysort", [32 * 512, D], F32, kind="Internal").ap()

    uid = [0]

    def sb(name, shape, dt=F32, stack=es):
        uid[0] += 1
        return stack.enter_context(nc.sbuf_tensor(f"{name}_{uid[0]}", list(shape), dt))

    def ps(name, shape, dt=F32, stack=es):
        uid[0] += 1
        return stack.enter_context(nc.psum_tensor(f"{name}_{uid[0]}", list(shape), dt))

    csem = P.dsem("const")
    ident = sb("ident", [128, 128], BF16)
    mask = sb("mask", [128, 3, 128], BF16)
    invf = sb("invf", [128, 8])
    posi = sb("posi", [128, NT], I32)
    rbias = sb("rbias", [128, NE])
    ones_bf = sb("ones_bf", [128, 128], BF16)
    comb = sb("comb", [128, NT, NE])
    cosT = sb("cosT", [128, NT, 8])
    sinT = sb("sinT", [128, NT, 8])
    rw_bf = sb("rw_bf", [128, 8, NE], BF16)
    memT = sb("memT", [128, 8, 256], BF16)
    T_const = Tk("const")
    T_comb = [Tk(f"comb{j}") for j in range(NT)]
    lg_all = sb("lg_all", [128, NT, NE])
    T_lg = [Tk(f"lg{j}") for j in range(NT)]
    T_cs = Tk("cossin")
    T_memT = Tk("memT")

    def cload(dst, src):
        P.dma("sp", lambda e: e.dma_start(out=dst, in_=src), csem, writes=[T_const])

    cload(ident[:], c_ident)
    cload(mask[:], c_mask)
    cload(invf[:], c_invf)
    cload(posi[:], posT)
    cload(rbias[:], router_bias.partition_broadcast(128))
    lstrict = sb("lstrict", [128, 128], BF16)
    tabs = sb("tabs", [128, 17 + S_MAX + 8])
    cload(lstrict[:], c_lstrict)
    cload(tabs[:], c_tabs)
    T_ones = Tk("ones2")
    P.op("pool", lambda e: e.memset(ones_bf[:], 1.0), writes=[T_ones])

    T_xsort_g = Tk("xsort_g")
    zt = sb("zt", [128, 1024], BF16)
    T_zt = Tk("zt")
    P.op("pool", lambda e: e.memset(zt[:], 0.0), writes=[T_zt])

    with ExitStack() as st:
        posf = sb("posf", [128, NT], F32, st)
        ang = sb("ang", [128, NT, 8], F32, st)
        ki = sb("ki", [128, NT, 8], I32, st)
        kf = sb("kf", [128, NT, 8], F32, st)
        rr = sb("rr", [128, NT, 8], F32, st)
        mm = sb("mm", [128, NT, 8], F32, st)
        T_t = Tk("ropetmp")
        P.op("dve", lambda e: e.tensor_copy(out=posf[:], in_=posi[:]), reads=[T_const], writes=[T_t])
        P.op("dve", lambda e: e.tensor_tensor(out=ang[:], in0=posf[:].unsqueeze(2).to_broadcast([128, NT, 8]),
                                              in1=invf[:].unsqueeze(1).to_broadcast([128, NT, 8]), op=ALU.mult),
             reads=[T_const, T_t], writes=[T_t])
        for which, dst in ((0, sinT), (1, cosT)):
            if which == 1:
                P.op("dve", lambda e: e.tensor_scalar_add(out=ang[:], in0=ang[:], scalar1=math.pi / 2),
                     reads=[T_t], writes=[T_t])
            P.op("dve", lambda e: e.tensor_scalar_mul(out=ki[:], in0=ang[:], scalar1=1.0 / TWO_PI),
                 reads=[T_t], writes=[T_t])
            P.op("dve", lambda e: e.tensor_copy(out=kf[:], in_=ki[:]), reads=[T_t], writes=[T_t])
            P.op("dve", lambda e: e.scalar_tensor_tensor(out=rr[:], in0=kf[:], scalar=-TWO_PI, in1=ang[:],
                                                          op0=ALU.mult, op1=ALU.add), reads=[T_t], writes=[T_t])
            P.op("dve", lambda e: e.tensor_scalar(out=mm[:], in0=rr[:], scalar1=math.pi, scalar2=None, op0=ALU.is_gt),
                 reads=[T_t], writes=[T_t])
            P.op("dve", lambda e: e.scalar_tensor_tensor(out=rr[:], in0=mm[:], scalar=-TWO_PI, in1=rr[:],
                                                          op0=ALU.mult, op1=ALU.add), reads=[T_t], writes=[T_t])
            P.op("dve", lambda e: e.tensor_scalar(out=mm[:], in0=rr[:], scalar1=-math.pi, scalar2=None, op0=ALU.is_lt),
                 reads=[T_t], writes=[T_t])
            P.op("dve", lambda e: e.scalar_tensor_tensor(out=rr[:], in0=mm[:], scalar=TWO_PI, in1=rr[:],
                                                          op0=ALU.mult, op1=ALU.add), reads=[T_t], writes=[T_t])
            P.op("dve", lambda e: e.tensor_scalar(out=rr[:], in0=rr[:], scalar1=3.1415925, scalar2=-3.1415925,
                                                   op0=ALU.min, op1=ALU.max), reads=[T_t], writes=[T_t])
            P.op("act", lambda e, dst=dst: e.activation(out=dst[:], in_=rr[:], func=AF.Sin),
                 reads=[T_t], writes=[T_cs])
        P.barrier()

    with ExitStack() as st:
        wsem = P.dsem("setupw")
        T_rw = Tk("rw")
        rwf = sb("rwf", [128, 8, NE], F32, st)
        P.dma("sp", lambda e: e.dma_start(out=rwf[:], in_=router_w.rearrange("(k p) f -> p k f", p=128)), wsem, writes=[T_rw])
        P.op("dve", lambda e: e.tensor_copy(out=rw_bf[:], in_=rwf[:]), reads=[T_rw], writes=[T_rw])
        memf = sb("memf", [128, 2, D], F32, st)
        memb = sb("memb", [128, 2, D], BF16, st)
        T_mem = Tk("mem")
        msem = P.dsem("mem")
        P.dma("sp", lambda e: e.dma_start(out=memf[:], in_=mem.rearrange("(k p) f -> p k f", p=128)), msem,
              writes=[T_mem])
        P.op("dve", lambda e: e.tensor_copy(out=memb[:], in_=memf[:]), reads=[T_mem], writes=[T_mem])
        ptr = ps("ptr_s", [128, 8, 128], BF16, st)
        T_ptr = Tk("ptr_s")
        for mc in range(2):
            def f(e, mc=mc):
                for dc in range(8):
                    i = e.transpose(out=ptr[:, dc, :], in_=memb[:, mc, dc * 128:(dc + 1) * 128], identity=ident[:])
                return i
            P.op("pe", f, reads=[T_mem, T_const], writes=[T_ptr])
            P.op("dve", lambda e, mc=mc: e.tensor_copy(out=memT[:, :, mc * 128:(mc + 1) * 128], in_=ptr[:]),
                 reads=[T_ptr], writes=[T_memT])
        P.barrier()

    def layer_norm(r, T_r, gb, T_gb, xo_f, xo_b, T_xo, scr, T_scr, tag, norm_on_act=False):
        st6, mv, sd, rstd, nmr = scr
        P.op("dve", lambda e: e.bn_stats(out=st6[:, 0, :], in_=r[:, 0:512]), reads=[T_r], writes=[T_scr])
        P.op("dve", lambda e: e.bn_stats(out=st6[:, 1, :], in_=r[:, 512:1024]), reads=[T_r], writes=[T_scr])
        P.op("dve", lambda e: e.bn_aggr(out=mv[:], in_=st6[:].rearrange("p a b -> p (a b)")),
             reads=[T_scr], writes=[T_scr])
        P.op("act", lambda e: e.activation(out=sd[:], in_=mv[:, 1:2], func=AF.Ln, bias=epsb[:], scale=1.0),
             reads=[T_scr, T_eps], writes=[T_scr])
        P.op("act", lambda e: e.activation(out=rstd[:], in_=sd[:], func=AF.Exp, scale=-0.5), reads=[T_scr], writes=[T_scr])
        if norm_on_act:
            P.op("dve", lambda e: e.scalar_tensor_tensor(out=nmr[:], in0=mv[:, 0:1], scalar=-1.0, in1=rstd[:],
                                                          op0=ALU.mult, op1=ALU.mult), reads=[T_scr], writes=[T_scr])
            P.op("act", lambda e: e.activation(out=r[:], in_=r[:], func=AF.Identity, bias=nmr[:], scale=rstd[:]),
                 reads=[T_scr], writes=[T_r])
        else:
            P.op("dve", lambda e: e.tensor_scalar(out=r[:], in0=r[:], scalar1=mv[:, 0:1], scalar2=rstd[:], op0=ALU.subtract, op1=ALU.mult),
                 reads=[T_scr], writes=[T_r])
        P.op("dve", lambda e: e.tensor_tensor(out=r[:], in0=r[:], in1=gb[:, 0, :], op=ALU.mult),
             reads=[T_gb], writes=[T_r])
        P.op("dve", lambda e: e.tensor_tensor(out=xo_f[:], in0=r[:], in1=gb[:, 1, :], op=ALU.add),
             reads=[T_r, T_gb], writes=[T_xo])
        if xo_b is not None:
            P.op("act", lambda e: e.copy(out=xo_b[:], in_=xo_f[:]), reads=[T_xo], writes=[T_xo])

    epsb = sb("epsb", [128, 1])
    T_eps = Tk("eps")
    P.op("dve", lambda e: e.memset(epsb[:], EPS), writes=[T_eps])

    NSTG = 2
    stg = [sb(f"stg{i}", [128, 512], F32) for i in range(NSTG)]
    T_stg = [Tk(f"stg{i}", P.dsem(f"stg{i}")) for i in range(NSTG)]
    stg_i = [0]

    def stream_weight(dst, src, K, C, T_dst, pool=None):
        stg_, T_stg_, cap = (stg, T_stg, 512) if pool is None else pool
        nst = len(stg_)
        if C >= cap:
            nk = 1
            ncol = C // ((C + cap - 1) // cap)
        else:
            nk, ncol = min(K, cap // C), C
        for k0 in range(0, K, nk):
            k1 = min(K, k0 + nk)
            for c0 in range(0, C, ncol):
                c1 = min(C, c0 + ncol)
                si = stg_i[0] % nst
                stg_i[0] += 1
                n = (k1 - k0) * (c1 - c0)
                sv = stg_[si][:, 0:n].rearrange("p (k f) -> p k f", f=c1 - c0)
                P.dma("sp", lambda e: e.dma_start(out=sv, in_=src[k0 * 128:k1 * 128, c0:c1].rearrange("(k p) f -> p k f", p=128)),
                      T_stg_[si].sem, writes=[T_stg_[si]])
                ce = ("pool", "dve", "act")[stg_i[0] % 3]
                if ce == "act":
                    P.op("act", lambda e: e.copy(out=dst[:, k0:k1, c0:c1], in_=sv), reads=[T_stg_[si]], writes=[T_dst])
                else:
                    P.op(ce, lambda e: e.tensor_copy(out=dst[:, k0:k1, c0:c1], in_=sv), reads=[T_stg_[si]], writes=[T_dst])

    def load_gb(stack, names, tag, sem):
        g = sb("gb_" + tag, [128, 2, D], F32, stack)
        T = Tk("gb_" + tag)
        P.dma("sp", lambda e: [e.dma_start(out=g[:, 0, :], in_=W[names[0]].partition_broadcast(128)),
                               e.dma_start(out=g[:, 1, :], in_=W[names[1]].partition_broadcast(128))],
              sem, writes=[T])
        return g, T

    def load_w(stack, name, src, kchunks, cols, sem):
        w = sb(name, [128, kchunks, cols], BF16, stack)
        T = Tk(name)
        pending_w.append((w, src, kchunks, cols, T))
        return w, T

    pending_w = []
    big_sems = [P.dsem(f"bigstg{i}") for i in range(5)]

    def transpose8(src_bf, T_src, dstT, T_dst, ptr, T_ptr, n=8, evac="act"):
        def f(e):
            for dc in range(n):
                i = e.transpose(out=ptr[:, dc, :], in_=src_bf[:, dc * 128:(dc + 1) * 128], identity=ident[:])
            return i
        P.op("pe", f, reads=[T_src, T_const], writes=[T_ptr])
        if evac == "act":
            P.op("act", lambda e: e.copy(out=dstT[:, 0:n, :], in_=ptr[:, 0:n, :]), reads=[T_ptr], writes=[T_dst])
        else:
            P.op("dve", lambda e: e.tensor_copy(out=dstT[:, 0:n, :], in_=ptr[:, 0:n, :]), reads=[T_ptr], writes=[T_dst])

    def phase_A(layer, src_dram, src_row0, dst_dram):
        pfx = f"l{layer}_"
        with ExitStack() as st:
            wsem = [P.dsem(f"A{layer}w{i}") for i in range(3)]
            gsem = P.dsem(f"A{layer}g")
            if layer == 0:
                win, T_win = load_w(st, "win", W["l0_w_in"], 8, 1792, wsem[1])
            else:
                win, T_win = load_w(st, "win", W["l1_w_in"], 8, D, wsem[1])
            wout, T_wout = load_w(st, "wout", W[pfx + "w_out"], 8, D, wsem[2])
            xq_w, T_xq = load_w(st, "xq_w", W[pfx + "xq"], 8, D, wsem[0])
            xo_w, T_xo = load_w(st, "xo_w", W[pfx + "xo"], 8, D, wsem[1])
            if layer == 0:
                zsem = P.dsem("zfill")
                P.dma("sp", lambda e: [e.dma_start(out=xsort[c_ * 128:(c_ + 1) * 128, :], in_=zt[:]) for c_ in range(S_MAX * 4)],
                      zsem, reads=[T_zt], writes=[T_xsort_g])
            gb1, T_gb1 = load_gb(st, (pfx + "ln1_g", pfx + "ln1_b"), "1", gsem)
            gb2, T_gb2 = load_gb(st, (pfx + "ln2_g", pfx + "ln2_b"), "2", gsem)

            pyb = ps("py_s1b", [128, 512], F32, st)
            T_pyb = Tk("py_s1b")
            pa = ps("pa", [128, 2, 512], F32, st)
            pb = ps("pb", [128, 2, 512], F32, st)
            pc = ps("pc", [128, 2, 512], F32, st)
            pd = ps("pd", [128, 512], F32, st)
            T_pa0, T_pa1, T_pb0, T_pb1, T_pc0, T_pc1, T_pd = [Tk(n) for n in ("pa0", "pa1", "pb0", "pb1", "pc0", "pc1", "pd")]
            def bfv(ap):
                return ap.bitcast(BF16).rearrange("p (a b) -> p a b", b=128)
            pa0v, pa1v, pb0v = bfv(pa[:, 0, :]), bfv(pa[:, 1, :]), bfv(pb[:, 0, :])

            KT = sb("KT", [128, 8, 256], BF16, st)
            V = sb("V", [128, 2, D], BF16, st)
            T_KT, T_V = Tk("KT"), Tk("V")
            if layer == 0:
                wsT = sb("wsT", [128, 8, 128], BF16, st); T_wsT = Tk("wsT")
                bsT = sb("bsT", [128, 8], F32, st)
                sgb = sb("sgb", [128, 2, 512], F32, st)
                sinkb = sb("sinkb", [128, 8], F32, st)
                esink = sb("esink", [128, 8], F32, st); T_esink = Tk("esink")
                T_c0 = Tk("c0")
                c0sem = P.dsem("A0c")
            st2 = ExitStack()
            xkv_w, T_xkv = load_w(st2, "xkv_w", W[pfx + "xkv"], 8, 2 * D, wsem[0])
            bigs = [sb(f"bigstg{i}", [128, 2048], F32, st2) for i in range(5)]
            T_bigs = [Tk(f"bigstg{i}", big_sems[i]) for i in range(5)]
            for (w_, src_, k_, c_, t_) in pending_w[-1:] + pending_w[:-1]:
                stream_weight(w_, src_, k_, c_, t_, pool=(bigs, T_bigs, 2048))
            del pending_w[:]
            if layer == 0:
                wsT_f = sb("wsT_f", [128, 8, 128], F32, st2)
                tril = sb("tril", [128, 128], F32, st2)
                P.dma("sp", lambda e: [e.dma_start(out=wsT_f[:], in_=W["l0_sgu_wT"]),
                                       e.dma_start(out=tril[:], in_=c_tril),
                                       e.dma_start(out=bsT[:], in_=W["l0_sgu_bT"]),
                                       e.dma_start(out=sgb[:, 0, :], in_=W["l0_sgu_ln_g"].partition_broadcast(128)),
                                       e.dma_start(out=sgb[:, 1, :], in_=W["l0_sgu_ln_b"].partition_broadcast(128)),
                                       e.dma_start(out=sinkb[:], in_=W["l0_sinks"].partition_broadcast(128))],
                      c0sem, writes=[T_c0])
                P.op("dve", lambda e: e.tensor_tensor(out=wsT[:], in0=wsT_f[:],
                                                      in1=tril[:].unsqueeze(1).to_broadcast([128, 8, 128]), op=ALU.mult),
                     reads=[T_c0], writes=[T_wsT])
                P.op("act", lambda e: e.activation(out=esink[:], in_=sinkb[:], func=AF.Exp), reads=[T_c0], writes=[T_esink])
            for oc in range(8):
                bank, T_bank = (pa[:, oc % 2, 0:256], (T_pa0, T_pa1)[oc % 2])
                def f(e, oc=oc, bank=bank):
                    for ic in range(8):
                        i = e.matmul(bank, lhsT=xkv_w[:, ic, oc * 128:(oc + 1) * 128], rhs=memT[:, ic, :],
                                     start=(ic == 0), stop=(ic == 7))
                    return i
                P.op("pe", f, reads=[T_xkv, T_memT], writes=[T_bank])
                P.op("act", lambda e, oc=oc, bank=bank: e.copy(out=KT[:, oc, :], in_=bank), reads=[T_bank], writes=[T_KT])
            for mc in range(2):
                for hf in range(2):
                    bank, T_bank = (pb[:, hf, :], (T_pb0, T_pb1)[hf])
                    def f(e, mc=mc, hf=hf, bank=bank):
                        for ic in range(8):
                            i = e.matmul(bank, lhsT=memT[:, ic, mc * 128:(mc + 1) * 128],
                                         rhs=xkv_w[:, ic, D + hf * 512:D + (hf + 1) * 512],
                                         start=(ic == 0), stop=(ic == 7))
                        return i
                    P.op("pe", f, reads=[T_xkv, T_memT], writes=[T_bank])
                    P.op("dve", lambda e, mc=mc, hf=hf, bank=bank: e.tensor_copy(out=V[:, mc, hf * 512:(hf + 1) * 512], in_=bank),
                         reads=[T_bank], writes=[T_V])

            P.barrier()
            st2.close()
            if stage == "S1":
                osem = P.dsem("dbg")
                P.dma("pool", lambda e: [e.dma_start(out=out[0:128, :], in_=KT[:, 0:4, :].rearrange("p a b -> p (a b)")),
                                       e.dma_start(out=out[128:256, :], in_=V[:, 0, :])],
                      osem, reads=[T_KT, T_V], writes=[Tk("o")])
                P.barrier()
                return
            NXS = 3
            xt = [sb(f"xt{i}", [128, D], F32, st) for i in range(NXS)]
            T_xt = [Tk(f"xt{i}", P.dsem(f"A{layer}xt{i}")) for i in range(NXS)]
            xb = sb("xb", [128, D], BF16, st); T_xb = Tk("xb")
            xT = sb("xT", [128, 8, 128], BF16, st); T_xT = Tk("xT")
            mixb = sb("mixb", [128, D], BF16, st); T_mixb = Tk("mixb")
            mixT2 = [sb(f"mixT{i}", [128, 8, 128], BF16, st) for i in range(2)]
            T_mixT2 = [Tk("mixT0"), Tk("mixT1")]
            r = sb("r", [128, D], F32, st); T_r = Tk("r", P.dsem(f"A{layer}r"))
            x1 = [sb(f"x1_{i}", [128, D], F32, st) for i in range(2)]
            x1b = [sb(f"x1b_{i}", [128, D], BF16, st) for i in range(2)]
            T_x1 = [Tk("x1_0"), Tk("x1_1")]
            r2 = sb("r2", [128, D], F32, st); T_r2 = Tk("r2")
            x1T = sb("x1T", [128, 8, 128], BF16, st); T_x1T = Tk("x1T")
            qxT = sb("qxT", [128, 8, 128], BF16, st); T_qxT = Tk("qxT")
            pxT = sb("pxT", [128, 2, 512], BF16, st); T_pxT = [Tk("pxT0"), Tk("pxT1")]
            rdn = sb("rdn", [128, 512], F32, st); T_rdn = Tk("rdn")
            oTn = sb("oTn", [128, 8, 128], BF16, st); T_oTn = Tk("oTn")
            x2 = [sb(f"x2_{i}", [128, D], F32, st) for i in range(2)]
            x2b = sb("x2b", [128, D], BF16, st); T_x2b = Tk("x2b")
            T_x2 = [Tk(f"x2_{i}", P.dsem(f"A{layer}x2_{i}")) for i in range(2)]
            lnscr = (sb("st6", [128, 2, 6], F32, st), sb("mv", [128, 2], F32, st), sb("sd", [128, 1], F32, st),
                     sb("rstd", [128, 1], F32, st), sb("nmr", [128, 1], F32, st))
            T_lnscr = Tk("lnscr")
            lnscr2 = (sb("st6_2", [128, 2, 6], F32, st), sb("mv_2", [128, 2], F32, st), sb("sd_2", [128, 1], F32, st),
                      sb("rstd_2", [128, 1], F32, st), sb("nmr_2", [128, 1], F32, st))
            T_lnscr2 = Tk("lnscr2")

            if layer == 0:
                kT = [sb(f"kT{i}", [128, 128], BF16, st) for i in range(2)]
                vaug = [sb(f"vaug{i}", [128, 2, 65], BF16, st) for i in range(2)]
                T_kT = [Tk("kT0"), Tk("kT1")]
                T_va = [Tk("va0"), Tk("va1")]
                for i in range(2):
                    P.op("pool", lambda e, i=i: e.memset(vaug[i][:], 1.0), writes=[T_va[i]])
                qkb = sb("qkb", [128, 640], BF16, st); T_qkb = Tk("qkb")
                qT = sb("qT", [128, 4, 128], BF16, st); T_qT = Tk("qT")
                rp = [sb(f"rp{i}", [128, 10, 8], F32, st) for i in range(4)]; T_rp = Tk("rp")
                pT = sb("pT", [128, 4, 512], BF16, st)
                T_pT = [Tk(f"pT{i}") for i in range(4)]
                den = sb("den", [128, 8], F32, st); T_den = Tk("den")
                u = sb("u", [128, 512], F32, st); T_u = Tk("u")
                gv = sb("gv", [128, 512], F32, st); T_gv = Tk("gv")
                gt = sb("gt", [128, 2, 512], F32, st); T_gt = Tk("gt")
                vn = sb("vn", [128, 512], BF16, st); T_vn = Tk("vn")
                g8 = {n: sb("g8_" + n, [128, 8], F32, st) for n in ("mean", "ss", "sd", "rstd")}
                T_g8 = Tk("g8")
            else:
                poolB = sb("poolB", [128, 4, 4, 128], BF16, st)
                T_poolB = Tk("poolB")
                P.dma("sp", lambda e: e.dma_start(out=poolB[:], in_=c_poolB), P.dsem("A1poolB"), writes=[T_poolB])
                hb = [sb(f"hb{i}", [128, D], BF16, st) for i in range(2)]
                T_hb = [Tk("hb0"), Tk("hb1")]
                pw, T_pw = None, None
                pw = sb("pw", [128, 4, 2, 256], BF16, st); T_pw = Tk("pw")
                for g in range(4):
                    stream_weight(pw[:, g, :, :], W["l1_pool_w"][g], 2, 256, T_pw)
                scT = sb("scT", [128, 8], F32, st); T_scT = Tk("scT")
                P.dma("sp", lambda e: e.dma_start(out=scT[:], in_=W["l1_pool_scaleT"]), gsem, writes=[T_scT])
                poT = sb("poT", [128, 8, 128], BF16, st); T_poT = Tk("poT")

            j0 = 0 if layer == 0 else 1

            def load_x(j):
                s = j % NXS
                P.dma("sp", lambda e: e.dma_start(out=xt[s][:], in_=src_dram[src_row0 + j * 128: src_row0 + (j + 1) * 128, :]),
                      T_xt[s].sem, writes=[T_xt[s]])

            load_x(j0)
            load_x(j0 + 1)

            def rope(src_ps, H, dst_bf, T_src, T_dst, j):
                T_srcs = T_src if isinstance(T_src, list) else [T_src]
                sv = src_ps.rearrange("p (h d) -> p h d", d=64)
                dv = dst_bf.rearrange("p (h d) -> p h d", d=64)
                P.op("act", lambda e: e.copy(out=dst_bf, in_=src_ps), reads=T_srcs, writes=[T_dst])
                if chk(131):
                    return
                cs = cosT[:, j, :].unsqueeze(1).to_broadcast([128, H, 8])
                sn = sinT[:, j, :].unsqueeze(1).to_broadcast([128, H, 8])
                t1, t2 = sv[:, :, 0:8], sv[:, :, 8:16]
                a, b, c, d_ = [x[:, 0:H, :] for x in rp]
                P.op("dve", lambda e: e.tensor_tensor(out=a, in0=t1, in1=cs, op=ALU.mult), reads=T_srcs + [T_cs], writes=[T_rp])
                if chk(132):
                    return
                P.op("dve", lambda e: e.tensor_tensor(out=b, in0=t2, in1=sn, op=ALU.mult), reads=T_srcs + [T_cs], writes=[T_rp])
                P.op("dve", lambda e: e.tensor_tensor(out=c, in0=t2, in1=cs, op=ALU.mult), reads=T_srcs + [T_cs], writes=[T_rp])
                P.op("dve", lambda e: e.tensor_tensor(out=d_, in0=t1, in1=sn, op=ALU.mult), reads=T_srcs + [T_cs], writes=[T_rp])
                if chk(133):
                    return
                P.op("dve", lambda e: e.tensor_tensor(out=dv[:, :, 0:8], in0=a, in1=b, op=ALU.subtract), reads=[T_rp], writes=[T_dst])
                P.op("dve", lambda e: e.tensor_tensor(out=dv[:, :, 8:16], in0=c, in1=d_, op=ALU.add), reads=[T_rp], writes=[T_dst])

            def proj(bank, T_bank, c0, ncol, src, T_src, w=win, T_w=T_win):
                def f(e):
                    for dc in range(8):
                        i = e.matmul(bank, lhsT=src[:, dc, :], rhs=w[:, dc, c0:c0 + ncol], start=(dc == 0), stop=(dc == 7))
                    return i
                P.op("pe", f, reads=[T_src, T_w], writes=[T_bank])

            def S1(j):
                s = j % NXS
                if j + 2 < NT:
                    load_x(j + 2)
                xj, T_xj = xt[s], T_xt[s]
                x1s = j % 2
                L = {"head": P.rec, "attn": [], "sgu": [], "tail": []}
                def sec(name):
                    P.rec = L[name]
                def finish():
                    A_, B_ = L["attn"], L["sgu"]
                    m = []
                    ia = ib = 0
                    while ia < len(A_) or ib < len(B_):
                        if ib >= len(B_) or (ia < len(A_) and ia * max(len(B_), 1) <= ib * max(len(A_), 1)):
                            m.append(A_[ia]); ia += 1
                        else:
                            m.append(B_[ib]); ib += 1
                    P.rec = L["head"] + m + L["tail"]
                mixT, T_mixT = mixT2[j % 2], T_mixT2[j % 2]
                P.op("dve", lambda e: e.tensor_copy(out=xb[:], in_=xj[:]), reads=[T_xj], writes=[T_xb])
                transpose8(xb, T_xb, xT, T_xT, pb0v, T_pb0)
                cur, prv = j % 2, (j - 1) % 2
                if layer == 0:
                    proj(pa[:, 1, 0:256], T_pa1, 512, 256, xT, T_xT)
                    proj(pa[:, 0, :], T_pa0, 0, 512, xT, T_xT)
                    if j > 0:
                        proj(pb[:, 0, :], T_pb0, 768, 512, xT, T_xT)
                        proj(pb[:, 1, :], T_pb1, 1280, 512, xT, T_xT)
                    sec("attn")
                    rope(pa[:].rearrange("p a b -> p (a b)")[:, 0:640], 10, qkb[:], [T_pa0, T_pa1], T_qkb, j)
                    P.op("act", lambda e: e.copy(out=vaug[cur][:, :, 0:64], in_=pa[:, 1, 128:256].rearrange("p (h d) -> p h d", d=64)),
                         reads=[T_pa1], writes=[T_va[cur]])
                    def f(e):
                        for dc in range(5):
                            ii = e.transpose(out=pa0v[:, dc, :], in_=qkb[:, dc * 128:(dc + 1) * 128], identity=ident[:])
                        return ii
                    P.op("pe", f, reads=[T_qkb, T_const], writes=[T_pa0])
                    P.op("dve", lambda e: e.tensor_copy(out=kT[cur][:], in_=pa0v[:, 4, :]), reads=[T_pa0], writes=[T_kT[cur]])
                    if j == 0:
                        finish()
                        return
                    P.op("act", lambda e: e.copy(out=qT[:], in_=pa0v[:, 0:4, :]), reads=[T_pa0], writes=[T_qT])
                    sec("sgu")
                    P.op("act", lambda e: e.activation(out=gt[:], in_=pb[:], func=AF.Square), reads=[T_pb0, T_pb1], writes=[T_gt])
                    P.op("dve", lambda e: e.tensor_scalar(out=gt[:], in0=gt[:], scalar1=0.044715, scalar2=1.0, op0=ALU.mult, op1=ALU.add),
                         reads=[], writes=[T_gt])
                    P.op("dve", lambda e: e.tensor_tensor(out=gt[:], in0=gt[:], in1=pb[:], op=ALU.mult), reads=[T_pb0, T_pb1], writes=[T_gt])
                    P.op("act", lambda e: e.activation(out=gt[:], in_=gt[:], func=AF.Sigmoid, scale=1.5957691216), reads=[], writes=[T_gt])
                    P.op("dve", lambda e: e.tensor_tensor(out=u[:], in0=gt[:, 0, :], in1=pb[:, 0, :], op=ALU.mult), reads=[T_gt, T_pb0], writes=[T_u])
                    P.op("dve", lambda e: e.tensor_tensor(out=gv[:], in0=gt[:, 1, :], in1=pb[:, 1, :], op=ALU.mult), reads=[T_gt, T_pb1], writes=[T_gv])
                    gv3 = gv[:].rearrange("p (g c) -> p g c", c=64)
                    P.op("dve", lambda e: e.reduce_sum(out=g8["mean"][:], in_=gv3, axis=AX.X), reads=[T_gv], writes=[T_g8])
                    P.op("dve", lambda e: e.tensor_scalar_mul(out=g8["mean"][:], in0=g8["mean"][:], scalar1=1.0 / 64), reads=[], writes=[T_g8])
                    P.op("dve", lambda e: e.tensor_tensor(out=gv3, in0=gv3, in1=g8["mean"][:].unsqueeze(2).to_broadcast([128, 8, 64]), op=ALU.subtract),
                         reads=[T_g8], writes=[T_gv])
                    P.op("act", lambda e: e.activation(out=gt[:, 0, :], in_=gv[:], func=AF.Square), reads=[T_gv, T_u], writes=[T_gt])
                    P.op("dve", lambda e: e.reduce_sum(out=g8["ss"][:], in_=gt[:, 0, :].rearrange("p (g c) -> p g c", c=64), axis=AX.X),
                         reads=[T_gt], writes=[T_g8])
                    P.op("act", lambda e: e.activation(out=g8["sd"][:], in_=g8["ss"][:], func=AF.Ln, bias=epsb[:], scale=1.0 / 64),
                         reads=[T_g8, T_eps], writes=[T_g8])
                    P.op("act", lambda e: e.activation(out=g8["rstd"][:], in_=g8["sd"][:], func=AF.Exp, scale=-0.5), reads=[], writes=[T_g8])
                    P.op("dve", lambda e: e.tensor_tensor(out=gv3, in0=gv3, in1=g8["rstd"][:].unsqueeze(2).to_broadcast([128, 8, 64]), op=ALU.mult),
                         reads=[T_g8], writes=[T_gv])
                    P.op("pool", lambda e: e.tensor_tensor(out=gv[:], in0=gv[:], in1=sgb[:, 0, :], op=ALU.mult), reads=[T_c0], writes=[T_gv])
                    P.op("pool", lambda e: e.tensor_tensor(out=vn[:], in0=gv[:], in1=sgb[:, 1, :], op=ALU.add), reads=[T_gv, T_c0], writes=[T_vn])
                    sec("attn")
                    sc_banks = [(pa[:, 0, :], T_pa0), (pa[:, 1, :], T_pa1), (pa[:, 0, :], T_pa0), (pa[:, 1, :], T_pa1)]
                    for kbi, slot in ((0, prv), (1, cur)):
                        for hg in range(2):
                            bank, T_bank = sc_banks[kbi * 2 + hg]
                            def f(e, bank=bank, slot=slot, hg=hg):
                                for c in range(4):
                                    i = e.matmul(bank[:, c * 128:(c + 1) * 128], lhsT=kT[slot][hg * 64:(hg + 1) * 64, :],
                                                 rhs=qT[hg * 64:(hg + 1) * 64, c, :], start=True, stop=True)
                                return i
                            P.op("pe", f, reads=[T_kT[slot], T_qT], writes=[T_bank])
                            idx = kbi * 2 + hg
                            P.op("act", lambda e, bank=bank, idx=idx: e.activation(out=pT[:, idx, :], in_=bank, func=AF.Exp, scale=0.125),
                                 reads=[T_bank], writes=[T_pT[idx]])
                            mi = 1 if kbi == 1 else (2 if j == 2 else 0)
                            P.op("dve", lambda e, idx=idx, mi=mi: e.tensor_tensor(
                                out=pT[:, idx, :].rearrange("p (c q) -> p c q", q=128),
                                in0=pT[:, idx, :].rearrange("p (c q) -> p c q", q=128),
                                in1=mask[:, mi, :].unsqueeze(1).to_broadcast([128, 4, 128]), op=ALU.mult),
                                reads=[T_const], writes=[T_pT[idx]])
                    def f(e):
                        for c in range(4):
                            for hg in range(2):
                                o_ap = pa[:, c // 2, ((c % 2) * 2 + hg) * 65:((c % 2) * 2 + hg + 1) * 65]
                                for kbi, slot in ((0, prv), (1, cur)):
                                    i = e.matmul(o_ap, lhsT=pT[:, kbi * 2 + hg, c * 128:(c + 1) * 128], rhs=vaug[slot][:, hg, :],
                                                 start=(kbi == 0), stop=(kbi == 1))
                        return i
                    P.op("pe", f, reads=T_pT + T_va, writes=[T_pa0, T_pa1])
                    ov = pa[:, :, 0:260].rearrange("p b (h e) -> p b h e", e=65)
                    P.op("dve", lambda e: e.tensor_tensor(out=den[:].rearrange("p (b h) -> p b h", b=2).unsqueeze(3), in0=ov[:, :, :, 64:65],
                                                          in1=esink[:].rearrange("p (b h) -> p b h", b=2).unsqueeze(3), op=ALU.add),
                         reads=[T_pa0, T_pa1, T_esink], writes=[T_den])
                    P.op("dve", lambda e: e.reciprocal(out=den[:], in_=den[:]), reads=[], writes=[T_den])
                    P.op("dve", lambda e: e.tensor_tensor(out=mixb[:, 0:512].rearrange("p (b h d) -> p b h d", b=2, h=4), in0=ov[:, :, :, 0:64],
                                                          in1=den[:].rearrange("p (b h) -> p b h", b=2).unsqueeze(3).to_broadcast([128, 2, 4, 64]),
                                                          op=ALU.mult),
                         reads=[T_pa0, T_pa1, T_den], writes=[T_mixb])
                    sec("sgu")
                    def f(e):
                        for g in range(8):
                            i = e.matmul(pb[:, 0, g * 64:(g + 1) * 64], lhsT=wsT[:, g, :], rhs=vn[:, g * 64:(g + 1) * 64], start=True, stop=True)
                        return i
                    P.op("pe", f, reads=[T_wsT, T_vn], writes=[T_pb0])
                    P.op("dve", lambda e: e.tensor_tensor(out=gv3, in0=pb[:, 0, :].rearrange("p (g c) -> p g c", c=64),
                                                          in1=bsT[:].unsqueeze(2).to_broadcast([128, 8, 64]), op=ALU.add),
                         reads=[T_pb0, T_c0, T_vn], writes=[T_gv])
                    P.op("dve", lambda e: e.tensor_tensor(out=mixb[:, 512:1024], in0=gv[:], in1=u[:], op=ALU.mult),
                         reads=[T_gv, T_u], writes=[T_mixb])
                    sec("tail")
                    transpose8(mixb, T_mixb, mixT, T_mixT, pa0v, T_pa0)
                else:
                    proj(pa[:, 0, :], T_pa0, 0, 512, xT, T_xT)
                    proj(pa[:, 1, :], T_pa1, 512, 512, xT, T_xT)
                    P.op("act", lambda e: e.copy(out=hb[cur][:], in_=pa[:].rearrange("p a b -> p (a b)")), reads=[T_pa0, T_pa1], writes=[T_hb[cur]])
                    if j == 1:
                        finish()
                        return
                    fo = 2 if j == 2 else 0
                    def f(e):
                        for cc in range(8):
                            g = cc // 2
                            e.matmul(pb[:, cc // 4, (cc % 4) * 128:(cc % 4 + 1) * 128], lhsT=hb[prv][:, cc * 128:(cc + 1) * 128],
                                     rhs=poolB[:, fo + 0, g, :], start=True, stop=False)
                            i = e.matmul(pb[:, cc // 4, (cc % 4) * 128:(cc % 4 + 1) * 128], lhsT=hb[cur][:, cc * 128:(cc + 1) * 128],
                                         rhs=poolB[:, fo + 1, g, :], start=False, stop=True)
                        return i
                    P.op("pe", f, reads=[T_hb[0], T_hb[1], T_poolB], writes=[T_pb0, T_pb1])
                    P.op("act", lambda e: e.copy(out=poT[:].rearrange("p a b -> p (a b)"), in_=pb[:].rearrange("p a b -> p (a b)")),
                         reads=[T_pb0, T_pb1], writes=[T_poT])
                    def f(e):
                        for dc in range(8):
                            g, i2 = dc // 2, dc % 2
                            for ci in range(2):
                                i = e.matmul(pa[:, dc // 4, (dc % 4) * 128:(dc % 4 + 1) * 128], lhsT=pw[:, g, ci, i2 * 128:(i2 + 1) * 128],
                                             rhs=poT[:, 2 * g + ci, :], start=(ci == 0), stop=(ci == 1))
                        return i
                    P.op("pe", f, reads=[T_pw, T_poT], writes=[T_pa0, T_pa1])
                    P.op("dve", lambda e: e.tensor_tensor(out=mixT[:], in0=pa[:].rearrange("p a (b t) -> p (a b) t", t=128),
                                                          in1=scT[:].unsqueeze(2).to_broadcast([128, 8, 128]), op=ALU.mult),
                         reads=[T_pa0, T_pa1, T_scT], writes=[T_mixT])
                finish()

            def S1b(j):
                mixT, T_mixT = mixT2[j % 2], T_mixT2[j % 2]
                x1s = j % 2
                P.dma("sp", lambda e: e.dma_start(out=r[:], in_=src_dram[src_row0 + j * 128: src_row0 + (j + 1) * 128, :]), T_r.sem, writes=[T_r])
                for hf in range(2):
                    def f(e, hf=hf):
                        for dc in range(8):
                            i = e.matmul(pyb[:], lhsT=mixT[:, dc, :], rhs=wout[:, dc, hf * 512:(hf + 1) * 512], start=(dc == 0), stop=(dc == 7))
                        return i
                    P.op("pe", f, reads=[T_mixT, T_wout], writes=[T_pyb])
                    P.op("dve", lambda e, hf=hf: e.scalar_tensor_tensor(out=r[:, hf * 512:(hf + 1) * 512], in0=r[:, hf * 512:(hf + 1) * 512], scalar=ALPHA,
                                                                          in1=pyb[:], op0=ALU.mult, op1=ALU.add),
                         reads=[T_pyb], writes=[T_r])
                layer_norm(r, T_r, gb1, T_gb1, x1[x1s], x1b[x1s], T_x1[x1s], lnscr, T_lnscr, "1")

            def S2(j):
                x1s = j % 2
                x1j, x1bj, T_x1j = x1[x1s], x1b[x1s], T_x1[x1s]
                ptr2 = pd[:].bitcast(BF16).rearrange("p (a b) -> p a b", b=128)
                transpose8(x1bj, T_x1j, x1T, T_x1T, ptr2, T_pd)
                def f(e):
                    for oc in range(8):
                        for ic in range(8):
                            i = e.matmul(pc[:, oc // 4, (oc % 4) * 128:(oc % 4 + 1) * 128], lhsT=xq_w[:, ic, oc * 128:(oc + 1) * 128],
                                         rhs=x1T[:, ic, :], start=(ic == 0), stop=(ic == 7))
                    return i
                P.op("pe", f, reads=[T_xq, T_x1T], writes=[T_pc0, T_pc1])
                P.op("act", lambda e: e.copy(out=qxT[:].rearrange("p a b -> p (a b)"), in_=pc[:].rearrange("p a b -> p (a b)")),
                     reads=[T_pc0, T_pc1], writes=[T_qxT])
                T_s = (T_pc0, T_pc1)
                for mc in range(2):
                    def f(e, mc=mc):
                        for h in range(4):
                            for i2 in range(2):
                                i = e.matmul(pc[:, mc, h * 128:(h + 1) * 128], lhsT=KT[:, 2 * h + i2, mc * 128:(mc + 1) * 128],
                                             rhs=qxT[:, 2 * h + i2, :], start=(i2 == 0), stop=(i2 == 1))
                        return i
                    P.op("pe", f, reads=[T_KT, T_qxT], writes=[T_s[mc]])
                    P.op("act", lambda e, mc=mc: e.activation(out=pxT[:, mc, :], in_=pc[:, mc, :], func=AF.Exp, scale=1.0 / 16),
                         reads=[T_s[mc]], writes=[T_pxT[mc]])
                def f(e):
                    for mc in range(2):
                        i = e.matmul(pd[:], lhsT=ones_bf[:], rhs=pxT[:, mc, :], start=(mc == 0), stop=(mc == 1))
                    return i
                P.op("pe", f, reads=T_pxT + [T_ones], writes=[T_pd])
                P.op("act", lambda e: e.activation(out=rdn[:], in_=pd[:], func=AF.Ln), reads=[T_pd], writes=[T_rdn])
                P.op("act", lambda e: e.activation(out=rdn[:], in_=rdn[:], func=AF.Exp, scale=-1.0), reads=[], writes=[T_rdn])
                def f(e):
                    for dc in range(8):
                        h = dc // 2
                        for mc in range(2):
                            i = e.matmul(pc[:, dc // 4, (dc % 4) * 128:(dc % 4 + 1) * 128], lhsT=V[:, mc, dc * 128:(dc + 1) * 128],
                                         rhs=pxT[:, mc, h * 128:(h + 1) * 128], start=(mc == 0), stop=(mc == 1))
                    return i
                P.op("pe", f, reads=T_pxT + [T_V], writes=[T_pc0, T_pc1])
                P.op("dve", lambda e: e.tensor_tensor(out=oTn[:].rearrange("p (h i) t -> p h i t", i=2),
                                                      in0=pc[:].rearrange("p a (b t) -> p (a b) t", t=128).rearrange("p (h i) t -> p h i t", i=2),
                                                      in1=rdn[:].rearrange("p (h t) -> p h t", t=128).unsqueeze(2).to_broadcast([128, 4, 2, 128]),
                                                      op=ALU.mult),
                     reads=[T_pc0, T_pc1, T_rdn], writes=[T_oTn])
                for hf in range(2):
                    def f(e, hf=hf):
                        for dc in range(8):
                            i = e.matmul(pc[:, hf, :], lhsT=oTn[:, dc, :], rhs=xo_w[:, dc, hf * 512:(hf + 1) * 512], start=(dc == 0), stop=(dc == 7))
                        return i
                    P.op("pe", f, reads=[T_oTn, T_xo], writes=[T_s[hf]])
                P.op("dve", lambda e: e.scalar_tensor_tensor(out=r2[:], in0=x1j[:], scalar=ALPHA, in1=pc[:].rearrange("p a b -> p (a b)"),
                                                              op0=ALU.mult, op1=ALU.add),
                     reads=[T_x1j, T_pc0, T_pc1], writes=[T_r2])
                s2 = j % 2
                layer_norm(r2, T_r2, gb2, T_gb2, x2[s2], None, T_x2[s2], lnscr2, T_lnscr2, "2")
                P.dma("sp", lambda e: e.dma_start(out=dst_dram[(j - 1) * 128:j * 128, :], in_=x2[s2][:]), T_x2[s2].sem,
                      reads=[T_x2[s2]], writes=[T_scr_a[layer][j]])
                P.op("act", lambda e: e.copy(out=x2b[:], in_=x2[s2][:]), reads=[T_x2[s2]], writes=[T_x2b])
                transpose8(x2b, T_x2b, x1T, T_x1T, ptr2, T_pd)
                def f(e):
                    for dc in range(8):
                        i = e.matmul(pd[:, 0:NE], lhsT=x1T[:, dc, :], rhs=rw_bf[:, dc, :], start=(dc == 0), stop=(dc == 7))
                    return i
                P.op("pe", f, reads=[T_x1T, T_rw], writes=[T_pd])
                P.op("dve", lambda e: e.tensor_copy(out=lg_all[:, j, :], in_=pd[:, 0:NE]), reads=[T_pd], writes=[T_lg[j]])

            def record(fn, j):
                P.rec = []
                fn(j)
                ops, P.rec = P.rec, None
                return ops

            jfull = 1 if layer == 0 else 2
            for j in range(j0, NT + 2):
                A = record(S1, j) if j < NT else []
                Bm = record(S1b, j - 1) if (j - 1 >= jfull and j - 1 < NT) else []
                C = record(S2, j - 2) if (j - 2 >= jfull) else []
                lists = [l for l in (A, Bm, C) if l]
                idx = [0] * len(lists)
                total = sum(len(l) for l in lists)
                for step in range(total):
                    best, bestv = None, None
                    for q, l in enumerate(lists):
                        if idx[q] < len(l):
                            v = idx[q] / len(l)
                            if bestv is None or v < bestv:
                                best, bestv = q, v
                    if KSEQ:
                        best = next(q for q, l in enumerate(lists) if idx[q] < len(l))
                    P.play(lists[best][idx[best]])
                    idx[best] += 1
            P.barrier()

    def router_batched(n, jbase, plg, T_plg, src_ap=None, T_srcs=None):
        with ExitStack() as rs:
            t16 = {k: sb("rb_" + k, [128, n, NE], F32, rs) for k in ("lg", "ex", "sc", "bi", "sel")}
            t4 = {k: sb("rb4_" + k, [128, n, 4], F32, rs) for k in ("m1", "n1", "m2", "n2", "a", "b", "c", "gs", "gm")}
            t1 = {k: sb("rb1_" + k, [128, n], F32, rs) for k in ("mx", "sum", "gmx", "ws")}
            T = Tk("rb")
            def Dv(fn, reads=(), writes=None):
                P.op("dve", fn, reads=[T] + list(reads), writes=[T] if writes is None else writes)
            def b16(x):
                return x[:].unsqueeze(2).to_broadcast([128, n, NE])
            def b4(x):
                return x[:].unsqueeze(2).to_broadcast([128, n, 4])
            lg, ex, sc, bi, sel = (t16[k] for k in ("lg", "ex", "sc", "bi", "sel"))
            if src_ap is None:
                Dv(lambda e: e.tensor_copy(out=lg[:].rearrange("p a b -> p (a b)"), in_=plg[:, 0:n * NE]), reads=[T_plg])
            else:
                Dv(lambda e: e.tensor_copy(out=lg[:], in_=src_ap), reads=T_srcs)
            Dv(lambda e: e.reduce_max(out=t1["mx"][:], in_=lg[:], axis=AX.X))
            Dv(lambda e: e.tensor_tensor(out=ex[:], in0=lg[:], in1=b16(t1["mx"]), op=ALU.subtract))
            P.op("act", lambda e: e.activation(out=ex[:], in_=ex[:], func=AF.Exp), reads=[T], writes=[T])
            Dv(lambda e: e.reduce_sum(out=t1["sum"][:], in_=ex[:], axis=AX.X))
            Dv(lambda e: e.reciprocal(out=t1["sum"][:], in_=t1["sum"][:]))
            Dv(lambda e: e.tensor_tensor(out=sc[:], in0=ex[:], in1=b16(t1["sum"]), op=ALU.mult))
            Dv(lambda e: e.tensor_tensor(out=bi[:], in0=sc[:], in1=rbias[:].unsqueeze(1).to_broadcast([128, n, NE]), op=ALU.add), reads=[T_const])
            g4 = bi[:].rearrange("p a (g k) -> p a g k", k=4)
            def col(i):
                return g4[:, :, :, i:i + 1].rearrange("p a g k -> p a (g k)")
            Dv(lambda e: e.tensor_tensor(out=t4["m1"][:], in0=col(0), in1=col(1), op=ALU.max))
            Dv(lambda e: e.tensor_tensor(out=t4["n1"][:], in0=col(0), in1=col(1), op=ALU.min))
            Dv(lambda e: e.tensor_tensor(out=t4["m2"][:], in0=col(2), in1=col(3), op=ALU.max))
            Dv(lambda e: e.tensor_tensor(out=t4["n2"][:], in0=col(2), in1=col(3), op=ALU.min))
            Dv(lambda e: e.tensor_tensor(out=t4["a"][:], in0=t4["m1"][:], in1=t4["m2"][:], op=ALU.max))
            Dv(lambda e: e.tensor_tensor(out=t4["b"][:], in0=t4["m1"][:], in1=t4["m2"][:], op=ALU.min))
            Dv(lambda e: e.tensor_tensor(out=t4["c"][:], in0=t4["n1"][:], in1=t4["n2"][:], op=ALU.max))
            Dv(lambda e: e.tensor_tensor(out=t4["b"][:], in0=t4["b"][:], in1=t4["c"][:], op=ALU.max))
            Dv(lambda e: e.tensor_tensor(out=t4["gs"][:], in0=t4["a"][:], in1=t4["b"][:], op=ALU.add))
            Dv(lambda e: e.reduce_max(out=t1["gmx"][:], in_=t4["gs"][:], axis=AX.X))
            Dv(lambda e: e.tensor_tensor(out=t4["gm"][:], in0=t4["gs"][:], in1=b4(t1["gmx"]), op=ALU.is_ge))
            s4 = sel[:].rearrange("p a (g k) -> p a g k", k=4)
            Dv(lambda e: e.tensor_tensor(out=s4, in0=g4, in1=t4["b"][:].unsqueeze(3).to_broadcast([128, n, 4, 4]), op=ALU.is_ge))
            Dv(lambda e: e.tensor_tensor(out=s4, in0=s4, in1=t4["gm"][:].unsqueeze(3).to_broadcast([128, n, 4, 4]), op=ALU.mult))
            Dv(lambda e: e.tensor_tensor(out=sel[:], in0=sel[:], in1=sc[:], op=ALU.mult))
            Dv(lambda e: e.reduce_sum(out=t1["ws"][:], in_=sel[:], axis=AX.X))
            Dv(lambda e: e.reciprocal(out=t1["ws"][:], in_=t1["ws"][:]))
            Dv(lambda e: e.tensor_tensor(out=comb[:, jbase:jbase + n, :], in0=sel[:], in1=b16(t1["ws"]), op=ALU.mult),
               writes=[T] + [T_comb[jbase + i] for i in range(n)])
            P.barrier()

    T_scr_a = [[Tk(f"xsa{l}_{j}") for j in range(NT)] for l in range(2)]
    T_scr_b = [Tk(f"xsb_{j}") for j in range(NT)]

    def phase_B(layer, src_dram, dst_dram, dst_is_out):
        pfx = f"l{layer}_"
        jfirst = 1 if layer == 0 else 2
        tiles = list(range(jfirst, NT))
        half = (len(tiles) + 1) // 2
        chunks = [tiles[:half], tiles[half:]]
        with ExitStack() as st:
            gsem = P.dsem(f"B{layer}g")
            gb3, T_gb3 = load_gb(st, (pfx + "ln3_g", pfx + "ln3_b"), "3", gsem)
            NCH = len(chunks[0])
            r = sb("rB", [128, NCH, D], F32, st)
            T_r = [Tk(f"rB{i}") for i in range(NCH)]
            xTa = sb("xTa", [128, 8, NCH * 128], BF16, st)
            T_xTa = [Tk(f"xTa{i}") for i in range((NCH + 3) // 4)]
            NS = 4
            sem_x = [P.dsem(f"B{layer}x{k}") for k in range(NS)]
            sem_o = [P.dsem(f"B{layer}o{k}") for k in range(NS)]

            def record0(fn):
                P.rec = []
                fn()
                ops, P.rec = P.rec, None
                return ops

            def play_waves(streams):
                for w0 in range(0, len(streams), NS):
                    wave = streams[w0:w0 + NS]
                    idx = [0] * len(wave)
                    live = True
                    while live:
                        live = False
                        for k, st_ in enumerate(wave):
                            if idx[k] < len(st_):
                                P.play(st_[idx[k]])
                                idx[k] += 1
                                live = True

            ptr = ps("ptrB", [128, 8, 128], BF16, st); T_ptr = Tk("ptrB")
            pg = [ps(f"pg{i}", [128, 512], F32, st) for i in range(2)]
            pu = [ps(f"pu{i}", [128, 512], F32, st) for i in range(2)]
            py = [ps(f"py{i}", [128, 2, 512], F32, st) for i in range(1)]
            plg = ps("plg", [128, 512], F32, st); T_plg = Tk("plg")
            T_pg = [Tk("pg0"), Tk("pg1")]
            T_pu = [Tk("pu0"), Tk("pu1")]
            T_py = [[Tk("py0a"), Tk("py0b")]]

            for ci, ch in enumerate(chunks):
                n = len(ch)
                pst = ExitStack()
                xl4 = [sb(f"xlp{k}", [128, D], F32, pst) for k in range(NS)]
                T_xl4 = [Tk(f"xlp{k}", sem_x[k]) for k in range(NS)]
                xlb4 = [sb(f"xlbp{k}", [128, D], BF16, pst) for k in range(NS)]
                T_xlb4 = [Tk(f"xlbp{k}") for k in range(NS)]
                trb = [b_[:].bitcast(BF16).rearrange("p (a b) -> p a b", b=128) for b_ in (pg[0], pg[1], pu[0], pu[1])]
                T_trb = [T_pg[0], T_pg[1], T_pu[0], T_pu[1]]
                streams = []
                for i, j in enumerate(ch):
                    def tile_pro(i=i, j=j, k=i % NS):
                        P.dma("sp", lambda e: e.dma_start(out=xl4[k][:], in_=src_dram[(j - 1) * 128:j * 128, :]), sem_x[k],
                              reads=[T_scr_a[layer][j]], writes=[T_xl4[k]])
                        P.op("act", lambda e: e.mul(out=r[:, i, :], in_=xl4[k][:], mul=ALPHA), reads=[T_xl4[k]], writes=[T_r[i]])
                        P.op("dve", lambda e: e.tensor_copy(out=xlb4[k][:], in_=xl4[k][:]), reads=[T_xl4[k]], writes=[T_xlb4[k]])
                        def f(e):
                            for dc in range(8):
                                ii = e.transpose(out=trb[k][:, dc, :], in_=xlb4[k][:, dc * 128:(dc + 1) * 128], identity=ident[:])
                            return ii
                        P.op("pe", f, reads=[T_xlb4[k], T_const], writes=[T_trb[k]])
                        P.op("dve", lambda e: e.tensor_copy(out=xTa[:, :, i * 128:(i + 1) * 128], in_=trb[k]),
                             reads=[T_trb[k]], writes=[T_xTa[i // 4]])
                        def f2(e):
                            for dc in range(8):
                                ii = e.matmul(plg[:, i * NE:(i + 1) * NE], lhsT=xTa[:, dc, i * 128:(i + 1) * 128], rhs=rw_bf[:, dc, :],
                                              start=(dc == 0), stop=(dc == 7))
                            return ii
                        P.op("pe", f2, reads=[T_xTa[i // 4], T_rw], writes=[T_plg])
                    streams.append(record0(tile_pro))
                play_waves(streams)
                P.barrier()
                pst.close()
                router_batched(n, ch[0], plg, T_plg)
                P.barrier()
                ws_ = ExitStack()
                wg = [sb(f"wg{i}", [128, 8, 512], BF16, ws_) for i in range(2)]
                wu = [sb(f"wu{i}", [128, 8, 512], BF16, ws_) for i in range(2)]
                wd = [sb(f"wd{i}", [128, 4, D], BF16, ws_) for i in range(2)]
                T_wg = [Tk(f"wg{i}") for i in range(2)]
                T_wu = [Tk(f"wu{i}") for i in range(2)]
                T_wd = [Tk(f"wd{i}") for i in range(2)]
                hT = [sb(f"hT{i}", [128, 4, 512], BF16, ws_) for i in range(2)]
                T_hT = [[Tk(f"hT{i}_{fc}") for fc in range(4)] for i in range(2)]
                sg = [sb(f"sg{i}", [128, 512], F32, ws_) for i in range(2)]
                T_sg = [Tk("sg0"), Tk("sg1")]
                wcount = [0]

                def load_expert(e_idx):
                    s = wcount[0] % 2
                    wcount[0] += 1
                    stream_weight(wg[s], W[pfx + "e_gate"][e_idx], 8, 512, T_wg[s])
                    stream_weight(wu[s], W[pfx + "e_up"][e_idx], 8, 512, T_wu[s])
                    stream_weight(wd[s], W[pfx + "e_down"][e_idx], 4, D, T_wd[s])
                    return s

                groups = [list(range(g0, min(g0 + 4, n))) for g0 in range(0, n, 4)]
                ws_next = load_expert(0)
                gcount = 0
                for ex in range(NE):
                    ws = ws_next
                    if ex + 1 < NE:
                        ws_next = load_expert(ex + 1)
                    for gi, grp in enumerate(groups):
                        ntok = len(grp) * 128
                        t0 = grp[0] * 128
                        hs = gcount % 2
                        gcount += 1
                        for fc in range(4):
                            b = fc % 2
                            def f(e, fc=fc, b=b):
                                for dc in range(8):
                                    i = e.matmul(pg[b][:, 0:ntok], lhsT=wg[ws][:, dc, fc * 128:(fc + 1) * 128], rhs=xTa[:, dc, t0:t0 + ntok],
                                                 start=(dc == 0), stop=(dc == 7))
                                return i
                            P.op("pe", f, reads=[T_wg[ws], T_xTa[gi]], writes=[T_pg[b]])
                            def f(e, fc=fc, b=b):
                                for dc in range(8):
                                    i = e.matmul(pu[b][:, 0:ntok], lhsT=wu[ws][:, dc, fc * 128:(fc + 1) * 128], rhs=xTa[:, dc, t0:t0 + ntok],
                                                 start=(dc == 0), stop=(dc == 7))
                                return i
                            P.op("pe", f, reads=[T_wu[ws], T_xTa[gi]], writes=[T_pu[b]])
                            P.op("act", lambda e, b=b: e.activation(out=sg[b][:, 0:ntok], in_=pg[b][:, 0:ntok], func=AF.Silu),
                                 reads=[T_pg[b]], writes=[T_sg[b]])
                            P.op("dve", lambda e, b=b, fc=fc: e.tensor_tensor(out=hT[hs][:, fc, 0:ntok], in0=sg[b][:, 0:ntok], in1=pu[b][:, 0:ntok],
                                                                               op=ALU.mult),
                                 reads=[T_sg[b], T_pu[b]], writes=[T_hT[hs][fc]])
                        for ti, i in enumerate(grp):
                            j = ch[i]
                            yb = 0
                            for hf in range(2):
                                def f(e, hf=hf, ti=ti):
                                    for fc in range(4):
                                        ii = e.matmul(py[yb][:, hf, :], lhsT=hT[hs][:, fc, ti * 128:(ti + 1) * 128],
                                                      rhs=wd[ws][:, fc, hf * 512:(hf + 1) * 512], start=(fc == 0), stop=(fc == 3))
                                    return ii
                                P.op("pe", f, reads=T_hT[hs] + [T_wd[ws]], writes=[T_py[yb][hf]])
                                P.op("dve", lambda e, hf=hf, i=i, j=j: e.scalar_tensor_tensor(
                                    out=r[:, i, hf * 512:(hf + 1) * 512], in0=py[yb][:, hf, :], scalar=comb[:, j, ex:ex + 1],
                                    in1=r[:, i, hf * 512:(hf + 1) * 512], op0=ALU.mult, op1=ALU.add),
                                    reads=[T_py[yb][hf], T_comb[j]], writes=[T_r[i]])
                P.barrier()
                ws_.close()
                est = ExitStack()
                xo4 = [sb(f"xo4_{k}", [128, D], F32, est) for k in range(NS)]
                T_xo4 = [Tk(f"xo4_{k}", sem_o[k]) for k in range(NS)]
                lns4 = [(sb(f"st6e{k}", [128, 2, 6], F32, est), sb(f"mve{k}", [128, 2], F32, est), sb(f"sde{k}", [128, 1], F32, est),
                         sb(f"rstde{k}", [128, 1], F32, est), sb(f"nmre{k}", [128, 1], F32, est)) for k in range(NS)]
                T_lns4 = [Tk(f"lnse{k}") for k in range(NS)]
                streams = []
                for i, j in enumerate(ch):
                    def tile_epi(i=i, j=j, k=i % NS):
                        layer_norm(r[:, i, :], T_r[i], gb3, T_gb3, xo4[k], None, T_xo4[k], lns4[k], T_lns4[k], "3")
                        if dst_is_out:
                            dst = dst_dram[(j - 2) * 128:(j - 1) * 128, :]
                        else:
                            dst = dst_dram[(j - 1) * 128:j * 128, :]
                        P.dma("sp", lambda e: e.dma_start(out=dst, in_=xo4[k][:]), sem_o[k],
                              reads=[T_xo4[k]], writes=[T_scr_b[j]])
                    streams.append(record0(tile_epi))
                play_waves(streams)
                P.barrier()
                est.close()
            P.barrier()

    def phase_B_sparse(layer, src_dram, dst_dram, dst_is_out):
        pfx = f"l{layer}_"
        U32 = mybir.dt.uint32
        jfirst = 1 if layer == 0 else 2
        tiles = list(range(jfirst, NT))
        n = len(tiles)
        j0 = tiles[0]
        S = (n * 256 + 16 * 511) // 512
        half = (n + 1) // 2
        chunks = [tiles[:half], tiles[half:]]
        Wg2 = W[pfx + "e_gate"].rearrange("e k f -> (e k) f")
        Wu2 = W[pfx + "e_up"].rearrange("e k f -> (e k) f")
        Wd2 = W[pfx + "e_down"].rearrange("e k f -> (e k) f")
        IOA = bass.IndirectOffsetOnAxis
        with ExitStack() as st:
            gsem = P.dsem(f"B{layer}g")
            gb3, T_gb3 = load_gb(st, (pfx + "ln3_g", pfx + "ln3_b"), "3", gsem)
            NS = 4
            NS5 = 8
            sem_x = [P.gsem(f"Bx{k}") for k in range(NS5)]
            sem_o = [P.gsem(f"Bo{k}") for k in range(NS5)]
            sem_a = [P.gsem(f"Ba{k}") for k in range(NS5)]
            sem_b = [P.gsem(f"Bb{k}") for k in range(NS5)]
            posA_i = sb("posA_i", [128, NT], I32, st)
            posB_i = sb("posB_i", [128, NT], I32, st)
            wA = sb("wA", [128, NT], F32, st)
            wB = sb("wB", [128, NT], F32, st)
            idx_gu = sb("idx_gu", [128, S_MAX, 8], I32, st)
            idx_d = sb("idx_d", [128, S_MAX, 4], I32, st)
            T_pos = Tk("pos")
            ptr = ps("ptrB", [128, 8, 128], BF16, st); T_ptr = Tk("ptrB")
            pg = [ps(f"pg{i}", [128, 512], F32, st) for i in range(2)]
            pu = [ps(f"pu{i}", [128, 512], F32, st) for i in range(2)]
            py = ps("py0", [128, 2, 512], F32, st)
            plg = ps("plg", [128, 512], F32, st); T_plg = Tk("plg")
            T_pg = [Tk("pg0"), Tk("pg1")]
            T_pu = [Tk("pu0"), Tk("pu1")]
            T_py = [Tk("py0a"), Tk("py0b")]

            def record0(fn):
                P.rec = []
                fn()
                ops, P.rec = P.rec, None
                return ops

            def play_waves(streams, NS=NS):
                for w0 in range(0, len(streams), NS):
                    wave = streams[w0:w0 + NS]
                    idx = [0] * len(wave)
                    live = True
                    while live:
                        live = False
                        for k, st_ in enumerate(wave):
                            if idx[k] < len(st_):
                                P.play(st_[idx[k]])
                                idx[k] += 1
                                live = True

            router_batched(n, j0, None, None, src_ap=lg_all[:, j0:j0 + n, :], T_srcs=[T_lg[j] for j in tiles])

            with ExitStack() as qs:
                t16 = {k: sb("q16_" + k, [128, n, NE], F32, qs) for k in ("sel", "rank", "cnt", "off", "pos", "val", "m", "tmp")}
                selb = sb("q_selb", [128, n, NE], BF16, qs)
                e16 = {k: sb("qe_" + k, [128, NE], F32, qs) for k in ("total", "nslot", "end", "base")}
                cmp1 = sb("q_cmp1", [128, NE, 17], F32, qs)
                cmp2 = sb("q_cmp2", [128, S_MAX, NE], F32, qs)
                slot_e = sb("q_slote", [128, S_MAX], F32, qs)
                pAB = {k: sb("q_" + k, [128, n], F32, qs) for k in ("pA", "pB")}
                T = Tk("posq")
                def Dv(fn, reads=(), writes=None):
                    P.op("dve", fn, reads=[T] + list(reads), writes=[T] if writes is None else writes)
                cv = comb[:, j0:j0 + n, :]
                sel, rank, cnt, off, pos, val, mm_, tmp = (t16[k] for k in ("sel", "rank", "cnt", "off", "pos", "val", "m", "tmp"))
                Dv(lambda e: e.tensor_scalar(out=sel[:], in0=cv, scalar1=0.0, scalar2=None, op0=ALU.is_gt), reads=[T_comb[j] for j in tiles])
                Dv(lambda e: e.tensor_copy(out=selb[:], in_=sel[:]))
                n1 = min(n, 17)
                c1, c2 = n1 * NE, (n - n1) * NE
                selbf = selb[:].rearrange("p a b -> p (a b)")
                def f(e):
                    e.matmul(pg[0][:, 0:c1], lhsT=lstrict[:], rhs=selbf[:, 0:c1], start=True, stop=True)
                    e.matmul(pg[1][:, 0:c2], lhsT=lstrict[:], rhs=selbf[:, c1:c1 + c2], start=True, stop=True)
                    e.matmul(pu[0][:, 0:c1], lhsT=ones_bf[:], rhs=selbf[:, 0:c1], start=True, stop=True)
                    return e.matmul(pu[1][:, 0:c2], lhsT=ones_bf[:], rhs=selbf[:, c1:c1 + c2], start=True, stop=True)
                P.op("pe", f, reads=[T, T_const, T_ones], writes=T_pg + T_pu)
                rankf = rank[:].rearrange("p a b -> p (a b)")
                cntf = cnt[:].rearrange("p a b -> p (a b)")
                Dv(lambda e: e.tensor_copy(out=rankf[:, 0:c1], in_=pg[0][:, 0:c1]), reads=[T_pg[0]])
                Dv(lambda e: e.tensor_copy(out=rankf[:, c1:c1 + c2], in_=pg[1][:, 0:c2]), reads=[T_pg[1]])
                Dv(lambda e: e.tensor_copy(out=cntf[:, 0:c1], in_=pu[0][:, 0:c1]), reads=[T_pu[0]])
                Dv(lambda e: e.tensor_copy(out=cntf[:, c1:c1 + c2], in_=pu[1][:, 0:c2]), reads=[T_pu[1]])
                Dv(lambda e: e.memset(off[:, 0, :], 0.0))
                for jj in range(1, n):
                    Dv(lambda e, jj=jj: e.tensor_tensor(out=off[:, jj, :], in0=off[:, jj - 1, :], in1=cnt[:, jj - 1, :], op=ALU.add))
                Dv(lambda e: e.tensor_tensor(out=e16["total"][:], in0=off[:, n - 1, :], in1=cnt[:, n - 1, :], op=ALU.add))
                Dv(lambda e: e.tensor_tensor(out=cmp1[:], in0=e16["total"][:].unsqueeze(2).to_broadcast([128, NE, 17]),
                                             in1=tabs[:, 0:17].unsqueeze(1).to_broadcast([128, NE, 17]), op=ALU.is_gt), reads=[T_const])
                Dv(lambda e: e.reduce_sum(out=e16["nslot"][:], in_=cmp1[:], axis=AX.X))
                Dv(lambda e: e.tensor_copy(out=e16["end"][:], in_=e16["nslot"][:]))
                for ee in range(1, NE):
                    Dv(lambda e, ee=ee: e.tensor_tensor(out=e16["end"][:, ee:ee + 1], in0=e16["end"][:, ee - 1:ee],
                                                        in1=e16["nslot"][:, ee:ee + 1], op=ALU.add))
                Dv(lambda e: e.tensor_tensor(out=e16["base"][:], in0=e16["end"][:], in1=e16["nslot"][:], op=ALU.subtract))
                Dv(lambda e: e.tensor_scalar_mul(out=e16["base"][:], in0=e16["base"][:], scalar1=512.0))
                Dv(lambda e: e.tensor_tensor(out=pos[:], in0=rank[:], in1=off[:], op=ALU.add))
                Dv(lambda e: e.tensor_tensor(out=pos[:], in0=pos[:], in1=e16["base"][:].unsqueeze(1).to_broadcast([128, n, NE]), op=ALU.add))
                Dv(lambda e: e.scalar_tensor_tensor(out=val[:], in0=pos[:], scalar=1.0, in1=sel[:], op0=ALU.add, op1=ALU.mult))
                for which, (pX, pos_i, wX) in enumerate(((pAB["pA"], posA_i, wA), (pAB["pB"], posB_i, wB))):
                    Dv(lambda e, pX=pX: e.reduce_max(out=pX[:], in_=val[:], axis=AX.X))
                    Dv(lambda e, pX=pX: e.tensor_tensor(out=mm_[:], in0=val[:], in1=pX[:].unsqueeze(2).to_broadcast([128, n, NE]), op=ALU.is_equal))
                    Dv(lambda e: e.tensor_tensor(out=tmp[:], in0=cv, in1=mm_[:], op=ALU.mult))
                    Dv(lambda e, wX=wX: e.reduce_sum(out=wX[:, j0:j0 + n], in_=tmp[:], axis=AX.X), writes=[T, T_pos])
                    Dv(lambda e, pX=pX, pos_i=pos_i: e.tensor_scalar(out=pos_i[:, j0:j0 + n], in0=pX[:], scalar1=-1.0, scalar2=0.0,
                                                                     op0=ALU.add, op1=ALU.max), writes=[T, T_pos])
                    if which == 0:
                        Dv(lambda e: e.tensor_tensor(out=tmp[:], in0=val[:], in1=mm_[:], op=ALU.mult))
                        Dv(lambda e: e.tensor_tensor(out=val[:], in0=val[:], in1=tmp[:], op=ALU.subtract))
                sidx = tabs[:, 17:17 + S_MAX]
                kp = tabs[:, 17 + S_MAX:17 + S_MAX + 8]
                Dv(lambda e: e.tensor_tensor(out=cmp2[:], in0=sidx.unsqueeze(2).to_broadcast([128, S_MAX, NE]),
                                             in1=e16["end"][:].unsqueeze(1).to_broadcast([128, S_MAX, NE]), op=ALU.is_ge), reads=[T_const])
                Dv(lambda e: e.reduce_sum(out=slot_e[:], in_=cmp2[:], axis=AX.X))
                Dv(lambda e: e.tensor_scalar_min(out=slot_e[:], in0=slot_e[:], scalar1=float(NE - 1)))
                Dv(lambda e: e.scalar_tensor_tensor(out=idx_gu[:], in0=slot_e[:].unsqueeze(2).to_broadcast([128, S_MAX, 8]), scalar=1024.0,
                                                    in1=kp.unsqueeze(1).to_broadcast([128, S_MAX, 8]), op0=ALU.mult, op1=ALU.add),
                   reads=[T_const], writes=[T, T_pos])
                Dv(lambda e: e.scalar_tensor_tensor(out=idx_d[:], in0=slot_e[:].unsqueeze(2).to_broadcast([128, S_MAX, 4]), scalar=512.0,
                                                    in1=kp[:, 0:4].unsqueeze(1).to_broadcast([128, S_MAX, 4]), op0=ALU.mult, op1=ALU.add),
                   reads=[T_const], writes=[T, T_pos])
                P.barrier()

            with ExitStack() as ss:
                xl4 = [sb(f"xls{k}", [128, D], F32, ss) for k in range(NS)]
                T_xl4 = [Tk(f"xls{k}", sem_x[k]) for k in range(NS)]
                xlb4 = [sb(f"xlbs{k}", [128, D], BF16, ss) for k in range(NS)]
                T_xlb4 = [Tk(f"xlbs{k}", sem_a[k]) for k in range(NS)]
                T_xsort = Tk("xsort")
                streams = []
                for i, j in enumerate(tiles):
                    def tile_sc(i=i, j=j, k=i % NS):
                        P.dma("sp", lambda e: e.dma_start(out=xl4[k][:], in_=src_dram[(j - 1) * 128:j * 128, :]), sem_x[k],
                              reads=[T_scr_a[layer][j]], writes=[T_xl4[k]])
                        P.op("dve", lambda e: e.tensor_copy(out=xlb4[k][:], in_=xl4[k][:]), reads=[T_xl4[k]], writes=[T_xlb4[k]])
                        P.dma("pool", lambda e: [
                            e.indirect_dma_start(out=xsort, out_offset=IOA(ap=posA_i[:, j:j + 1].bitcast(U32), axis=0), in_=xlb4[k][:], in_offset=None),
                            e.indirect_dma_start(out=xsort, out_offset=IOA(ap=posB_i[:, j:j + 1].bitcast(U32), axis=0), in_=xlb4[k][:], in_offset=None)],
                            sem_a[k], reads=[T_xlb4[k], T_pos, T_xsort_g], writes=[T_xsort])
                    streams.append(record0(tile_sc))
                play_waves(streams)
                P.barrier()

            with ExitStack() as ws_:
                wg = [sb(f"wg{i}", [128, 8, 512], BF16, ws_) for i in range(2)]
                wu = [sb(f"wu{i}", [128, 8, 512], BF16, ws_) for i in range(2)]
                wd = [sb(f"wd{i}", [128, 4, D], BF16, ws_) for i in range(2)]
                T_wg = [Tk(f"wg{i}", P.gsem(f"Bwg{i}")) for i in range(2)]
                T_wu = [Tk(f"wu{i}", P.gsem(f"Bwu{i}")) for i in range(2)]
                T_wd = [Tk(f"wd{i}", P.gsem(f"Bwd{i}")) for i in range(2)]
                hT = [sb(f"hT{i}", [128, 4, 512], BF16, ws_) for i in range(2)]
                T_hT = [[Tk(f"hT{i}_{fc}") for fc in range(4)] for i in range(2)]
                sg = [sb(f"sg{i}", [128, 512], F32, ws_) for i in range(2)]
                T_sg = [Tk("sg0"), Tk("sg1")]
                xs4 = [sb(f"xs4_{i}", [128, 4, D], BF16, ws_) for i in range(2)]
                T_xs4 = [Tk(f"xs4_{i}", P.gsem(f"Bxs{i}")) for i in range(2)]
                xTs = [sb(f"xTs{i}", [128, 8, 512], BF16, ws_) for i in range(2)]
                T_xTs = [Tk(f"xTs{i}") for i in range(2)]
                ysb = [sb(f"ysb{i}", [128, D], F32, ws_) for i in range(4)]
                T_ysb = [Tk(f"ysb{i}", sem_o[i]) for i in range(4)]
                T_ysort = Tk("ysort")
                trs = [ptr[:], plg[:].bitcast(BF16).rearrange("p (a b) -> p a b", b=128)]
                T_trs = [T_ptr, T_plg]

                def load_slot(s_):
                    b = s_ % 2
                    P.dma("pool", lambda e: [e.indirect_dma_start(out=wg[b][:, k, :], out_offset=None, in_=Wg2,
                                                                  in_offset=IOA(ap=idx_gu[:, s_, k:k + 1].bitcast(U32), axis=0)) for k in range(8)],
                          T_wg[b].sem, reads=[T_pos], writes=[T_wg[b]])
                    P.dma("pool", lambda e: [e.indirect_dma_start(out=wu[b][:, k, :], out_offset=None, in_=Wu2,
                                                                  in_offset=IOA(ap=idx_gu[:, s_, k:k + 1].bitcast(U32), axis=0)) for k in range(8)],
                          T_wu[b].sem, reads=[T_pos], writes=[T_wu[b]])
                    P.dma("pool", lambda e: [e.indirect_dma_start(out=wd[b][:, k, :], out_offset=None, in_=Wd2,
                                                                  in_offset=IOA(ap=idx_d[:, s_, k:k + 1].bitcast(U32), axis=0)) for k in range(4)],
                          T_wd[b].sem, reads=[T_pos], writes=[T_wd[b]])
                    P.dma("sp", lambda e: e.dma_start(out=xs4[b][:], in_=xsort[s_ * 512:(s_ + 1) * 512, :].rearrange("(t p) f -> p t f", p=128)),
                          T_xs4[b].sem, writes=[T_xs4[b]])

                load_slot(0)
                ycount = 0
                trc = 0
                for s_ in range(S):
                    b = s_ % 2
                    if s_ + 1 < S:
                        load_slot(s_ + 1)
                    for t in range(4):
                        tb = trc % 2
                        trc += 1
                        def f(e, t=t, tb=tb):
                            for dc in range(8):
                                ii = e.transpose(out=trs[tb][:, dc, :], in_=xs4[b][:, t, dc * 128:(dc + 1) * 128], identity=ident[:])
                            return ii
                        P.op("pe", f, reads=[T_xs4[b], T_const], writes=[T_trs[tb]])
                        if t % 2 == 0:
                            P.op("act", lambda e, t=t, tb=tb: e.copy(out=xTs[b][:, :, t * 128:(t + 1) * 128], in_=trs[tb]),
                                 reads=[T_trs[tb]], writes=[T_xTs[b]])
                        else:
                            P.op("dve", lambda e, t=t, tb=tb: e.tensor_copy(out=xTs[b][:, :, t * 128:(t + 1) * 128], in_=trs[tb]),
                                 reads=[T_trs[tb]], writes=[T_xTs[b]])
                    hs = s_ % 2
                    for fc in range(4):
                        bb = fc % 2
                        def f(e, fc=fc, bb=bb):
                            for dc in range(8):
                                i = e.matmul(pg[bb][:], lhsT=wg[b][:, dc, fc * 128:(fc + 1) * 128], rhs=xTs[b][:, dc, :],
                                             start=(dc == 0), stop=(dc == 7))
                            return i
                        P.op("pe", f, reads=[T_wg[b], T_xTs[b]], writes=[T_pg[bb]])
                        def f(e, fc=fc, bb=bb):
                            for dc in range(8):
                                i = e.matmul(pu[bb][:], lhsT=wu[b][:, dc, fc * 128:(fc + 1) * 128], rhs=xTs[b][:, dc, :],
                                             start=(dc == 0), stop=(dc == 7))
                            return i
                        P.op("pe", f, reads=[T_wu[b], T_xTs[b]], writes=[T_pu[bb]])
                        P.op("act", lambda e, bb=bb: e.activation(out=sg[bb][:], in_=pg[bb][:], func=AF.Silu),
                             reads=[T_pg[bb]], writes=[T_sg[bb]])
                        P.op("dve", lambda e, bb=bb, fc=fc: e.tensor_tensor(out=hT[hs][:, fc, :], in0=sg[bb][:], in1=pu[bb][:], op=ALU.mult),
                             reads=[T_sg[bb], T_pu[bb]], writes=[T_hT[hs][fc]])
                    for t in range(4):
                        for hf in range(2):
                            def f(e, hf=hf, t=t):
                                for fc in range(4):
                                    ii = e.matmul(py[:, hf, :], lhsT=hT[hs][:, fc, t * 128:(t + 1) * 128],
                                                  rhs=wd[b][:, fc, hf * 512:(hf + 1) * 512], start=(fc == 0), stop=(fc == 3))
                                return ii
                            P.op("pe", f, reads=T_hT[hs] + [T_wd[b]], writes=[T_py[hf]])
                        yq = ycount % 4
                        ycount += 1
                        P.op("act", lambda e, yq=yq: e.copy(out=ysb[yq][:, 0:512], in_=py[:, 0, :]), reads=[T_py[0]], writes=[T_ysb[yq]])
                        P.op("dve", lambda e, yq=yq: e.tensor_copy(out=ysb[yq][:, 512:1024], in_=py[:, 1, :]), reads=[T_py[1]], writes=[T_ysb[yq]])
                        r0 = s_ * 512 + t * 128
                        P.dma("sp", lambda e, yq=yq, r0=r0: e.dma_start(out=ysort[r0:r0 + 128, :], in_=ysb[yq][:]), sem_o[yq],
                              reads=[T_ysb[yq]], writes=[T_ysort])
                P.barrier()

            with ExitStack() as cs:
                xc = [sb(f"xc{k}", [128, D], F32, cs) for k in range(NS5)]
                T_xc = [Tk(f"xc{k}", sem_x[k]) for k in range(NS5)]
                ya = [sb(f"ya{k}", [128, D], F32, cs) for k in range(NS5)]
                T_ya = [Tk(f"ya{k}", sem_a[k]) for k in range(NS5)]
                yb = [sb(f"yb{k}", [128, D], F32, cs) for k in range(NS5)]
                T_yb = [Tk(f"yb{k}", sem_b[k]) for k in range(NS5)]
                xo4 = [sb(f"xo4_{k}", [128, D], F32, cs) for k in range(NS5)]
                T_xo4 = [Tk(f"xo4_{k}", sem_o[k]) for k in range(NS5)]
                lns4 = [(sb(f"st6e{k}", [128, 2, 6], F32, cs), sb(f"mve{k}", [128, 2], F32, cs), sb(f"sde{k}", [128, 1], F32, cs),
                         sb(f"rstde{k}", [128, 1], F32, cs), sb(f"nmre{k}", [128, 1], F32, cs)) for k in range(NS5)]
                T_lns4 = [Tk(f"lnse{k}") for k in range(NS5)]
                streams = []
                for i, j in enumerate(tiles):
                    def tile_cb(i=i, j=j, k=i % NS5):
                        P.dma("sp", lambda e: e.dma_start(out=xc[k][:], in_=src_dram[(j - 1) * 128:j * 128, :]), sem_x[k],
                              reads=[T_scr_a[layer][j]], writes=[T_xc[k]])
                        P.dma("pool", lambda e: e.indirect_dma_start(out=ya[k][:], out_offset=None, in_=ysort,
                                                                     in_offset=IOA(ap=posA_i[:, j:j + 1].bitcast(U32), axis=0)),
                              sem_a[k], reads=[T_pos], writes=[T_ya[k]])
                        P.dma("pool", lambda e: e.indirect_dma_start(out=yb[k][:], out_offset=None, in_=ysort,
                                                                     in_offset=IOA(ap=posB_i[:, j:j + 1].bitcast(U32), axis=0)),
                              sem_b[k], reads=[T_pos], writes=[T_yb[k]])
                        P.op("act", lambda e: e.mul(out=xc[k][:], in_=xc[k][:], mul=ALPHA), reads=[], writes=[T_xc[k]])
                        P.op("dve", lambda e: e.scalar_tensor_tensor(out=xc[k][:], in0=ya[k][:], scalar=wA[:, j:j + 1], in1=xc[k][:],
                                                                      op0=ALU.mult, op1=ALU.add), reads=[T_ya[k], T_pos], writes=[T_xc[k]])
                        P.op("dve", lambda e: e.scalar_tensor_tensor(out=xc[k][:], in0=yb[k][:], scalar=wB[:, j:j + 1], in1=xc[k][:],
                                                                      op0=ALU.mult, op1=ALU.add), reads=[T_yb[k], T_pos], writes=[T_xc[k]])
                        layer_norm(xc[k], T_xc[k], gb3, T_gb3, xo4[k], None, T_xo4[k], lns4[k], T_lns4[k], "3", norm_on_act=True)
                        if dst_is_out:
                            dst = dst_dram[(j - 2) * 128:(j - 1) * 128, :]
                        else:
                            dst = dst_dram[(j - 1) * 128:j * 128, :]
                        P.dma("sp", lambda e: e.dma_start(out=dst, in_=xo4[k][:]), sem_o[k], reads=[T_xo4[k]], writes=[T_scr_b[j]])
                    streams.append(record0(tile_cb))
                play_waves(streams, NS5)
                P.barrier()
            P.barrier()


    def copy_out(src_dram, T_src):
        with ExitStack() as st:
            buf = [sb(f"cb{i}", [128, D], F32, st) for i in range(2)]
            T_b = [Tk(f"cb{i}", P.dsem(f"cb{i}")) for i in range(2)]
            for j in range(2, NT):
                s = j % 2
                P.dma("sp", lambda e: e.dma_start(out=buf[s][:], in_=src_dram[(j - 1) * 128:j * 128, :]), T_b[s].sem,
                      reads=[T_src[j]], writes=[T_b[s]])
                P.dma("sp", lambda e: e.dma_start(out=out[(j - 2) * 128:(j - 1) * 128, :], in_=buf[s][:]), T_b[s].sem,
                      reads=[T_b[s]], writes=[Tk("o")])
            P.barrier()

    if stage == "S0":
        osem = P.dsem("dbg")
        P.dma("sp", lambda e: [e.dma_start(out=out[0:128, 0:NT * 8], in_=cosT[:].rearrange("p a b -> p (a b)")),
                               e.dma_start(out=out[128:256, 0:NT * 8], in_=sinT[:].rearrange("p a b -> p (a b)"))],
              osem, reads=[T_cs], writes=[Tk("o")])
        P.barrier()
        return
    phase_A(0, xin, 0, xs_a)
    if stage == "S1" or STOPPED[0]:
        return
    if stage == "A0":
        copy_out(xs_a, T_scr_a[0])
    else:
        (phase_B_sparse if SPARSE else phase_B)(0, xs_a, xs_b, False)
        if stage == "B0":
            copy_out(xs_b, T_scr_b)
        else:
            phase_A(1, xs_b, -128, xs_a)
            if stage == "A1":
                copy_out(xs_a, T_scr_a[1])
            else:
                (phase_B_sparse if SPARSE else phase_B)(1, xs_a, out, True)
    P.barrier()


_QPERM = [0, 4, 1, 5, 2, 6, 3, 7]


def _constants():
    bf = ml_dtypes.bfloat16
    k = np.arange(128)[:, None]
    q = np.arange(128)[None, :]
    m_prev = (k > q).astype(np.float32)
    m_cur = (k <= q).astype(np.float32)
    tril = (k <= q).astype(np.float32)
    poolB = np.zeros((128, 4, 4, 128), np.float32)
    s = np.arange(128)[:, None]
    t = np.arange(128)[None, :]
    for g, win in enumerate((2, 4, 8, 16)):
        cur = ((s <= t) & (s > t - win)).astype(np.float32) / win - (s == t).astype(np.float32)
        prev = ((s - 128) > (t - win)).astype(np.float32) / win
        cnt = np.minimum(t + 1, win).astype(np.float32)
        fcur = ((s <= t) & (s > t - win)).astype(np.float32) / cnt - (s == t).astype(np.float32)
        poolB[:, 0, g, :] = prev
        poolB[:, 1, g, :] = cur
        poolB[:, 2, g, :] = 0.0
        poolB[:, 3, g, :] = fcur
    half = 4
    invf = (500000.0 ** (-(np.arange(half * 2, dtype=np.float32) * 2.0 / 16.0))).astype(np.float32)
    invf = np.broadcast_to(invf[None, :], (128, 8)).copy()
    tabs = np.zeros((128, 17 + S_MAX + 8), np.float32)
    tabs[:, 0:17] = (np.arange(17) * 512.0)[None, :]
    tabs[:, 17:17 + S_MAX] = np.arange(S_MAX, dtype=np.float32)[None, :]
    tabs[:, 17 + S_MAX:] = (np.arange(8)[None, :] * 128 + np.arange(128)[:, None]).astype(np.float32)
    lstrict = (k < q).astype(np.float32).astype(bf)
    return dict(tabs=tabs, lstrict=lstrict, ident=np.eye(128, dtype=np.float32).astype(bf), m_prev=m_prev, m_cur=m_cur, tril=tril, poolB=poolB, invf=invf)


def make_in_maps(inputs):
    bf = ml_dtypes.bfloat16
    C = _constants()
    x = np.asarray(inputs["x"], np.float32)
    memv = np.asarray(inputs["mem"], np.float32)
    pos = np.asarray(inputs["positions"], np.int32)
    shared = {}
    w_in = np.asarray(inputs["l0_w_in"], np.float32)
    qcols = np.concatenate([np.arange(h * 64, (h + 1) * 64) for h in _QPERM])
    w_in_p = np.concatenate([w_in[:, qcols], w_in[:, 512:]], axis=1)
    w_out = np.asarray(inputs["l0_w_out"], np.float32)
    w_out_p = np.concatenate([w_out[qcols, :], w_out[512:, :]], axis=0)
    shared["l0_w_in"] = np.ascontiguousarray(w_in_p)
    shared["l0_w_out"] = np.ascontiguousarray(w_out_p)
    shared["l0_sinks"] = np.ascontiguousarray(np.asarray(inputs["l0_sinks"], np.float32)[_QPERM])
    shared["l0_sgu_ln_g"] = np.asarray(inputs["l0_sgu_ln_g"], np.float32)
    shared["l0_sgu_ln_b"] = np.asarray(inputs["l0_sgu_ln_b"], np.float32)
    shared["l0_sgu_wT"] = np.ascontiguousarray(np.transpose(np.asarray(inputs["l0_sgu_w"], np.float32), (2, 0, 1)))
    shared["l0_sgu_bT"] = np.ascontiguousarray(np.asarray(inputs["l0_sgu_b"], np.float32).T)
    shared["l1_w_in"] = np.asarray(inputs["l1_w_in"], np.float32)
    shared["l1_pool_w"] = np.asarray(inputs["l1_pool_w"], np.float32)
    shared["l1_pool_scaleT"] = np.ascontiguousarray(np.asarray(inputs["l1_pool_scale"], np.float32).reshape(8, 128).T)
    shared["l1_w_out"] = np.asarray(inputs["l1_w_out"], np.float32)
    for l in range(2):
        p = f"l{l}_"
        for nm in ("ln1_g", "ln1_b", "ln2_g", "ln2_b", "ln3_g", "ln3_b", "xq", "xkv", "xo", "e_gate", "e_up", "e_down"):
            shared[p + nm] = np.asarray(inputs[p + nm], np.float32)
    shared["router_w"] = np.asarray(inputs["router_w"], np.float32)
    shared["router_bias"] = np.asarray(inputs["router_bias"], np.float32)
    shared["c_ident"] = C["ident"]
    shared["c_tril"] = C["tril"]
    shared["c_invf"] = C["invf"]
    shared["c_lstrict"] = C["lstrict"]
    shared["c_tabs"] = C["tabs"]
    in_maps = []
    for c in range(NCORES):
        b, h = c // 2, c % 2
        m = dict(shared)
        xin = np.zeros((NT * 128, D), np.float32)
        pp = np.zeros((NT * 128,), np.int32)
        if h == 0:
            xin[256:] = x[b, 0:TOK]
            pp[256:] = pos[b, 0:TOK]
        else:
            xin[:] = x[b, TOK - 256:2 * TOK]
            pp[:] = pos[b, TOK - 256:2 * TOK]
        m["xin"] = xin
        m["posT"] = np.ascontiguousarray(pp.reshape(NT, 128).T)
        m["mem"] = np.ascontiguousarray(memv[b])
        first_prev = np.zeros_like(C["m_prev"]) if h == 0 else C["m_prev"]
        m["c_mask"] = np.ascontiguousarray(np.stack([C["m_prev"], C["m_cur"], first_prev], axis=1)).astype(bf)
        pB = C["poolB"].copy()
        if h == 1:
            pB[:, 2] = pB[:, 0]
            pB[:, 3] = pB[:, 1]
        m["c_poolB"] = pB.astype(bf)
        in_maps.append(m)
    return in_maps


_NC_CACHE = {}


def kernel(**inputs):
    stage = "B1"
    if stage not in _NC_CACHE:
        _NC_CACHE[stage] = build_program(stage)
    nc = _NC_CACHE[stage]
    in_maps = make_in_maps(inputs)
    res = run_bass_kernel_spmd(nc, in_maps, core_ids=list(range(NCORES)))
    outs = [np.asarray(r["out"], np.float32) for r in res.results]
    full = np.zeros((4, 8192, D), np.float32)
    for c in range(NCORES):
        full[c // 2, (c % 2) * TOK:(c % 2 + 1) * TOK] = outs[c]
    return full
```

```python
import math
import os
from contextlib import ExitStack

import numpy as np
import ml_dtypes

import concourse.bass as bass
import concourse.mybir as mybir
from concourse.bass_utils import run_bass_kernel_spmd

F32 = mybir.dt.float32
BF16 = mybir.dt.bfloat16
I32 = mybir.dt.int32
AF = mybir.ActivationFunctionType
ALU = mybir.AluOpType
AX = mybir.AxisListType

NCORES = 8
D = 1024
NT = 34
TOK = 4096
ALPHA = (2.0 * 2) ** 0.25
EPS = 1e-5
NE = 16
S_MAX = 33
TWO_PI = 2.0 * math.pi


class Sem:
    def __init__(self, h, name):
        self.h = h
        self.name = name
        self.cnt = 0


class Tk:
    __slots__ = ("name", "w", "r", "sem", "psum")

    def __init__(self, name, sem=None):
        self.name = name
        self.w = None
        self.r = {}
        self.sem = sem
        self.psum = name.startswith(("ptr", "pa", "pb", "pc", "pd", "pg", "pu", "py", "plg"))


class Prog:
    def __init__(self, nc, es):
        self.nc = nc
        self.eng = {"pe": nc.tensor, "act": nc.scalar, "dve": nc.vector, "pool": nc.gpsimd, "sp": nc.sync}
        self.esem = {k: Sem(es.enter_context(nc.semaphore("e_" + k)), "e_" + k) for k in self.eng}
        self.seen = {k: {} for k in self.eng}
        self.dsems = []
        self.es = es
        self.cast_out = []
        self.rec = None
        self.gsems = {}
        self.all_dma_tokens = {}

    def gsem(self, name):
        if name not in self.gsems:
            self.gsems[name] = self.dsem(name)
        return self.gsems[name]

    def dsem(self, name):
        s = Sem(self.es.enter_context(self.nc.semaphore("d_" + name)), "d_" + name)
        self.dsems.append(s)
        return s

    def _wait(self, e, toks):
        need = {}
        for tok in toks:
            if tok is None:
                continue
            s, v = tok
            if e == "pe" and s is self.esem["pe"] and not KPESYNC:
                continue
            if need.get(s.name, (None, -1))[1] < v:
                need[s.name] = (s, v)
        seen = self.seen[e]
        for name, (s, v) in need.items():
            if seen.get(name, -1) >= v:
                continue
            self.eng[e].wait_ge(s.h, v)
            seen[name] = v

    def _deps(self, reads, writes, e=None):
        toks = []
        for t in reads:
            toks.append(t.w)
            if t.psum:
                toks.extend(tok for tok in t.r.values() if e is None or tok[0] is not self.esem[e])
        for t in writes:
            toks.append(t.w)
            toks.extend(t.r.values())
        return toks

    def _commit(self, tok, reads, writes):
        for t in writes:
            t.w = tok
            t.r = {}
        for t in reads:
            s, v = tok
            if t.r.get(s.name, (None, -1))[1] < v:
                t.r[s.name] = tok

    def play(self, item):
        kind = item[0]
        if kind == "op":
            self.op(*item[1:])
        else:
            self.dma(*item[1:])

    def op(self, e, fn, reads=(), writes=()):
        if self.rec is not None:
            self.rec.append(("op", e, fn, list(reads), list(writes)))
            return
        self._wait(e, self._deps(reads, writes, e))
        ins = fn(self.eng[e])
        s = self.esem[e]
        s.cnt += 1
        ins.then_inc(s.h, 1)
        self._commit((s, s.cnt), reads, writes)

    def dma(self, e, fn, sem, reads=(), writes=()):
        if self.rec is not None:
            self.rec.append(("dma", e, fn, sem, list(reads), list(writes)))
            return None
        self._wait(e, self._deps(reads, writes))
        if e == "pool":
            if len(self.cast_out) >= 2:
                self._wait(e, [self.cast_out[-2]])
        ins = fn(self.eng[e])
        if not isinstance(ins, (list, tuple)):
            ins = [ins]
        for i in ins:
            i.then_inc(sem.h, 16)
            sem.cnt += 16
        tok = (sem, sem.cnt)
        if e == "pool":
            self.cast_out.append(tok)
        self.all_dma_tokens[sem.name] = tok
        self._commit(tok, reads, writes)
        return tok

    def barrier(self):
        toks = [(s, s.cnt) for s in self.esem.values()] + list(self.all_dma_tokens.values())
        for e in self.eng:
            self._wait(e, toks)


def bview(ap, shape):
    return ap.to_broadcast(shape)


class StopBuild(Exception):
    pass


import os
KSTOP = int(os.environ.get("KSTOP", "0"))
KTILES = int(os.environ.get("KTILES", "0"))
KSEQ = int(os.environ.get("KSEQ", "0"))
SPARSE = int(os.environ.get("SPARSE", "1"))
KSPLIT = int(os.environ.get("KSPLIT", "0"))
KPESYNC = int(os.environ.get("KPESYNC", "0"))


STOPPED = [False]


def chk(n):
    if KSTOP == n:
        STOPPED[0] = True
        return True
    return False


def build_program(stage="B1"):
    nc = bass.Bass("TRN2", target_bir_lowering=False)
    es = ExitStack()
    with es:
        P = Prog(nc, es)
        try:
            _build(nc, es, stage, P)
        except StopBuild:
            pass
        P.barrier()
    return nc


def _build(nc, es, stage, P):
    def din(name, shape, dt=F32):
        return nc.dram_tensor(name, list(shape), dt, kind="ExternalInput").ap()

    xin = din("xin", [NT * 128, D])
    posT = din("posT", [128, NT], I32)
    mem = din("mem", [256, D])
    router_w = din("router_w", [D, NE])
    router_bias = din("router_bias", [NE])
    W = {}
    W["l0_w_in"] = din("l0_w_in", [D, 1792])
    W["l0_sinks"] = din("l0_sinks", [8])
    W["l0_sgu_ln_g"] = din("l0_sgu_ln_g", [512])
    W["l0_sgu_ln_b"] = din("l0_sgu_ln_b", [512])
    W["l0_sgu_wT"] = din("l0_sgu_wT", [128, 8, 128])
    W["l0_sgu_bT"] = din("l0_sgu_bT", [128, 8])
    W["l0_w_out"] = din("l0_w_out", [D, D])
    W["l1_w_in"] = din("l1_w_in", [D, D])
    W["l1_pool_w"] = din("l1_pool_w", [4, 256, 256])
    W["l1_pool_scaleT"] = din("l1_pool_scaleT", [128, 8])
    W["l1_w_out"] = din("l1_w_out", [D, D])
    for l in range(2):
        p = f"l{l}_"
        for nm in ("ln1_g", "ln1_b", "ln2_g", "ln2_b", "ln3_g", "ln3_b"):
            W[p + nm] = din(p + nm, [D])
        W[p + "xq"] = din(p + "xq", [D, D])
        W[p + "xkv"] = din(p + "xkv", [D, 2 * D])
        W[p + "xo"] = din(p + "xo", [D, D])
        W[p + "e_gate"] = din(p + "e_gate", [NE, D, 512])
        W[p + "e_up"] = din(p + "e_up", [NE, D, 512])
        W[p + "e_down"] = din(p + "e_down", [NE, 512, D])
    c_ident = din("c_ident", [128, 128], BF16)
    c_mask = din("c_mask", [128, 3, 128], BF16)
    c_tril = din("c_tril", [128, 128])
    c_poolB = din("c_poolB", [128, 4, 4, 128], BF16)
    c_invf = din("c_invf", [128, 8])
    c_lstrict = din("c_lstrict", [128, 128], BF16)
    c_tabs = din("c_tabs", [128, 17 + S_MAX + 8])

    out = nc.dram_tensor("out", [TOK, D], F32, kind="ExternalOutput").ap()
    xs_a = nc.dram_tensor("xs_a", [(NT - 1) * 128, D], F32, kind="Internal").ap()
    xs_b = nc.dram_tensor("xs_b", [(NT - 1) * 128, D], F32, kind="Internal").ap()
    xsort = nc.dram_tensor("xsort", [S_MAX * 512, D], BF16, kind="Internal").ap()
    ysort = nc.dram_tensor("ysort", [32 * 512, D], F32, kind="Internal").ap()

    uid = [0]

    def sb(name, shape, dt=F32, stack=es):
        uid[0] += 1
        return stack.enter_context(nc.sbuf_tensor(f"{name}_{uid[0]}", list(shape), dt))

    def ps(name, shape, dt=F32, stack=es):
        uid[0] += 1
        return stack.enter_context(nc.psum_tensor(f"{name}_{uid[0]}", list(shape), dt))

    csem = P.dsem("const")
    ident = sb("ident", [128, 128], BF16)
    mask = sb("mask", [128, 3, 128], BF16)
    invf = sb("invf", [128, 8])
    posi = sb("posi", [128, NT], I32)
    rbias = sb("rbias", [128, NE])
    ones_bf = sb("ones_bf", [128, 128], BF16)
    comb = sb("comb", [128, NT, NE])
    cosT = sb("cosT", [128, NT, 8])
    sinT = sb("sinT", [128, NT, 8])
    rw_bf = sb("rw_bf", [128, 8, NE], BF16)
    memT = sb("memT", [128, 8, 256], BF16)
    T_const = Tk("const")
    T_comb = [Tk(f"comb{j}") for j in range(NT)]
    lg_all = sb("lg_all", [128, NT, NE])
    T_lg = [Tk(f"lg{j}") for j in range(NT)]
    T_cs = Tk("cossin")
    T_memT = Tk("memT")

    def cload(dst, src):
        P.dma("sp", lambda e: e.dma_start(out=dst, in_=src), csem, writes=[T_const])

    cload(ident[:], c_ident)
    cload(mask[:], c_mask)
    cload(invf[:], c_invf)
    cload(posi[:], posT)
    cload(rbias[:], router_bias.partition_broadcast(128))
    lstrict = sb("lstrict", [128, 128], BF16)
    tabs = sb("tabs", [128, 17 + S_MAX + 8])
    cload(lstrict[:], c_lstrict)
    cload(tabs[:], c_tabs)
    T_ones = Tk("ones2")
    P.op("pool", lambda e: e.memset(ones_bf[:], 1.0), writes=[T_ones])

    T_xsort_g = Tk("xsort_g")
    zt = sb("zt", [128, 1024], BF16)
    T_zt = Tk("zt")
    P.op("pool", lambda e: e.memset(zt[:], 0.0), writes=[T_zt])

    with ExitStack() as st:
        posf = sb("posf", [128, NT], F32, st)
        ang = sb("ang", [128, NT, 8], F32, st)
        ki = sb("ki", [128, NT, 8], I32, st)
        kf = sb("kf", [128, NT, 8], F32, st)
        rr = sb("rr", [128, NT, 8], F32, st)
        mm = sb("mm", [128, NT, 8], F32, st)
        T_t = Tk("ropetmp")
        P.op("dve", lambda e: e.tensor_copy(out=posf[:], in_=posi[:]), reads=[T_const], writes=[T_t])
        P.op("dve", lambda e: e.tensor_tensor(out=ang[:], in0=posf[:].unsqueeze(2).to_broadcast([128, NT, 8]),
                                              in1=invf[:].unsqueeze(1).to_broadcast([128, NT, 8]), op=ALU.mult),
             reads=[T_const, T_t], writes=[T_t])
        for which, dst in ((0, sinT), (1, cosT)):
            if which == 1:
                P.op("dve", lambda e: e.tensor_scalar_add(out=ang[:], in0=ang[:], scalar1=math.pi / 2),
                     reads=[T_t], writes=[T_t])
            P.op("dve", lambda e: e.tensor_scalar_mul(out=ki[:], in0=ang[:], scalar1=1.0 / TWO_PI),
                 reads=[T_t], writes=[T_t])
            P.op("dve", lambda e: e.tensor_copy(out=kf[:], in_=ki[:]), reads=[T_t], writes=[T_t])
            P.op("dve", lambda e: e.scalar_tensor_tensor(out=rr[:], in0=kf[:], scalar=-TWO_PI, in1=ang[:],
                                                          op0=ALU.mult, op1=ALU.add), reads=[T_t], writes=[T_t])
            P.op("dve", lambda e: e.tensor_scalar(out=mm[:], in0=rr[:], scalar1=math.pi, scalar2=None, op0=ALU.is_gt),
                 reads=[T_t], writes=[T_t])
            P.op("dve", lambda e: e.scalar_tensor_tensor(out=rr[:], in0=mm[:], scalar=-TWO_PI, in1=rr[:],
                                                          op0=ALU.mult, op1=ALU.add), reads=[T_t], writes=[T_t])
            P.op("dve", lambda e: e.tensor_scalar(out=mm[:], in0=rr[:], scalar1=-math.pi, scalar2=None, op0=ALU.is_lt),
                 reads=[T_t], writes=[T_t])
            P.op("dve", lambda e: e.scalar_tensor_tensor(out=rr[:], in0=mm[:], scalar=TWO_PI, in1=rr[:],
                                                          op0=ALU.mult, op1=ALU.add), reads=[T_t], writes=[T_t])
            P.op("dve", lambda e: e.tensor_scalar(out=rr[:], in0=rr[:], scalar1=3.1415925, scalar2=-3.1415925,
                                                   op0=ALU.min, op1=ALU.max), reads=[T_t], writes=[T_t])
            P.op("act", lambda e, dst=dst: e.activation(out=dst[:], in_=rr[:], func=AF.Sin),
                 reads=[T_t], writes=[T_cs])
        P.barrier()

    with ExitStack() as st:
        wsem = P.dsem("setupw")
        T_rw = Tk("rw")
        rwf = sb("rwf", [128, 8, NE], F32, st)
        P.dma("sp", lambda e: e.dma_start(out=rwf[:], in_=router_w.rearrange("(k p) f -> p k f", p=128)), wsem, writes=[T_rw])
        P.op("dve", lambda e: e.tensor_copy(out=rw_bf[:], in_=rwf[:]), reads=[T_rw], writes=[T_rw])
        memf = sb("memf", [128, 2, D], F32, st)
        memb = sb("memb", [128, 2, D], BF16, st)
        T_mem = Tk("mem")
        msem = P.dsem("mem")
        P.dma("sp", lambda e: e.dma_start(out=memf[:], in_=mem.rearrange("(k p) f -> p k f", p=128)), msem,
              writes=[T_mem])
        P.op("dve", lambda e: e.tensor_copy(out=memb[:], in_=memf[:]), reads=[T_mem], writes=[T_mem])
        ptr = ps("ptr_s", [128, 8, 128], BF16, st)
        T_ptr = Tk("ptr_s")
        for mc in range(2):
            def f(e, mc=mc):
                for dc in range(8):
                    i = e.transpose(out=ptr[:, dc, :], in_=memb[:, mc, dc * 128:(dc + 1) * 128], identity=ident[:])
                return i
            P.op("pe", f, reads=[T_mem, T_const], writes=[T_ptr])
            P.op("dve", lambda e, mc=mc: e.tensor_copy(out=memT[:, :, mc * 128:(mc + 1) * 128], in_=ptr[:]),
                 reads=[T_ptr], writes=[T_memT])
        P.barrier()

    def layer_norm(r, T_r, gb, T_gb, xo_f, xo_b, T_xo, scr, T_scr, tag, norm_on_act=False):
        st6, mv, sd, rstd, nmr = scr
        P.op("dve", lambda e: e.bn_stats(out=st6[:, 0, :], in_=r[:, 0:512]), reads=[T_r], writes=[T_scr])
        P.op("dve", lambda e: e.bn_stats(out=st6[:, 1, :], in_=r[:, 512:1024]), reads=[T_r], writes=[T_scr])
        P.op("dve", lambda e: e.bn_aggr(out=mv[:], in_=st6[:].rearrange("p a b -> p (a b)")),
             reads=[T_scr], writes=[T_scr])
        P.op("act", lambda e: e.activation(out=sd[:], in_=mv[:, 1:2], func=AF.Ln, bias=epsb[:], scale=1.0),
             reads=[T_scr, T_eps], writes=[T_scr])
        P.op("act", lambda e: e.activation(out=rstd[:], in_=sd[:], func=AF.Exp, scale=-0.5), reads=[T_scr], writes=[T_scr])
        if norm_on_act:
            P.op("dve", lambda e: e.scalar_tensor_tensor(out=nmr[:], in0=mv[:, 0:1], scalar=-1.0, in1=rstd[:],
                                                          op0=ALU.mult, op1=ALU.mult), reads=[T_scr], writes=[T_scr])
            P.op("act", lambda e: e.activation(out=r[:], in_=r[:], func=AF.Identity, bias=nmr[:], scale=rstd[:]),
                 reads=[T_scr], writes=[T_r])
        else:
            P.op("dve", lambda e: e.tensor_scalar(out=r[:], in0=r[:], scalar1=mv[:, 0:1], scalar2=rstd[:], op0=ALU.subtract, op1=ALU.mult),
                 reads=[T_scr], writes=[T_r])
        P.op("dve", lambda e: e.tensor_tensor(out=r[:], in0=r[:], in1=gb[:, 0, :], op=ALU.mult),
             reads=[T_gb], writes=[T_r])
        P.op("dve", lambda e: e.tensor_tensor(out=xo_f[:], in0=r[:], in1=gb[:, 1, :], op=ALU.add),
             reads=[T_r, T_gb], writes=[T_xo])
        if xo_b is not None:
            P.op("act", lambda e: e.copy(out=xo_b[:], in_=xo_f[:]), reads=[T_xo], writes=[T_xo])

    epsb = sb("epsb", [128, 1])
    T_eps = Tk("eps")
    P.op("dve", lambda e: e.memset(epsb[:], EPS), writes=[T_eps])

    NSTG = 2
    stg = [sb(f"stg{i}", [128, 512], F32) for i in range(NSTG)]
    T_stg = [Tk(f"stg{i}", P.dsem(f"stg{i}")) for i in range(NSTG)]
    stg_i = [0]

    def stream_weight(dst, src, K, C, T_dst, pool=None):
        stg_, T_stg_, cap = (stg, T_stg, 512) if pool is None else pool
        nst = len(stg_)
        if C >= cap:
            nk = 1
            ncol = C // ((C + cap - 1) // cap)
        else:
            nk, ncol = min(K, cap // C), C
        for k0 in range(0, K, nk):
            k1 = min(K, k0 + nk)
            for c0 in range(0, C, ncol):
                c1 = min(C, c0 + ncol)
                si = stg_i[0] % nst
                stg_i[0] += 1
                n = (k1 - k0) * (c1 - c0)
                sv = stg_[si][:, 0:n].rearrange("p (k f) -> p k f", f=c1 - c0)
                P.dma("sp", lambda e: e.dma_start(out=sv, in_=src[k0 * 128:k1 * 128, c0:c1].rearrange("(k p) f -> p k f", p=128)),
                      T_stg_[si].sem, writes=[T_stg_[si]])
                ce = ("pool", "dve", "act")[stg_i[0] % 3]
                if ce == "act":
                    P.op("act", lambda e: e.copy(out=dst[:, k0:k1, c0:c1], in_=sv), reads=[T_stg_[si]], writes=[T_dst])
                else:
                    P.op(ce, lambda e: e.tensor_copy(out=dst[:, k0:k1, c0:c1], in_=sv), reads=[T_stg_[si]], writes=[T_dst])

    def load_gb(stack, names, tag, sem):
        g = sb("gb_" + tag, [128, 2, D], F32, stack)
        T = Tk("gb_" + tag)
        P.dma("sp", lambda e: [e.dma_start(out=g[:, 0, :], in_=W[names[0]].partition_broadcast(128)),
                               e.dma_start(out=g[:, 1, :], in_=W[names[1]].partition_broadcast(128))],
              sem, writes=[T])
        return g, T

    def load_w(stack, name, src, kchunks, cols, sem):
        w = sb(name, [128, kchunks, cols], BF16, stack)
        T = Tk(name)
        pending_w.append((w, src, kchunks, cols, T))
        return w, T

    pending_w = []
    big_sems = [P.dsem(f"bigstg{i}") for i in range(5)]

    def transpose8(src_bf, T_src, dstT, T_dst, ptr, T_ptr, n=8, evac="act"):
        def f(e):
            for dc in range(n):
                i = e.transpose(out=ptr[:, dc, :], in_=src_bf[:, dc * 128:(dc + 1) * 128], identity=ident[:])
            return i
        P.op("pe", f, reads=[T_src, T_const], writes=[T_ptr])
        if evac == "act":
            P.op("act", lambda e: e.copy(out=dstT[:, 0:n, :], in_=ptr[:, 0:n, :]), reads=[T_ptr], writes=[T_dst])
        else:
            P.op("dve", lambda e: e.tensor_copy(out=dstT[:, 0:n, :], in_=ptr[:, 0:n, :]), reads=[T_ptr], writes=[T_dst])

    def phase_A(layer, src_dram, src_row0, dst_dram):
        pfx = f"l{layer}_"
        with ExitStack() as st:
            wsem = [P.dsem(f"A{layer}w{i}") for i in range(3)]
            gsem = P.dsem(f"A{layer}g")
            if layer == 0:
                win, T_win = load_w(st, "win", W["l0_w_in"], 8, 1792, wsem[1])
            else:
                win, T_win = load_w(st, "win", W["l1_w_in"], 8, D, wsem[1])
            wout, T_wout = load_w(st, "wout", W[pfx + "w_out"], 8, D, wsem[2])
            xq_w, T_xq = load_w(st, "xq_w", W[pfx + "xq"], 8, D, wsem[0])
            xo_w, T_xo = load_w(st, "xo_w", W[pfx + "xo"], 8, D, wsem[1])
            if layer == 0:
                zsem = P.dsem("zfill")
                P.dma("sp", lambda e: [e.dma_start(out=xsort[c_ * 128:(c_ + 1) * 128, :], in_=zt[:]) for c_ in range(S_MAX * 4)],
                      zsem, reads=[T_zt], writes=[T_xsort_g])
            gb1, T_gb1 = load_gb(st, (pfx + "ln1_g", pfx + "ln1_b"), "1", gsem)
            gb2, T_gb2 = load_gb(st, (pfx + "ln2_g", pfx + "ln2_b"), "2", gsem)

            pyb = ps("py_s1b", [128, 512], F32, st)
            T_pyb = Tk("py_s1b")
            pa = ps("pa", [128, 2, 512], F32, st)
            pb = ps("pb", [128, 2, 512], F32, st)
            pc = ps("pc", [128, 2, 512], F32, st)
            pd = ps("pd", [128, 512], F32, st)
            T_pa0, T_pa1, T_pb0, T_pb1, T_pc0, T_pc1, T_pd = [Tk(n) for n in ("pa0", "pa1", "pb0", "pb1", "pc0", "pc1", "pd")]
            def bfv(ap):
                return ap.bitcast(BF16).rearrange("p (a b) -> p a b", b=128)
            pa0v, pa1v, pb0v = bfv(pa[:, 0, :]), bfv(pa[:, 1, :]), bfv(pb[:, 0, :])

            KT = sb("KT", [128, 8, 256], BF16, st)
            V = sb("V", [128, 2, D], BF16, st)
            T_KT, T_V = Tk("KT"), Tk("V")
            if layer == 0:
                wsT = sb("wsT", [128, 8, 128], BF16, st); T_wsT = Tk("wsT")
                bsT = sb("bsT", [128, 8], F32, st)
                sgb = sb("sgb", [128, 2, 512], F32, st)
                sinkb = sb("sinkb", [128, 8], F32, st)
                esink = sb("esink", [128, 8], F32, st); T_esink = Tk("esink")
                T_c0 = Tk("c0")
                c0sem = P.dsem("A0c")
            st2 = ExitStack()
            xkv_w, T_xkv = load_w(st2, "xkv_w", W[pfx + "xkv"], 8, 2 * D, wsem[0])
            bigs = [sb(f"bigstg{i}", [128, 2048], F32, st2) for i in range(5)]
            T_bigs = [Tk(f"bigstg{i}", big_sems[i]) for i in range(5)]
            for (w_, src_, k_, c_, t_) in pending_w[-1:] + pending_w[:-1]:
                stream_weight(w_, src_, k_, c_, t_, pool=(bigs, T_bigs, 2048))
            del pending_w[:]
            if layer == 0:
                wsT_f = sb("wsT_f", [128, 8, 128], F32, st2)
                tril = sb("tril", [128, 128], F32, st2)
                P.dma("sp", lambda e: [e.dma_start(out=wsT_f[:], in_=W["l0_sgu_wT"]),
                                       e.dma_start(out=tril[:], in_=c_tril),
                                       e.dma_start(out=bsT[:], in_=W["l0_sgu_bT"]),
                                       e.dma_start(out=sgb[:, 0, :], in_=W["l0_sgu_ln_g"].partition_broadcast(128)),
                                       e.dma_start(out=sgb[:, 1, :], in_=W["l0_sgu_ln_b"].partition_broadcast(128)),
                                       e.dma_start(out=sinkb[:], in_=W["l0_sinks"].partition_broadcast(128))],
                      c0sem, writes=[T_c0])
                P.op("dve", lambda e: e.tensor_tensor(out=wsT[:], in0=wsT_f[:],
                                                      in1=tril[:].unsqueeze(1).to_broadcast([128, 8, 128]), op=ALU.mult),
                     reads=[T_c0], writes=[T_wsT])
                P.op("act", lambda e: e.activation(out=esink[:], in_=sinkb[:], func=AF.Exp), reads=[T_c0], writes=[T_esink])
            for oc in range(8):
                bank, T_bank = (pa[:, oc % 2, 0:256], (T_pa0, T_pa1)[oc % 2])
                def f(e, oc=oc, bank=bank):
                    for ic in range(8):
                        i = e.matmul(bank, lhsT=xkv_w[:, ic, oc * 128:(oc + 1) * 128], rhs=memT[:, ic, :],
                                     start=(ic == 0), stop=(ic == 7))
                    return i
                P.op("pe", f, reads=[T_xkv, T_memT], writes=[T_bank])
                P.op("act", lambda e, oc=oc, bank=bank: e.copy(out=KT[:, oc, :], in_=bank), reads=[T_bank], writes=[T_KT])
            for mc in range(2):
                for hf in range(2):
                    bank, T_bank = (pb[:, hf, :], (T_pb0, T_pb1)[hf])
                    def f(e, mc=mc, hf=hf, bank=bank):
                        for ic in range(8):
                            i = e.matmul(bank, lhsT=memT[:, ic, mc * 128:(mc + 1) * 128],
                                         rhs=xkv_w[:, ic, D + hf * 512:D + (hf + 1) * 512],
                                         start=(ic == 0), stop=(ic == 7))
                        return i
                    P.op("pe", f, reads=[T_xkv, T_memT], writes=[T_bank])
                    P.op("dve", lambda e, mc=mc, hf=hf, bank=bank: e.tensor_copy(out=V[:, mc, hf * 512:(hf + 1) * 512], in_=bank),
                         reads=[T_bank], writes=[T_V])

            P.barrier()
            st2.close()
            if stage == "S1":
                osem = P.dsem("dbg")
                P.dma("pool", lambda e: [e.dma_start(out=out[0:128, :], in_=KT[:, 0:4, :].rearrange("p a b -> p (a b)")),
                                       e.dma_start(out=out[128:256, :], in_=V[:, 0, :])],
                      osem, reads=[T_KT, T_V], writes=[Tk("o")])
                P.barrier()
                return
            NXS = 3
            xt = [sb(f"xt{i}", [128, D], F32, st) for i in range(NXS)]
            T_xt = [Tk(f"xt{i}", P.dsem(f"A{layer}xt{i}")) for i in range(NXS)]
            xb = sb("xb", [128, D], BF16, st); T_xb = Tk("xb")
            xT = sb("xT", [128, 8, 128], BF16, st); T_xT = Tk("xT")
            mixb = sb("mixb", [128, D], BF16, st); T_mixb = Tk("mixb")
            mixT2 = [sb(f"mixT{i}", [128, 8, 128], BF16, st) for i in range(2)]
            T_mixT2 = [Tk("mixT0"), Tk("mixT1")]
            r = sb("r", [128, D], F32, st); T_r = Tk("r", P.dsem(f"A{layer}r"))
            x1 = [sb(f"x1_{i}", [128, D], F32, st) for i in range(2)]
            x1b = [sb(f"x1b_{i}", [128, D], BF16, st) for i in range(2)]
            T_x1 = [Tk("x1_0"), Tk("x1_1")]
            r2 = sb("r2", [128, D], F32, st); T_r2 = Tk("r2")
            x1T = sb("x1T", [128, 8, 128], BF16, st); T_x1T = Tk("x1T")
            qxT = sb("qxT", [128, 8, 128], BF16, st); T_qxT = Tk("qxT")
            pxT = sb("pxT", [128, 2, 512], BF16, st); T_pxT = [Tk("pxT0"), Tk("pxT1")]
            rdn = sb("rdn", [128, 512], F32, st); T_rdn = Tk("rdn")
            oTn = sb("oTn", [128, 8, 128], BF16, st); T_oTn = Tk("oTn")
            x2 = [sb(f"x2_{i}", [128, D], F32, st) for i in range(2)]
            x2b = sb("x2b", [128, D], BF16, st); T_x2b = Tk("x2b")
            T_x2 = [Tk(f"x2_{i}", P.dsem(f"A{layer}x2_{i}")) for i in range(2)]
            lnscr = (sb("st6", [128, 2, 6], F32, st), sb("mv", [128, 2], F32, st), sb("sd", [128, 1], F32, st),
                     sb("rstd", [128, 1], F32, st), sb("nmr", [128, 1], F32, st))
            T_lnscr = Tk("lnscr")
            lnscr2 = (sb("st6_2", [128, 2, 6], F32, st), sb("mv_2", [128, 2], F32, st), sb("sd_2", [128, 1], F32, st),
                      sb("rstd_2", [128, 1], F32, st), sb("nmr_2", [128, 1], F32, st))
            T_lnscr2 = Tk("lnscr2")

            if layer == 0:
                kT = [sb(f"kT{i}", [128, 128], BF16, st) for i in range(2)]
                vaug = [sb(f"vaug{i}", [128, 2, 65], BF16, st) for i in range(2)]
                T_kT = [Tk("kT0"), Tk("kT1")]
                T_va = [Tk("va0"), Tk("va1")]
                for i in range(2):
                    P.op("pool", lambda e, i=i: e.memset(vaug[i][:], 1.0), writes=[T_va[i]])
                qkb = sb("qkb", [128, 640], BF16, st); T_qkb = Tk("qkb")
                qT = sb("qT", [128, 4, 128], BF16, st); T_qT = Tk("qT")
                rp = [sb(f"rp{i}", [128, 10, 8], F32, st) for i in range(4)]; T_rp = Tk("rp")
                pT = sb("pT", [128, 4, 512], BF16, st)
                T_pT = [Tk(f"pT{i}") for i in range(4)]
                den = sb("den", [128, 8], F32, st); T_den = Tk("den")
                u = sb("u", [128, 512], F32, st); T_u = Tk("u")
                gv = sb("gv", [128, 512], F32, st); T_gv = Tk("gv")
                gt = sb("gt", [128, 2, 512], F32, st); T_gt = Tk("gt")
                vn = sb("vn", [128, 512], BF16, st); T_vn = Tk("vn")
                g8 = {n: sb("g8_" + n, [128, 8], F32, st) for n in ("mean", "ss", "sd", "rstd")}
                T_g8 = Tk("g8")
            else:
                poolB = sb("poolB", [128, 4, 4, 128], BF16, st)
                T_poolB = Tk("poolB")
                P.dma("sp", lambda e: e.dma_start(out=poolB[:], in_=c_poolB), P.dsem("A1poolB"), writes=[T_poolB])
                hb = [sb(f"hb{i}", [128, D], BF16, st) for i in range(2)]
                T_hb = [Tk("hb0"), Tk("hb1")]
                pw, T_pw = None, None
                pw = sb("pw", [128, 4, 2, 256], BF16, st); T_pw = Tk("pw")
                for g in range(4):
                    stream_weight(pw[:, g, :, :], W["l1_pool_w"][g], 2, 256, T_pw)
                scT = sb("scT", [128, 8], F32, st); T_scT = Tk("scT")
                P.dma("sp", lambda e: e.dma_start(out=scT[:], in_=W["l1_pool_scaleT"]), gsem, writes=[T_scT])
                poT = sb("poT", [128, 8, 128], BF16, st); T_poT = Tk("poT")

            j0 = 0 if layer == 0 else 1

            def load_x(j):
                s = j % NXS
                P.dma("sp", lambda e: e.dma_start(out=xt[s][:], in_=src_dram[src_row0 + j * 128: src_row0 + (j + 1) * 128, :]),
                      T_xt[s].sem, writes=[T_xt[s]])

            load_x(j0)
            load_x(j0 + 1)

            def rope(src_ps, H, dst_bf, T_src, T_dst, j):
                T_srcs = T_src if isinstance(T_src, list) else [T_src]
                sv = src_ps.rearrange("p (h d) -> p h d", d=64)
                dv = dst_bf.rearrange("p (h d) -> p h d", d=64)
                P.op("act", lambda e: e.copy(out=dst_bf, in_=src_ps), reads=T_srcs, writes=[T_dst])
                if chk(131):
                    return
                cs = cosT[:, j, :].unsqueeze(1).to_broadcast([128, H, 8])
                sn = sinT[:, j, :].unsqueeze(1).to_broadcast([128, H, 8])
                t1, t2 = sv[:, :, 0:8], sv[:, :, 8:16]
                a, b, c, d_ = [x[:, 0:H, :] for x in rp]
                P.op("dve", lambda e: e.tensor_tensor(out=a, in0=t1, in1=cs, op=ALU.mult), reads=T_srcs + [T_cs], writes=[T_rp])
                if chk(132):
                    return
                P.op("dve", lambda e: e.tensor_tensor(out=b, in0=t2, in1=sn, op=ALU.mult), reads=T_srcs + [T_cs], writes=[T_rp])
                P.op("dve", lambda e: e.tensor_tensor(out=c, in0=t2, in1=cs, op=ALU.mult), reads=T_srcs + [T_cs], writes=[T_rp])
                P.op("dve", lambda e: e.tensor_tensor(out=d_, in0=t1, in1=sn, op=ALU.mult), reads=T_srcs + [T_cs], writes=[T_rp])
                if chk(133):
                    return
                P.op("dve", lambda e: e.tensor_tensor(out=dv[:, :, 0:8], in0=a, in1=b, op=ALU.subtract), reads=[T_rp], writes=[T_dst])
                P.op("dve", lambda e: e.tensor_tensor(out=dv[:, :, 8:16], in0=c, in1=d_, op=ALU.add), reads=[T_rp], writes=[T_dst])

            def proj(bank, T_bank, c0, ncol, src, T_src, w=win, T_w=T_win):
                def f(e):
                    for dc in range(8):
                        i = e.matmul(bank, lhsT=src[:, dc, :], rhs=w[:, dc, c0:c0 + ncol], start=(dc == 0), stop=(dc == 7))
                    return i
                P.op("pe", f, reads=[T_src, T_w], writes=[T_bank])

            def S1(j):
                s = j % NXS
                if j + 2 < NT:
                    load_x(j + 2)
                xj, T_xj = xt[s], T_xt[s]
                x1s = j % 2
                L = {"head": P.rec, "attn": [], "sgu": [], "tail": []}
                def sec(name):
                    P.rec = L[name]
                def finish():
                    A_, B_ = L["attn"], L["sgu"]
                    m = []
                    ia = ib = 0
                    while ia < len(A_) or ib < len(B_):
                        if ib >= len(B_) or (ia < len(A_) and ia * max(len(B_), 1) <= ib * max(len(A_), 1)):
                            m.append(A_[ia]); ia += 1
                        else:
                            m.append(B_[ib]); ib += 1
                    P.rec = L["head"] + m + L["tail"]
                mixT, T_mixT = mixT2[j % 2], T_mixT2[j % 2]
                P.op("dve", lambda e: e.tensor_copy(out=xb[:], in_=xj[:]), reads=[T_xj], writes=[T_xb])
                transpose8(xb, T_xb, xT, T_xT, pb0v, T_pb0)
                cur, prv = j % 2, (j - 1) % 2
                if layer == 0:
                    proj(pa[:, 1, 0:256], T_pa1, 512, 256, xT, T_xT)
                    proj(pa[:, 0, :], T_pa0, 0, 512, xT, T_xT)
                    if j > 0:
                        proj(pb[:, 0, :], T_pb0, 768, 512, xT, T_xT)
                        proj(pb[:, 1, :], T_pb1, 1280, 512, xT, T_xT)
                    sec("attn")
                    rope(pa[:].rearrange("p a b -> p (a b)")[:, 0:640], 10, qkb[:], [T_pa0, T_pa1], T_qkb, j)
                    P.op("act", lambda e: e.copy(out=vaug[cur][:, :, 0:64], in_=pa[:, 1, 128:256].rearrange("p (h d) -> p h d", d=64)),
                         reads=[T_pa1], writes=[T_va[cur]])
                    def f(e):
                        for dc in range(5):
                            ii = e.transpose(out=pa0v[:, dc, :], in_=qkb[:, dc * 128:(dc + 1) * 128], identity=ident[:])
                        return ii
                    P.op("pe", f, reads=[T_qkb, T_const], writes=[T_pa0])
                    P.op("dve", lambda e: e.tensor_copy(out=kT[cur][:], in_=pa0v[:, 4, :]), reads=[T_pa0], writes=[T_kT[cur]])
                    if j == 0:
                        finish()
                        return
                    P.op("act", lambda e: e.copy(out=qT[:], in_=pa0v[:, 0:4, :]), reads=[T_pa0], writes=[T_qT])
                    sec("sgu")
                    P.op("act", lambda e: e.activation(out=gt[:], in_=pb[:], func=AF.Square), reads=[T_pb0, T_pb1], writes=[T_gt])
                    P.op("dve", lambda e: e.tensor_scalar(out=gt[:], in0=gt[:], scalar1=0.044715, scalar2=1.0, op0=ALU.mult, op1=ALU.add),
                         reads=[], writes=[T_gt])
                    P.op("dve", lambda e: e.tensor_tensor(out=gt[:], in0=gt[:], in1=pb[:], op=ALU.mult), reads=[T_pb0, T_pb1], writes=[T_gt])
                    P.op("act", lambda e: e.activation(out=gt[:], in_=gt[:], func=AF.Sigmoid, scale=1.5957691216), reads=[], writes=[T_gt])
                    P.op("dve", lambda e: e.tensor_tensor(out=u[:], in0=gt[:, 0, :], in1=pb[:, 0, :], op=ALU.mult), reads=[T_gt, T_pb0], writes=[T_u])
                    P.op("dve", lambda e: e.tensor_tensor(out=gv[:], in0=gt[:, 1, :], in1=pb[:, 1, :], op=ALU.mult), reads=[T_gt, T_pb1], writes=[T_gv])
                    gv3 = gv[:].rearrange("p (g c) -> p g c", c=64)
                    P.op("dve", lambda e: e.reduce_sum(out=g8["mean"][:], in_=gv3, axis=AX.X), reads=[T_gv], writes=[T_g8])
                    P.op("dve", lambda e: e.tensor_scalar_mul(out=g8["mean"][:], in0=g8["mean"][:], scalar1=1.0 / 64), reads=[], writes=[T_g8])
                    P.op("dve", lambda e: e.tensor_tensor(out=gv3, in0=gv3, in1=g8["mean"][:].unsqueeze(2).to_broadcast([128, 8, 64]), op=ALU.subtract),
                         reads=[T_g8], writes=[T_gv])
                    P.op("act", lambda e: e.activation(out=gt[:, 0, :], in_=gv[:], func=AF.Square), reads=[T_gv, T_u], writes=[T_gt])
                    P.op("dve", lambda e: e.reduce_sum(out=g8["ss"][:], in_=gt[:, 0, :].rearrange("p (g c) -> p g c", c=64), axis=AX.X),
                         reads=[T_gt], writes=[T_g8])
                    P.op("act", lambda e: e.activation(out=g8["sd"][:], in_=g8["ss"][:], func=AF.Ln, bias=epsb[:], scale=1.0 / 64),
                         reads=[T_g8, T_eps], writes=[T_g8])
                    P.op("act", lambda e: e.activation(out=g8["rstd"][:], in_=g8["sd"][:], func=AF.Exp, scale=-0.5), reads=[], writes=[T_g8])
                    P.op("dve", lambda e: e.tensor_tensor(out=gv3, in0=gv3, in1=g8["rstd"][:].unsqueeze(2).to_broadcast([128, 8, 64]), op=ALU.mult),
                         reads=[T_g8], writes=[T_gv])
                    P.op("pool", lambda e: e.tensor_tensor(out=gv[:], in0=gv[:], in1=sgb[:, 0, :], op=ALU.mult), reads=[T_c0], writes=[T_gv])
                    P.op("pool", lambda e: e.tensor_tensor(out=vn[:], in0=gv[:], in1=sgb[:, 1, :], op=ALU.add), reads=[T_gv, T_c0], writes=[T_vn])
                    sec("attn")
                    sc_banks = [(pa[:, 0, :], T_pa0), (pa[:, 1, :], T_pa1), (pa[:, 0, :], T_pa0), (pa[:, 1, :], T_pa1)]
                    for kbi, slot in ((0, prv), (1, cur)):
                        for hg in range(2):
                            bank, T_bank = sc_banks[kbi * 2 + hg]
                            def f(e, bank=bank, slot=slot, hg=hg):
                                for c in range(4):
                                    i = e.matmul(bank[:, c * 128:(c + 1) * 128], lhsT=kT[slot][hg * 64:(hg + 1) * 64, :],
                                                 rhs=qT[hg * 64:(hg + 1) * 64, c, :], start=True, stop=True)
                                return i
                            P.op("pe", f, reads=[T_kT[slot], T_qT], writes=[T_bank])
                            idx = kbi * 2 + hg
                            P.op("act", lambda e, bank=bank, idx=idx: e.activation(out=pT[:, idx, :], in_=bank, func=AF.Exp, scale=0.125),
                                 reads=[T_bank], writes=[T_pT[idx]])
                            mi = 1 if kbi == 1 else (2 if j == 2 else 0)
                            P.op("dve", lambda e, idx=idx, mi=mi: e.tensor_tensor(
                                out=pT[:, idx, :].rearrange("p (c q) -> p c q", q=128),
                                in0=pT[:, idx, :].rearrange("p (c q) -> p c q", q=128),
                                in1=mask[:, mi, :].unsqueeze(1).to_broadcast([128, 4, 128]), op=ALU.mult),
                                reads=[T_const], writes=[T_pT[idx]])
                    def f(e):
                        for c in range(4):
                            for hg in range(2):
                                o_ap = pa[:, c // 2, ((c % 2) * 2 + hg) * 65:((c % 2) * 2 + hg + 1) * 65]
                                for kbi, slot in ((0, prv), (1, cur)):
                                    i = e.matmul(o_ap, lhsT=pT[:, kbi * 2 + hg, c * 128:(c + 1) * 128], rhs=vaug[slot][:, hg, :],
                                                 start=(kbi == 0), stop=(kbi == 1))
                        return i
                    P.op("pe", f, reads=T_pT + T_va, writes=[T_pa0, T_pa1])
                    ov = pa[:, :, 0:260].rearrange("p b (h e) -> p b h e", e=65)
                    P.op("dve", lambda e: e.tensor_tensor(out=den[:].rearrange("p (b h) -> p b h", b=2).unsqueeze(3), in0=ov[:, :, :, 64:65],
                                                          in1=esink[:].rearrange("p (b h) -> p b h", b=2).unsqueeze(3), op=ALU.add),
                         reads=[T_pa0, T_pa1, T_esink], writes=[T_den])
                    P.op("dve", lambda e: e.reciprocal(out=den[:], in_=den[:]), reads=[], writes=[T_den])
                    P.op("dve", lambda e: e.tensor_tensor(out=mixb[:, 0:512].rearrange("p (b h d) -> p b h d", b=2, h=4), in0=ov[:, :, :, 0:64],
                                                          in1=den[:].rearrange("p (b h) -> p b h", b=2).unsqueeze(3).to_broadcast([128, 2, 4, 64]),
                                                          op=ALU.mult),
                         reads=[T_pa0, T_pa1, T_den], writes=[T_mixb])
                    sec("sgu")
                    def f(e):
                        for g in range(8):
                            i = e.matmul(pb[:, 0, g * 64:(g + 1) * 64], lhsT=wsT[:, g, :], rhs=vn[:, g * 64:(g + 1) * 64], start=True, stop=True)
                        return i
                    P.op("pe", f, reads=[T_wsT, T_vn], writes=[T_pb0])
                    P.op("dve", lambda e: e.tensor_tensor(out=gv3, in0=pb[:, 0, :].rearrange("p (g c) -> p g c", c=64),
                                                          in1=bsT[:].unsqueeze(2).to_broadcast([128, 8, 64]), op=ALU.add),
                         reads=[T_pb0, T_c0, T_vn], writes=[T_gv])
                    P.op("dve", lambda e: e.tensor_tensor(out=mixb[:, 512:1024], in0=gv[:], in1=u[:], op=ALU.mult),
                         reads=[T_gv, T_u], writes=[T_mixb])
                    sec("tail")
                    transpose8(mixb, T_mixb, mixT, T_mixT, pa0v, T_pa0)
                else:
                    proj(pa[:, 0, :], T_pa0, 0, 512, xT, T_xT)
                    proj(pa[:, 1, :], T_pa1, 512, 512, xT, T_xT)
                    P.op("act", lambda e: e.copy(out=hb[cur][:], in_=pa[:].rearrange("p a b -> p (a b)")), reads=[T_pa0, T_pa1], writes=[T_hb[cur]])
                    if j == 1:
                        finish()
                        return
                    fo = 2 if j == 2 else 0
                    def f(e):
                        for cc in range(8):
                            g = cc // 2
                            e.matmul(pb[:, cc // 4, (cc % 4) * 128:(cc % 4 + 1) * 128], lhsT=hb[prv][:, cc * 128:(cc + 1) * 128],
                                     rhs=poolB[:, fo + 0, g, :], start=True, stop=False)
                            i = e.matmul(pb[:, cc // 4, (cc % 4) * 128:(cc % 4 + 1) * 128], lhsT=hb[cur][:, cc * 128:(cc + 1) * 128],
                                         rhs=poolB[:, fo + 1, g, :], start=False, stop=True)
                        return i
                    P.op("pe", f, reads=[T_hb[0], T_hb[1], T_poolB], writes=[T_pb0, T_pb1])
                    P.op("act", lambda e: e.copy(out=poT[:].rearrange("p a b -> p (a b)"), in_=pb[:].rearrange("p a b -> p (a b)")),
                         reads=[T_pb0, T_pb1], writes=[T_poT])
                    def f(e):
                        for dc in range(8):
                            g, i2 = dc // 2, dc % 2
                            for ci in range(2):
                                i = e.matmul(pa[:, dc // 4, (dc % 4) * 128:(dc % 4 + 1) * 128], lhsT=pw[:, g, ci, i2 * 128:(i2 + 1) * 128],
                                             rhs=poT[:, 2 * g + ci, :], start=(ci == 0), stop=(ci == 1))
                        return i
                    P.op("pe", f, reads=[T_pw, T_poT], writes=[T_pa0, T_pa1])
                    P.op("dve", lambda e: e.tensor_tensor(out=mixT[:], in0=pa[:].rearrange("p a (b t) -> p (a b) t", t=128),
                                                          in1=scT[:].unsqueeze(2).to_broadcast([128, 8, 128]), op=ALU.mult),
                         reads=[T_pa0, T_pa1, T_scT], writes=[T_mixT])
                finish()

            def S1b(j):
                mixT, T_mixT = mixT2[j % 2], T_mixT2[j % 2]
                x1s = j % 2
                P.dma("sp", lambda e: e.dma_start(out=r[:], in_=src_dram[src_row0 + j * 128: src_row0 + (j + 1) * 128, :]), T_r.sem, writes=[T_r])
                for hf in range(2):
                    def f(e, hf=hf):
                        for dc in range(8):
                            i = e.matmul(pyb[:], lhsT=mixT[:, dc, :], rhs=wout[:, dc, hf * 512:(hf + 1) * 512], start=(dc == 0), stop=(dc == 7))
                        return i
                    P.op("pe", f, reads=[T_mixT, T_wout], writes=[T_pyb])
                    P.op("dve", lambda e, hf=hf: e.scalar_tensor_tensor(out=r[:, hf * 512:(hf + 1) * 512], in0=r[:, hf * 512:(hf + 1) * 512], scalar=ALPHA,
                                                                          in1=pyb[:], op0=ALU.mult, op1=ALU.add),
                         reads=[T_pyb], writes=[T_r])
                layer_norm(r, T_r, gb1, T_gb1, x1[x1s], x1b[x1s], T_x1[x1s], lnscr, T_lnscr, "1")

            def S2(j):
                x1s = j % 2
                x1j, x1bj, T_x1j = x1[x1s], x1b[x1s], T_x1[x1s]
                ptr2 = pd[:].bitcast(BF16).rearrange("p (a b) -> p a b", b=128)
                transpose8(x1bj, T_x1j, x1T, T_x1T, ptr2, T_pd)
                def f(e):
                    for oc in range(8):
                        for ic in range(8):
                            i = e.matmul(pc[:, oc // 4, (oc % 4) * 128:(oc % 4 + 1) * 128], lhsT=xq_w[:, ic, oc * 128:(oc + 1) * 128],
                                         rhs=x1T[:, ic, :], start=(ic == 0), stop=(ic == 7))
                    return i
                P.op("pe", f, reads=[T_xq, T_x1T], writes=[T_pc0, T_pc1])
                P.op("act", lambda e: e.copy(out=qxT[:].rearrange("p a b -> p (a b)"), in_=pc[:].rearrange("p a b -> p (a b)")),
                     reads=[T_pc0, T_pc1], writes=[T_qxT])
                T_s = (T_pc0, T_pc1)
                for mc in range(2):
                    def f(e, mc=mc):
                        for h in range(4):
                            for i2 in range(2):
                                i = e.matmul(pc[:, mc, h * 128:(h + 1) * 128], lhsT=KT[:, 2 * h + i2, mc * 128:(mc + 1) * 128],
                                             rhs=qxT[:, 2 * h + i2, :], start=(i2 == 0), stop=(i2 == 1))
                        return i
                    P.op("pe", f, reads=[T_KT, T_qxT], writes=[T_s[mc]])
                    P.op("act", lambda e, mc=mc: e.activation(out=pxT[:, mc, :], in_=pc[:, mc, :], func=AF.Exp, scale=1.0 / 16),
                         reads=[T_s[mc]], writes=[T_pxT[mc]])
                def f(e):
                    for mc in range(2):
                        i = e.matmul(pd[:], lhsT=ones_bf[:], rhs=pxT[:, mc, :], start=(mc == 0), stop=(mc == 1))
                    return i
                P.op("pe", f, reads=T_pxT + [T_ones], writes=[T_pd])
                P.op("act", lambda e: e.activation(out=rdn[:], in_=pd[:], func=AF.Ln), reads=[T_pd], writes=[T_rdn])
                P.op("act", lambda e: e.activation(out=rdn[:], in_=rdn[:], func=AF.Exp, scale=-1.0), reads=[], writes=[T_rdn])
                def f(e):
                    for dc in range(8):
                        h = dc // 2
                        for mc in range(2):
                            i = e.matmul(pc[:, dc // 4, (dc % 4) * 128:(dc % 4 + 1) * 128], lhsT=V[:, mc, dc * 128:(dc + 1) * 128],
                                         rhs=pxT[:, mc, h * 128:(h + 1) * 128], start=(mc == 0), stop=(mc == 1))
                    return i
                P.op("pe", f, reads=T_pxT + [T_V], writes=[T_pc0, T_pc1])
                P.op("dve", lambda e: e.tensor_tensor(out=oTn[:].rearrange("p (h i) t -> p h i t", i=2),
                                                      in0=pc[:].rearrange("p a (b t) -> p (a b) t", t=128).rearrange("p (h i) t -> p h i t", i=2),
                                                      in1=rdn[:].rearrange("p (h t) -> p h t", t=128).unsqueeze(2).to_broadcast([128, 4, 2, 128]),
                                                      op=ALU.mult),
                     reads=[T_pc0, T_pc1, T_rdn], writes=[T_oTn])
                for hf in range(2):
                    def f(e, hf=hf):
                        for dc in range(8):
                            i = e.matmul(pc[:, hf, :], lhsT=oTn[:, dc, :], rhs=xo_w[:, dc, hf * 512:(hf + 1) * 512], start=(dc == 0), stop=(dc == 7))
                        return i
                    P.op("pe", f, reads=[T_oTn, T_xo], writes=[T_s[hf]])
                P.op("dve", lambda e: e.scalar_tensor_tensor(out=r2[:], in0=x1j[:], scalar=ALPHA, in1=pc[:].rearrange("p a b -> p (a b)"),
                                                              op0=ALU.mult, op1=ALU.add),
                     reads=[T_x1j, T_pc0, T_pc1], writes=[T_r2])
                s2 = j % 2
                layer_norm(r2, T_r2, gb2, T_gb2, x2[s2], None, T_x2[s2], lnscr2, T_lnscr2, "2")
                P.dma("sp", lambda e: e.dma_start(out=dst_dram[(j - 1) * 128:j * 128, :], in_=x2[s2][:]), T_x2[s2].sem,
                      reads=[T_x2[s2]], writes=[T_scr_a[layer][j]])
                P.op("act", lambda e: e.copy(out=x2b[:], in_=x2[s2][:]), reads=[T_x2[s2]], writes=[T_x2b])
                transpose8(x2b, T_x2b, x1T, T_x1T, ptr2, T_pd)
                def f(e):
                    for dc in range(8):
                        i = e.matmul(pd[:, 0:NE], lhsT=x1T[:, dc, :], rhs=rw_bf[:, dc, :], start=(dc == 0), stop=(dc == 7))
                    return i
                P.op("pe", f, reads=[T_x1T, T_rw], writes=[T_pd])
                P.op("dve", lambda e: e.tensor_copy(out=lg_all[:, j, :], in_=pd[:, 0:NE]), reads=[T_pd], writes=[T_lg[j]])

            def record(fn, j):
                P.rec = []
                fn(j)
                ops, P.rec = P.rec, None
                return ops

            jfull = 1 if layer == 0 else 2
            for j in range(j0, NT + 2):
                A = record(S1, j) if j < NT else []
                Bm = record(S1b, j - 1) if (j - 1 >= jfull and j - 1 < NT) else []
                C = record(S2, j - 2) if (j - 2 >= jfull) else []
                lists = [l for l in (A, Bm, C) if l]
                idx = [0] * len(lists)
                total = sum(len(l) for l in lists)
                for step in range(total):
                    best, bestv = None, None
                    for q, l in enumerate(lists):
                        if idx[q] < len(l):
                            v = idx[q] / len(l)
                            if bestv is None or v < bestv:
                                best, bestv = q, v
                    if KSEQ:
                        best = next(q for q, l in enumerate(lists) if idx[q] < len(l))
                    P.play(lists[best][idx[best]])
                    idx[best] += 1
            P.barrier()

    def router_batched(n, jbase, plg, T_plg, src_ap=None, T_srcs=None):
        with ExitStack() as rs:
            t16 = {k: sb("rb_" + k, [128, n, NE], F32, rs) for k in ("lg", "ex", "sc", "bi", "sel")}
            t4 = {k: sb("rb4_" + k, [128, n, 4], F32, rs) for k in ("m1", "n1", "m2", "n2", "a", "b", "c", "gs", "gm")}
            t1 = {k: sb("rb1_" + k, [128, n], F32, rs) for k in ("mx", "sum", "gmx", "ws")}
            T = Tk("rb")
            def Dv(fn, reads=(), writes=None):
                P.op("dve", fn, reads=[T] + list(reads), writes=[T] if writes is None else writes)
            def b16(x):
                return x[:].unsqueeze(2).to_broadcast([128, n, NE])
            def b4(x):
                return x[:].unsqueeze(2).to_broadcast([128, n, 4])
            lg, ex, sc, bi, sel = (t16[k] for k in ("lg", "ex", "sc", "bi", "sel"))
            if src_ap is None:
                Dv(lambda e: e.tensor_copy(out=lg[:].rearrange("p a b -> p (a b)"), in_=plg[:, 0:n * NE]), reads=[T_plg])
            else:
                Dv(lambda e: e.tensor_copy(out=lg[:], in_=src_ap), reads=T_srcs)
            Dv(lambda e: e.reduce_max(out=t1["mx"][:], in_=lg[:], axis=AX.X))
            Dv(lambda e: e.tensor_tensor(out=ex[:], in0=lg[:], in1=b16(t1["mx"]), op=ALU.subtract))
            P.op("act", lambda e: e.activation(out=ex[:], in_=ex[:], func=AF.Exp), reads=[T], writes=[T])
            Dv(lambda e: e.reduce_sum(out=t1["sum"][:], in_=ex[:], axis=AX.X))
            Dv(lambda e: e.reciprocal(out=t1["sum"][:], in_=t1["sum"][:]))
            Dv(lambda e: e.tensor_tensor(out=sc[:], in0=ex[:], in1=b16(t1["sum"]), op=ALU.mult))
            Dv(lambda e: e.tensor_tensor(out=bi[:], in0=sc[:], in1=rbias[:].unsqueeze(1).to_broadcast([128, n, NE]), op=ALU.add), reads=[T_const])
            g4 = bi[:].rearrange("p a (g k) -> p a g k", k=4)
            def col(i):
                return g4[:, :, :, i:i + 1].rearrange("p a g k -> p a (g k)")
            Dv(lambda e: e.tensor_tensor(out=t4["m1"][:], in0=col(0), in1=col(1), op=ALU.max))
            Dv(lambda e: e.tensor_tensor(out=t4["n1"][:], in0=col(0), in1=col(1), op=ALU.min))
            Dv(lambda e: e.tensor_tensor(out=t4["m2"][:], in0=col(2), in1=col(3), op=ALU.max))
            Dv(lambda e: e.tensor_tensor(out=t4["n2"][:], in0=col(2), in1=col(3), op=ALU.min))
            Dv(lambda e: e.tensor_tensor(out=t4["a"][:], in0=t4["m1"][:], in1=t4["m2"][:], op=ALU.max))
            Dv(lambda e: e.tensor_tensor(out=t4["b"][:], in0=t4["m1"][:], in1=t4["m2"][:], op=ALU.min))
            Dv(lambda e: e.tensor_tensor(out=t4["c"][:], in0=t4["n1"][:], in1=t4["n2"][:], op=ALU.max))
            Dv(lambda e: e.tensor_tensor(out=t4["b"][:], in0=t4["b"][:], in1=t4["c"][:], op=ALU.max))
            Dv(lambda e: e.tensor_tensor(out=t4["gs"][:], in0=t4["a"][:], in1=t4["b"][:], op=ALU.add))
            Dv(lambda e: e.reduce_max(out=t1["gmx"][:], in_=t4["gs"][:], axis=AX.X))
            Dv(lambda e: e.tensor_tensor(out=t4["gm"][:], in0=t4["gs"][:], in1=b4(t1["gmx"]), op=ALU.is_ge))
            s4 = sel[:].rearrange("p a (g k) -> p a g k", k=4)
            Dv(lambda e: e.tensor_tensor(out=s4, in0=g4, in1=t4["b"][:].unsqueeze(3).to_broadcast([128, n, 4, 4]), op=ALU.is_ge))
            Dv(lambda e: e.tensor_tensor(out=s4, in0=s4, in1=t4["gm"][:].unsqueeze(3).to_broadcast([128, n, 4, 4]), op=ALU.mult))
            Dv(lambda e: e.tensor_tensor(out=sel[:], in0=sel[:], in1=sc[:], op=ALU.mult))
            Dv(lambda e: e.reduce_sum(out=t1["ws"][:], in_=sel[:], axis=AX.X))
            Dv(lambda e: e.reciprocal(out=t1["ws"][:], in_=t1["ws"][:]))
            Dv(lambda e: e.tensor_tensor(out=comb[:, jbase:jbase + n, :], in0=sel[:], in1=b16(t1["ws"]), op=ALU.mult),
               writes=[T] + [T_comb[jbase + i] for i in range(n)])
            P.barrier()

    T_scr_a = [[Tk(f"xsa{l}_{j}") for j in range(NT)] for l in range(2)]
    T_scr_b = [Tk(f"xsb_{j}") for j in range(NT)]

    def phase_B(layer, src_dram, dst_dram, dst_is_out):
        pfx = f"l{layer}_"
        jfirst = 1 if layer == 0 else 2
        tiles = list(range(jfirst, NT))
        half = (len(tiles) + 1) // 2
        chunks = [tiles[:half], tiles[half:]]
        with ExitStack() as st:
            gsem = P.dsem(f"B{layer}g")
            gb3, T_gb3 = load_gb(st, (pfx + "ln3_g", pfx + "ln3_b"), "3", gsem)
            NCH = len(chunks[0])
            r = sb("rB", [128, NCH, D], F32, st)
            T_r = [Tk(f"rB{i}") for i in range(NCH)]
            xTa = sb("xTa", [128, 8, NCH * 128], BF16, st)
            T_xTa = [Tk(f"xTa{i}") for i in range((NCH + 3) // 4)]
            NS = 4
            sem_x = [P.dsem(f"B{layer}x{k}") for k in range(NS)]
            sem_o = [P.dsem(f"B{layer}o{k}") for k in range(NS)]

            def record0(fn):
                P.rec = []
                fn()
                ops, P.rec = P.rec, None
                return ops

            def play_waves(streams):
                for w0 in range(0, len(streams), NS):
                    wave = streams[w0:w0 + NS]
                    idx = [0] * len(wave)
                    live = True
                    while live:
                        live = False
                        for k, st_ in enumerate(wave):
                            if idx[k] < len(st_):
                                P.play(st_[idx[k]])
                                idx[k] += 1
                                live = True

            ptr = ps("ptrB", [128, 8, 128], BF16, st); T_ptr = Tk("ptrB")
            pg = [ps(f"pg{i}", [128, 512], F32, st) for i in range(2)]
            pu = [ps(f"pu{i}", [128, 512], F32, st) for i in range(2)]
            py = [ps(f"py{i}", [128, 2, 512], F32, st) for i in range(1)]
            plg = ps("plg", [128, 512], F32, st); T_plg = Tk("plg")
            T_pg = [Tk("pg0"), Tk("pg1")]
            T_pu = [Tk("pu0"), Tk("pu1")]
            T_py = [[Tk("py0a"), Tk("py0b")]]

            for ci, ch in enumerate(chunks):
                n = len(ch)
                pst = ExitStack()
                xl4 = [sb(f"xlp{k}", [128, D], F32, pst) for k in range(NS)]
                T_xl4 = [Tk(f"xlp{k}", sem_x[k]) for k in range(NS)]
                xlb4 = [sb(f"xlbp{k}", [128, D], BF16, pst) for k in range(NS)]
                T_xlb4 = [Tk(f"xlbp{k}") for k in range(NS)]
                trb = [b_[:].bitcast(BF16).rearrange("p (a b) -> p a b", b=128) for b_ in (pg[0], pg[1], pu[0], pu[1])]
                T_trb = [T_pg[0], T_pg[1], T_pu[0], T_pu[1]]
                streams = []
                for i, j in enumerate(ch):
                    def tile_pro(i=i, j=j, k=i % NS):
                        P.dma("sp", lambda e: e.dma_start(out=xl4[k][:], in_=src_dram[(j - 1) * 128:j * 128, :]), sem_x[k],
                              reads=[T_scr_a[layer][j]], writes=[T_xl4[k]])
                        P.op("act", lambda e: e.mul(out=r[:, i, :], in_=xl4[k][:], mul=ALPHA), reads=[T_xl4[k]], writes=[T_r[i]])
                        P.op("dve", lambda e: e.tensor_copy(out=xlb4[k][:], in_=xl4[k][:]), reads=[T_xl4[k]], writes=[T_xlb4[k]])
                        def f(e):
                            for dc in range(8):
                                ii = e.transpose(out=trb[k][:, dc, :], in_=xlb4[k][:, dc * 128:(dc + 1) * 128], identity=ident[:])
                            return ii
                        P.op("pe", f, reads=[T_xlb4[k], T_const], writes=[T_trb[k]])
                        P.op("dve", lambda e: e.tensor_copy(out=xTa[:, :, i * 128:(i + 1) * 128], in_=trb[k]),
                             reads=[T_trb[k]], writes=[T_xTa[i // 4]])
                        def f2(e):
                            for dc in range(8):
                                ii = e.matmul(plg[:, i * NE:(i + 1) * NE], lhsT=xTa[:, dc, i * 128:(i + 1) * 128], rhs=rw_bf[:, dc, :],
                                              start=(dc == 0), stop=(dc == 7))
                            return ii
                        P.op("pe", f2, reads=[T_xTa[i // 4], T_rw], writes=[T_plg])
                    streams.append(record0(tile_pro))
                play_waves(streams)
                P.barrier()
                pst.close()
                router_batched(n, ch[0], plg, T_plg)
                P.barrier()
                ws_ = ExitStack()
                wg = [sb(f"wg{i}", [128, 8, 512], BF16, ws_) for i in range(2)]
                wu = [sb(f"wu{i}", [128, 8, 512], BF16, ws_) for i in range(2)]
                wd = [sb(f"wd{i}", [128, 4, D], BF16, ws_) for i in range(2)]
                T_wg = [Tk(f"wg{i}") for i in range(2)]
                T_wu = [Tk(f"wu{i}") for i in range(2)]
                T_wd = [Tk(f"wd{i}") for i in range(2)]
                hT = [sb(f"hT{i}", [128, 4, 512], BF16, ws_) for i in range(2)]
                T_hT = [[Tk(f"hT{i}_{fc}") for fc in range(4)] for i in range(2)]
                sg = [sb(f"sg{i}", [128, 512], F32, ws_) for i in range(2)]
                T_sg = [Tk("sg0"), Tk("sg1")]
                wcount = [0]

                def load_expert(e_idx):
                    s = wcount[0] % 2
                    wcount[0] += 1
                    stream_weight(wg[s], W[pfx + "e_gate"][e_idx], 8, 512, T_wg[s])
                    stream_weight(wu[s], W[pfx + "e_up"][e_idx], 8, 512, T_wu[s])
                    stream_weight(wd[s], W[pfx + "e_down"][e_idx], 4, D, T_wd[s])
                    return s

                groups = [list(range(g0, min(g0 + 4, n))) for g0 in range(0, n, 4)]
                ws_next = load_expert(0)
                gcount = 0
                for ex in range(NE):
                    ws = ws_next
                    if ex + 1 < NE:
                        ws_next = load_expert(ex + 1)
                    for gi, grp in enumerate(groups):
                        ntok = len(grp) * 128
                        t0 = grp[0] * 128
                        hs = gcount % 2
                        gcount += 1
                        for fc in range(4):
                            b = fc % 2
                            def f(e, fc=fc, b=b):
                                for dc in range(8):
                                    i = e.matmul(pg[b][:, 0:ntok], lhsT=wg[ws][:, dc, fc * 128:(fc + 1) * 128], rhs=xTa[:, dc, t0:t0 + ntok],
                                                 start=(dc == 0), stop=(dc == 7))
                                return i
                            P.op("pe", f, reads=[T_wg[ws], T_xTa[gi]], writes=[T_pg[b]])
                            def f(e, fc=fc, b=b):
                                for dc in range(8):
                                    i = e.matmul(pu[b][:, 0:ntok], lhsT=wu[ws][:, dc, fc * 128:(fc + 1) * 128], rhs=xTa[:, dc, t0:t0 + ntok],
                                                 start=(dc == 0), stop=(dc == 7))
                                return i
                            P.op("pe", f, reads=[T_wu[ws], T_xTa[gi]], writes=[T_pu[b]])
                            P.op("act", lambda e, b=b: e.activation(out=sg[b][:, 0:ntok], in_=pg[b][:, 0:ntok], func=AF.Silu),
                                 reads=[T_pg[b]], writes=[T_sg[b]])
                            P.op("dve", lambda e, b=b, fc=fc: e.tensor_tensor(out=hT[hs][:, fc, 0:ntok], in0=sg[b][:, 0:ntok], in1=pu[b][:, 0:ntok],
                                                                               op=ALU.mult),
                                 reads=[T_sg[b], T_pu[b]], writes=[T_hT[hs][fc]])
                        for ti, i in enumerate(grp):
                            j = ch[i]
                            yb = 0
                            for hf in range(2):
                                def f(e, hf=hf, ti=ti):
                                    for fc in range(4):
                                        ii = e.matmul(py[yb][:, hf, :], lhsT=hT[hs][:, fc, ti * 128:(ti + 1) * 128],
                                                      rhs=wd[ws][:, fc, hf * 512:(hf + 1) * 512], start=(fc == 0), stop=(fc == 3))
                                    return ii
                                P.op("pe", f, reads=T_hT[hs] + [T_wd[ws]], writes=[T_py[yb][hf]])
                                P.op("dve", lambda e, hf=hf, i=i, j=j: e.scalar_tensor_tensor(
                                    out=r[:, i, hf * 512:(hf + 1) * 512], in0=py[yb][:, hf, :], scalar=comb[:, j, ex:ex + 1],
                                    in1=r[:, i, hf * 512:(hf + 1) * 512], op0=ALU.mult, op1=ALU.add),
                                    reads=[T_py[yb][hf], T_comb[j]], writes=[T_r[i]])
                P.barrier()
                ws_.close()
                est = ExitStack()
                xo4 = [sb(f"xo4_{k}", [128, D], F32, est) for k in range(NS)]
                T_xo4 = [Tk(f"xo4_{k}", sem_o[k]) for k in range(NS)]
                lns4 = [(sb(f"st6e{k}", [128, 2, 6], F32, est), sb(f"mve{k}", [128, 2], F32, est), sb(f"sde{k}", [128, 1], F32, est),
                         sb(f"rstde{k}", [128, 1], F32, est), sb(f"nmre{k}", [128, 1], F32, est)) for k in range(NS)]
                T_lns4 = [Tk(f"lnse{k}") for k in range(NS)]
                streams = []
                for i, j in enumerate(ch):
                    def tile_epi(i=i, j=j, k=i % NS):
                        layer_norm(r[:, i, :], T_r[i], gb3, T_gb3, xo4[k], None, T_xo4[k], lns4[k], T_lns4[k], "3")
                        if dst_is_out:
                            dst = dst_dram[(j - 2) * 128:(j - 1) * 128, :]
                        else:
                            dst = dst_dram[(j - 1) * 128:j * 128, :]
                        P.dma("sp", lambda e: e.dma_start(out=dst, in_=xo4[k][:]), sem_o[k],
                              reads=[T_xo4[k]], writes=[T_scr_b[j]])
                    streams.append(record0(tile_epi))
                play_waves(streams)
                P.barrier()
                est.close()
            P.barrier()

    def phase_B_sparse(layer, src_dram, dst_dram, dst_is_out):
        pfx = f"l{layer}_"
        U32 = mybir.dt.uint32
        jfirst = 1 if layer == 0 else 2
        tiles = list(range(jfirst, NT))
        n = len(tiles)
        j0 = tiles[0]
        S = (n * 256 + 16 * 511) // 512
        half = (n + 1) // 2
        chunks = [tiles[:half], tiles[half:]]
        Wg2 = W[pfx + "e_gate"].rearrange("e k f -> (e k) f")
        Wu2 = W[pfx + "e_up"].rearrange("e k f -> (e k) f")
        Wd2 = W[pfx + "e_down"].rearrange("e k f -> (e k) f")
        IOA = bass.IndirectOffsetOnAxis
        with ExitStack() as st:
            gsem = P.dsem(f"B{layer}g")
            gb3, T_gb3 = load_gb(st, (pfx + "ln3_g", pfx + "ln3_b"), "3", gsem)
            NS = 4
            NS5 = 8
            sem_x = [P.gsem(f"Bx{k}") for k in range(NS5)]
            sem_o = [P.gsem(f"Bo{k}") for k in range(NS5)]
            sem_a = [P.gsem(f"Ba{k}") for k in range(NS5)]
            sem_b = [P.gsem(f"Bb{k}") for k in range(NS5)]
            posA_i = sb("posA_i", [128, NT], I32, st)
            posB_i = sb("posB_i", [128, NT], I32, st)
            wA = sb("wA", [128, NT], F32, st)
            wB = sb("wB", [128, NT], F32, st)
            idx_gu = sb("idx_gu", [128, S_MAX, 8], I32, st)
            idx_d = sb("idx_d", [128, S_MAX, 4], I32, st)
            T_pos = Tk("pos")
            ptr = ps("ptrB", [128, 8, 128], BF16, st); T_ptr = Tk("ptrB")
            pg = [ps(f"pg{i}", [128, 512], F32, st) for i in range(2)]
            pu = [ps(f"pu{i}", [128, 512], F32, st) for i in range(2)]
            py = ps("py0", [128, 2, 512], F32, st)
            plg = ps("plg", [128, 512], F32, st); T_plg = Tk("plg")
            T_pg = [Tk("pg0"), Tk("pg1")]
            T_pu = [Tk("pu0"), Tk("pu1")]
            T_py = [Tk("py0a"), Tk("py0b")]

            def record0(fn):
                P.rec = []
                fn()
                ops, P.rec = P.rec, None
                return ops

            def play_waves(streams, NS=NS):
                n_ = len(streams)
                L_ = max(len(x) for x in streams)
                stride = max(1, L_ // NS)
                slots = [None] * NS
                nxt = [k for k in range(NS)]
                done = 0
                rnd = 0
                while done < n_:
                    for k in range(NS):
                        if slots[k] is None and nxt[k] < n_ and rnd >= k * stride:
                            slots[k] = [streams[nxt[k]], 0]
                            nxt[k] += NS
                        if slots[k] is not None:
                            st_, ix = slots[k]
                            P.play(st_[ix])
                            slots[k][1] += 1
                            if slots[k][1] >= len(st_):
                                slots[k] = None
                                done += 1
                    rnd += 1

            router_batched(n, j0, None, None, src_ap=lg_all[:, j0:j0 + n, :], T_srcs=[T_lg[j] for j in tiles])

            with ExitStack() as qs:
                t16 = {k: sb("q16_" + k, [128, n, NE], F32, qs) for k in ("sel", "rank", "cnt", "off", "pos", "val", "m", "tmp")}
                selb = sb("q_selb", [128, n, NE], BF16, qs)
                e16 = {k: sb("qe_" + k, [128, NE], F32, qs) for k in ("total", "nslot", "end", "base")}
                cmp1 = sb("q_cmp1", [128, NE, 17], F32, qs)
                cmp2 = sb("q_cmp2", [128, S_MAX, NE], F32, qs)
                slot_e = sb("q_slote", [128, S_MAX], F32, qs)
                pAB = {k: sb("q_" + k, [128, n], F32, qs) for k in ("pA", "pB")}
                T = Tk("posq")
                def Dv(fn, reads=(), writes=None):
                    P.op("dve", fn, reads=[T] + list(reads), writes=[T] if writes is None else writes)
                cv = comb[:, j0:j0 + n, :]
                sel, rank, cnt, off, pos, val, mm_, tmp = (t16[k] for k in ("sel", "rank", "cnt", "off", "pos", "val", "m", "tmp"))
                Dv(lambda e: e.tensor_scalar(out=sel[:], in0=cv, scalar1=0.0, scalar2=None, op0=ALU.is_gt), reads=[T_comb[j] for j in tiles])
                Dv(lambda e: e.tensor_copy(out=selb[:], in_=sel[:]))
                n1 = min(n, 17)
                c1, c2 = n1 * NE, (n - n1) * NE
                selbf = selb[:].rearrange("p a b -> p (a b)")
                def f(e):
                    e.matmul(pg[0][:, 0:c1], lhsT=lstrict[:], rhs=selbf[:, 0:c1], start=True, stop=True)
                    e.matmul(pg[1][:, 0:c2], lhsT=lstrict[:], rhs=selbf[:, c1:c1 + c2], start=True, stop=True)
                    e.matmul(pu[0][:, 0:c1], lhsT=ones_bf[:], rhs=selbf[:, 0:c1], start=True, stop=True)
                    return e.matmul(pu[1][:, 0:c2], lhsT=ones_bf[:], rhs=selbf[:, c1:c1 + c2], start=True, stop=True)
                P.op("pe", f, reads=[T, T_const, T_ones], writes=T_pg + T_pu)
                rankf = rank[:].rearrange("p a b -> p (a b)")
                cntf = cnt[:].rearrange("p a b -> p (a b)")
                Dv(lambda e: e.tensor_copy(out=rankf[:, 0:c1], in_=pg[0][:, 0:c1]), reads=[T_pg[0]])
                Dv(lambda e: e.tensor_copy(out=rankf[:, c1:c1 + c2], in_=pg[1][:, 0:c2]), reads=[T_pg[1]])
                Dv(lambda e: e.tensor_copy(out=cntf[:, 0:c1], in_=pu[0][:, 0:c1]), reads=[T_pu[0]])
                Dv(lambda e: e.tensor_copy(out=cntf[:, c1:c1 + c2], in_=pu[1][:, 0:c2]), reads=[T_pu[1]])
                Dv(lambda e: e.memset(off[:, 0, :], 0.0))
                for jj in range(1, n):
                    Dv(lambda e, jj=jj: e.tensor_tensor(out=off[:, jj, :], in0=off[:, jj - 1, :], in1=cnt[:, jj - 1, :], op=ALU.add))
                Dv(lambda e: e.tensor_tensor(out=e16["total"][:], in0=off[:, n - 1, :], in1=cnt[:, n - 1, :], op=ALU.add))
                Dv(lambda e: e.tensor_tensor(out=cmp1[:], in0=e16["total"][:].unsqueeze(2).to_broadcast([128, NE, 17]),
                                             in1=tabs[:, 0:17].unsqueeze(1).to_broadcast([128, NE, 17]), op=ALU.is_gt), reads=[T_const])
                Dv(lambda e: e.reduce_sum(out=e16["nslot"][:], in_=cmp1[:], axis=AX.X))
                Dv(lambda e: e.tensor_copy(out=e16["end"][:], in_=e16["nslot"][:]))
                for ee in range(1, NE):
                    Dv(lambda e, ee=ee: e.tensor_tensor(out=e16["end"][:, ee:ee + 1], in0=e16["end"][:, ee - 1:ee],
                                                        in1=e16["nslot"][:, ee:ee + 1], op=ALU.add))
                Dv(lambda e: e.tensor_tensor(out=e16["base"][:], in0=e16["end"][:], in1=e16["nslot"][:], op=ALU.subtract))
                Dv(lambda e: e.tensor_scalar_mul(out=e16["base"][:], in0=e16["base"][:], scalar1=512.0))
                Dv(lambda e: e.tensor_tensor(out=pos[:], in0=rank[:], in1=off[:], op=ALU.add))
                Dv(lambda e: e.tensor_tensor(out=pos[:], in0=pos[:], in1=e16["base"][:].unsqueeze(1).to_broadcast([128, n, NE]), op=ALU.add))
                Dv(lambda e: e.scalar_tensor_tensor(out=val[:], in0=pos[:], scalar=1.0, in1=sel[:], op0=ALU.add, op1=ALU.mult))
                for which, (pX, pos_i, wX) in enumerate(((pAB["pA"], posA_i, wA), (pAB["pB"], posB_i, wB))):
                    Dv(lambda e, pX=pX: e.reduce_max(out=pX[:], in_=val[:], axis=AX.X))
                    Dv(lambda e, pX=pX: e.tensor_tensor(out=mm_[:], in0=val[:], in1=pX[:].unsqueeze(2).to_broadcast([128, n, NE]), op=ALU.is_equal))
                    Dv(lambda e: e.tensor_tensor(out=tmp[:], in0=cv, in1=mm_[:], op=ALU.mult))
                    Dv(lambda e, wX=wX: e.reduce_sum(out=wX[:, j0:j0 + n], in_=tmp[:], axis=AX.X), writes=[T, T_pos])
                    Dv(lambda e, pX=pX, pos_i=pos_i: e.tensor_scalar(out=pos_i[:, j0:j0 + n], in0=pX[:], scalar1=-1.0, scalar2=0.0,
                                                                     op0=ALU.add, op1=ALU.max), writes=[T, T_pos])
                    if which == 0:
                        Dv(lambda e: e.tensor_tensor(out=tmp[:], in0=val[:], in1=mm_[:], op=ALU.mult))
                        Dv(lambda e: e.tensor_tensor(out=val[:], in0=val[:], in1=tmp[:], op=ALU.subtract))
                sidx = tabs[:, 17:17 + S_MAX]
                kp = tabs[:, 17 + S_MAX:17 + S_MAX + 8]
                Dv(lambda e: e.tensor_tensor(out=cmp2[:], in0=sidx.unsqueeze(2).to_broadcast([128, S_MAX, NE]),
                                             in1=e16["end"][:].unsqueeze(1).to_broadcast([128, S_MAX, NE]), op=ALU.is_ge), reads=[T_const])
                Dv(lambda e: e.reduce_sum(out=slot_e[:], in_=cmp2[:], axis=AX.X))
                Dv(lambda e: e.tensor_scalar_min(out=slot_e[:], in0=slot_e[:], scalar1=float(NE - 1)))
                Dv(lambda e: e.scalar_tensor_tensor(out=idx_gu[:], in0=slot_e[:].unsqueeze(2).to_broadcast([128, S_MAX, 8]), scalar=1024.0,
                                                    in1=kp.unsqueeze(1).to_broadcast([128, S_MAX, 8]), op0=ALU.mult, op1=ALU.add),
                   reads=[T_const], writes=[T, T_pos])
                Dv(lambda e: e.scalar_tensor_tensor(out=idx_d[:], in0=slot_e[:].unsqueeze(2).to_broadcast([128, S_MAX, 4]), scalar=512.0,
                                                    in1=kp[:, 0:4].unsqueeze(1).to_broadcast([128, S_MAX, 4]), op0=ALU.mult, op1=ALU.add),
                   reads=[T_const], writes=[T, T_pos])
                P.barrier()

            with ExitStack() as ss:
                xl4 = [sb(f"xls{k}", [128, D], F32, ss) for k in range(NS5)]
                T_xl4 = [Tk(f"xls{k}", sem_x[k]) for k in range(NS5)]
                xlb4 = [sb(f"xlbs{k}", [128, D], BF16, ss) for k in range(NS5)]
                T_xlb4 = [Tk(f"xlbs{k}", sem_a[k]) for k in range(NS5)]
                T_xsort = Tk("xsort")
                streams = []
                for i, j in enumerate(tiles):
                    def tile_sc(i=i, j=j, k=i % NS5):
                        P.dma("sp", lambda e: e.dma_start(out=xl4[k][:], in_=src_dram[(j - 1) * 128:j * 128, :]), sem_x[k],
                              reads=[T_scr_a[layer][j]], writes=[T_xl4[k]])
                        P.op("dve", lambda e: e.tensor_copy(out=xlb4[k][:], in_=xl4[k][:]), reads=[T_xl4[k]], writes=[T_xlb4[k]])
                        P.dma("pool", lambda e: [
                            e.indirect_dma_start(out=xsort, out_offset=IOA(ap=posA_i[:, j:j + 1].bitcast(U32), axis=0), in_=xlb4[k][:], in_offset=None),
                            e.indirect_dma_start(out=xsort, out_offset=IOA(ap=posB_i[:, j:j + 1].bitcast(U32), axis=0), in_=xlb4[k][:], in_offset=None)],
                            sem_a[k], reads=[T_xlb4[k], T_pos, T_xsort_g], writes=[T_xsort])
                    streams.append(record0(tile_sc))
                play_waves(streams, NS5)
                P.barrier()

            with ExitStack() as ws_:
                wg = [sb(f"wg{i}", [128, 8, 512], BF16, ws_) for i in range(2)]
                wu = [sb(f"wu{i}", [128, 8, 512], BF16, ws_) for i in range(2)]
                wd = [sb(f"wd{i}", [128, 4, D], BF16, ws_) for i in range(2)]
                T_wg = [Tk(f"wg{i}", P.gsem(f"Bwg{i}")) for i in range(2)]
                T_wu = [Tk(f"wu{i}", P.gsem(f"Bwu{i}")) for i in range(2)]
                T_wd = [Tk(f"wd{i}", P.gsem(f"Bwd{i}")) for i in range(2)]
                hT = [sb(f"hT{i}", [128, 4, 512], BF16, ws_) for i in range(2)]
                T_hT = [[Tk(f"hT{i}_{fc}") for fc in range(4)] for i in range(2)]
                sg = [sb(f"sg{i}", [128, 512], F32, ws_) for i in range(2)]
                T_sg = [Tk("sg0"), Tk("sg1")]
                xs4 = [sb(f"xs4_{i}", [128, 4, D], BF16, ws_) for i in range(2)]
                T_xs4 = [Tk(f"xs4_{i}", P.gsem(f"Bxs{i}")) for i in range(2)]
                xTs = [sb(f"xTs{i}", [128, 8, 512], BF16, ws_) for i in range(2)]
                T_xTs = [Tk(f"xTs{i}") for i in range(2)]
                ysb = [sb(f"ysb{i}", [128, D], F32, ws_) for i in range(4)]
                T_ysb = [Tk(f"ysb{i}", sem_o[i]) for i in range(4)]
                T_ysort = Tk("ysort")
                trs = [ptr[:], plg[:].bitcast(BF16).rearrange("p (a b) -> p a b", b=128)]
                T_trs = [T_ptr, T_plg]

                def load_slot(s_):
                    b = s_ % 2
                    P.dma("pool", lambda e: [e.indirect_dma_start(out=wg[b][:, k, :], out_offset=None, in_=Wg2,
                                                                  in_offset=IOA(ap=idx_gu[:, s_, k:k + 1].bitcast(U32), axis=0)) for k in range(8)],
                          T_wg[b].sem, reads=[T_pos], writes=[T_wg[b]])
                    P.dma("pool", lambda e: [e.indirect_dma_start(out=wu[b][:, k, :], out_offset=None, in_=Wu2,
                                                                  in_offset=IOA(ap=idx_gu[:, s_, k:k + 1].bitcast(U32), axis=0)) for k in range(8)],
                          T_wu[b].sem, reads=[T_pos], writes=[T_wu[b]])
                    P.dma("pool", lambda e: [e.indirect_dma_start(out=wd[b][:, k, :], out_offset=None, in_=Wd2,
                                                                  in_offset=IOA(ap=idx_d[:, s_, k:k + 1].bitcast(U32), axis=0)) for k in range(4)],
                          T_wd[b].sem, reads=[T_pos], writes=[T_wd[b]])
                    P.dma("sp", lambda e: e.dma_start(out=xs4[b][:], in_=xsort[s_ * 512:(s_ + 1) * 512, :].rearrange("(t p) f -> p t f", p=128)),
                          T_xs4[b].sem, writes=[T_xs4[b]])

                load_slot(0)
                ycount = 0
                trc = 0
                for s_ in range(S):
                    b = s_ % 2
                    if s_ + 1 < S:
                        load_slot(s_ + 1)
                    for t in range(4):
                        tb = trc % 2
                        trc += 1
                        def f(e, t=t, tb=tb):
                            for dc in range(8):
                                ii = e.transpose(out=trs[tb][:, dc, :], in_=xs4[b][:, t, dc * 128:(dc + 1) * 128], identity=ident[:])
                            return ii
                        P.op("pe", f, reads=[T_xs4[b], T_const], writes=[T_trs[tb]])
                        if t % 2 == 0:
                            P.op("act", lambda e, t=t, tb=tb: e.copy(out=xTs[b][:, :, t * 128:(t + 1) * 128], in_=trs[tb]),
                                 reads=[T_trs[tb]], writes=[T_xTs[b]])
                        else:
                            P.op("dve", lambda e, t=t, tb=tb: e.tensor_copy(out=xTs[b][:, :, t * 128:(t + 1) * 128], in_=trs[tb]),
                                 reads=[T_trs[tb]], writes=[T_xTs[b]])
                    hs = s_ % 2
                    for fc in range(4):
                        bb = fc % 2
                        def f(e, fc=fc, bb=bb):
                            for dc in range(8):
                                i = e.matmul(pg[bb][:], lhsT=wg[b][:, dc, fc * 128:(fc + 1) * 128], rhs=xTs[b][:, dc, :],
                                             start=(dc == 0), stop=(dc == 7))
                            return i
                        P.op("pe", f, reads=[T_wg[b], T_xTs[b]], writes=[T_pg[bb]])
                        def f(e, fc=fc, bb=bb):
                            for dc in range(8):
                                i = e.matmul(pu[bb][:], lhsT=wu[b][:, dc, fc * 128:(fc + 1) * 128], rhs=xTs[b][:, dc, :],
                                             start=(dc == 0), stop=(dc == 7))
                            return i
                        P.op("pe", f, reads=[T_wu[b], T_xTs[b]], writes=[T_pu[bb]])
                        P.op("act", lambda e, bb=bb: e.activation(out=sg[bb][:], in_=pg[bb][:], func=AF.Silu),
                             reads=[T_pg[bb]], writes=[T_sg[bb]])
                        P.op("dve", lambda e, bb=bb, fc=fc: e.tensor_tensor(out=hT[hs][:, fc, :], in0=sg[bb][:], in1=pu[bb][:], op=ALU.mult),
                             reads=[T_sg[bb], T_pu[bb]], writes=[T_hT[hs][fc]])
                    for t in range(4):
                        for hf in range(2):
                            def f(e, hf=hf, t=t):
                                for fc in range(4):
                                    ii = e.matmul(py[:, hf, :], lhsT=hT[hs][:, fc, t * 128:(t + 1) * 128],
                                                  rhs=wd[b][:, fc, hf * 512:(hf + 1) * 512], start=(fc == 0), stop=(fc == 3))
                                return ii
                            P.op("pe", f, reads=T_hT[hs] + [T_wd[b]], writes=[T_py[hf]])
                        yq = ycount % 4
                        ycount += 1
                        P.op("act", lambda e, yq=yq: e.copy(out=ysb[yq][:, 0:512], in_=py[:, 0, :]), reads=[T_py[0]], writes=[T_ysb[yq]])
                        P.op("dve", lambda e, yq=yq: e.tensor_copy(out=ysb[yq][:, 512:1024], in_=py[:, 1, :]), reads=[T_py[1]], writes=[T_ysb[yq]])
                        r0 = s_ * 512 + t * 128
                        P.dma("sp", lambda e, yq=yq, r0=r0: e.dma_start(out=ysort[r0:r0 + 128, :], in_=ysb[yq][:]), sem_o[yq],
                              reads=[T_ysb[yq]], writes=[T_ysort])
                P.barrier()

            with ExitStack() as cs:
                xc = [sb(f"xc{k}", [128, D], F32, cs) for k in range(NS5)]
                T_xc = [Tk(f"xc{k}", sem_x[k]) for k in range(NS5)]
                ya = [sb(f"ya{k}", [128, D], F32, cs) for k in range(NS5)]
                T_ya = [Tk(f"ya{k}", sem_a[k]) for k in range(NS5)]
                yb = [sb(f"yb{k}", [128, D], F32, cs) for k in range(NS5)]
                T_yb = [Tk(f"yb{k}", sem_b[k]) for k in range(NS5)]
                xo4 = [sb(f"xo4_{k}", [128, D], F32, cs) for k in range(NS5)]
                T_xo4 = [Tk(f"xo4_{k}", sem_o[k]) for k in range(NS5)]
                lns4 = [(sb(f"st6e{k}", [128, 2, 6], F32, cs), sb(f"mve{k}", [128, 2], F32, cs), sb(f"sde{k}", [128, 1], F32, cs),
                         sb(f"rstde{k}", [128, 1], F32, cs), sb(f"nmre{k}", [128, 1], F32, cs)) for k in range(NS5)]
                T_lns4 = [Tk(f"lnse{k}") for k in range(NS5)]
                streams = []
                for i, j in enumerate(tiles):
                    def tile_cb(i=i, j=j, k=i % NS5):
                        P.dma("sp", lambda e: e.dma_start(out=xc[k][:], in_=src_dram[(j - 1) * 128:j * 128, :]), sem_x[k],
                              reads=[T_scr_a[layer][j]], writes=[T_xc[k]])
                        P.dma("pool", lambda e: e.indirect_dma_start(out=ya[k][:], out_offset=None, in_=ysort,
                                                                     in_offset=IOA(ap=posA_i[:, j:j + 1].bitcast(U32), axis=0)),
                              sem_a[k], reads=[T_pos], writes=[T_ya[k]])
                        P.dma("pool", lambda e: e.indirect_dma_start(out=yb[k][:], out_offset=None, in_=ysort,
                                                                     in_offset=IOA(ap=posB_i[:, j:j + 1].bitcast(U32), axis=0)),
                              sem_b[k], reads=[T_pos], writes=[T_yb[k]])
                        P.op("act", lambda e: e.mul(out=xc[k][:], in_=xc[k][:], mul=ALPHA), reads=[], writes=[T_xc[k]])
                        P.op("dve", lambda e: e.scalar_tensor_tensor(out=xc[k][:], in0=ya[k][:], scalar=wA[:, j:j + 1], in1=xc[k][:],
                                                                      op0=ALU.mult, op1=ALU.add), reads=[T_ya[k], T_pos], writes=[T_xc[k]])
                        P.op("dve", lambda e: e.scalar_tensor_tensor(out=xc[k][:], in0=yb[k][:], scalar=wB[:, j:j + 1], in1=xc[k][:],
                                                                      op0=ALU.mult, op1=ALU.add), reads=[T_yb[k], T_pos], writes=[T_xc[k]])
                        layer_norm(xc[k], T_xc[k], gb3, T_gb3, xo4[k], None, T_xo4[k], lns4[k], T_lns4[k], "3", norm_on_act=True)
                        if dst_is_out:
                            dst = dst_dram[(j - 2) * 128:(j - 1) * 128, :]
                        else:
                            dst = dst_dram[(j - 1) * 128:j * 128, :]
                        P.dma("sp", lambda e: e.dma_start(out=dst, in_=xo4[k][:]), sem_o[k], reads=[T_xo4[k]], writes=[T_scr_b[j]])
                    streams.append(record0(tile_cb))
                play_waves(streams, NS5)
                P.barrier()
            P.barrier()


    def copy_out(src_dram, T_src):
        with ExitStack() as st:
            buf = [sb(f"cb{i}", [128, D], F32, st) for i in range(2)]
            T_b = [Tk(f"cb{i}", P.dsem(f"cb{i}")) for i in range(2)]
            for j in range(2, NT):
                s = j % 2
                P.dma("sp", lambda e: e.dma_start(out=buf[s][:], in_=src_dram[(j - 1) * 128:j * 128, :]), T_b[s].sem,
                      reads=[T_src[j]], writes=[T_b[s]])
                P.dma("sp", lambda e: e.dma_start(out=out[(j - 2) * 128:(j - 1) * 128, :], in_=buf[s][:]), T_b[s].sem,
                      reads=[T_b[s]], writes=[Tk("o")])
            P.barrier()

    if stage == "S0":
        osem = P.dsem("dbg")
        P.dma("sp", lambda e: [e.dma_start(out=out[0:128, 0:NT * 8], in_=cosT[:].rearrange("p a b -> p (a b)")),
                               e.dma_start(out=out[128:256, 0:NT * 8], in_=sinT[:].rearrange("p a b -> p (a b)"))],
              osem, reads=[T_cs], writes=[Tk("o")])
        P.barrier()
        return
    phase_A(0, xin, 0, xs_a)
    if stage == "S1" or STOPPED[0]:
        return
    if stage == "A0":
        copy_out(xs_a, T_scr_a[0])
    else:
        (phase_B_sparse if SPARSE else phase_B)(0, xs_a, xs_b, False)
        if stage == "B0":
            copy_out(xs_b, T_scr_b)
        else:
            phase_A(1, xs_b, -128, xs_a)
            if stage == "A1":
                copy_out(xs_a, T_scr_a[1])
            else:
                (phase_B_sparse if SPARSE else phase_B)(1, xs_a, out, True)
    P.barrier()


_QPERM = [0, 4, 1, 5, 2, 6, 3, 7]


def _constants():
    bf = ml_dtypes.bfloat16
    k = np.arange(128)[:, None]
    q = np.arange(128)[None, :]
    m_prev = (k > q).astype(np.float32)
    m_cur = (k <= q).astype(np.float32)
    tril = (k <= q).astype(np.float32)
    poolB = np.zeros((128, 4, 4, 128), np.float32)
    s = np.arange(128)[:, None]
    t = np.arange(128)[None, :]
    for g, win in enumerate((2, 4, 8, 16)):
        cur = ((s <= t) & (s > t - win)).astype(np.float32) / win - (s == t).astype(np.float32)
        prev = ((s - 128) > (t - win)).astype(np.float32) / win
        cnt = np.minimum(t + 1, win).astype(np.float32)
        fcur = ((s <= t) & (s > t - win)).astype(np.float32) / cnt - (s == t).astype(np.float32)
        poolB[:, 0, g, :] = prev
        poolB[:, 1, g, :] = cur
        poolB[:, 2, g, :] = 0.0
        poolB[:, 3, g, :] = fcur
    half = 4
    invf = (500000.0 ** (-(np.arange(half * 2, dtype=np.float32) * 2.0 / 16.0))).astype(np.float32)
    invf = np.broadcast_to(invf[None, :], (128, 8)).copy()
    tabs = np.zeros((128, 17 + S_MAX + 8), np.float32)
    tabs[:, 0:17] = (np.arange(17) * 512.0)[None, :]
    tabs[:, 17:17 + S_MAX] = np.arange(S_MAX, dtype=np.float32)[None, :]
    tabs[:, 17 + S_MAX:] = (np.arange(8)[None, :] * 128 + np.arange(128)[:, None]).astype(np.float32)
    lstrict = (k < q).astype(np.float32).astype(bf)
    return dict(tabs=tabs, lstrict=lstrict, ident=np.eye(128, dtype=np.float32).astype(bf), m_prev=m_prev, m_cur=m_cur, tril=tril, poolB=poolB, invf=invf)


def make_in_maps(inputs):
    bf = ml_dtypes.bfloat16
    C = _constants()
    x = np.asarray(inputs["x"], np.float32)
    memv = np.asarray(inputs["mem"], np.float32)
    pos = np.asarray(inputs["positions"], np.int32)
    shared = {}
    w_in = np.asarray(inputs["l0_w_in"], np.float32)
    qcols = np.concatenate([np.arange(h * 64, (h + 1) * 64) for h in _QPERM])
    w_in_p = np.concatenate([w_in[:, qcols], w_in[:, 512:]], axis=1)
    w_out = np.asarray(inputs["l0_w_out"], np.float32)
    w_out_p = np.concatenate([w_out[qcols, :], w_out[512:, :]], axis=0)
    shared["l0_w_in"] = np.ascontiguousarray(w_in_p)
    shared["l0_w_out"] = np.ascontiguousarray(w_out_p)
    shared["l0_sinks"] = np.ascontiguousarray(np.asarray(inputs["l0_sinks"], np.float32)[_QPERM])
    shared["l0_sgu_ln_g"] = np.asarray(inputs["l0_sgu_ln_g"], np.float32)
    shared["l0_sgu_ln_b"] = np.asarray(inputs["l0_sgu_ln_b"], np.float32)
    shared["l0_sgu_wT"] = np.ascontiguousarray(np.transpose(np.asarray(inputs["l0_sgu_w"], np.float32), (2, 0, 1)))
    shared["l0_sgu_bT"] = np.ascontiguousarray(np.asarray(inputs["l0_sgu_b"], np.float32).T)
    shared["l1_w_in"] = np.asarray(inputs["l1_w_in"], np.float32)
    shared["l1_pool_w"] = np.asarray(inputs["l1_pool_w"], np.float32)
    shared["l1_pool_scaleT"] = np.ascontiguousarray(np.asarray(inputs["l1_pool_scale"], np.float32).reshape(8, 128).T)
    shared["l1_w_out"] = np.asarray(inputs["l1_w_out"], np.float32)
    for l in range(2):
        p = f"l{l}_"
        for nm in ("ln1_g", "ln1_b", "ln2_g", "ln2_b", "ln3_g", "ln3_b", "xq", "xkv", "xo", "e_gate", "e_up", "e_down"):
            shared[p + nm] = np.asarray(inputs[p + nm], np.float32)
    shared["router_w"] = np.asarray(inputs["router_w"], np.float32)
    shared["router_bias"] = np.asarray(inputs["router_bias"], np.float32)
    shared["c_ident"] = C["ident"]
    shared["c_tril"] = C["tril"]
    shared["c_invf"] = C["invf"]
    shared["c_lstrict"] = C["lstrict"]
    shared["c_tabs"] = C["tabs"]
    in_maps = []
    for c in range(NCORES):
        b, h = c // 2, c % 2
        m = dict(shared)
        xin = np.zeros((NT * 128, D), np.float32)
        pp = np.zeros((NT * 128,), np.int32)
        if h == 0:
            xin[256:] = x[b, 0:TOK]
            pp[256:] = pos[b, 0:TOK]
        else:
            xin[:] = x[b, TOK - 256:2 * TOK]
            pp[:] = pos[b, TOK - 256:2 * TOK]
        m["xin"] = xin
        m["posT"] = np.ascontiguousarray(pp.reshape(NT, 128).T)
        m["mem"] = np.ascontiguousarray(memv[b])
        first_prev = np.zeros_like(C["m_prev"]) if h == 0 else C["m_prev"]
        m["c_mask"] = np.ascontiguousarray(np.stack([C["m_prev"], C["m_cur"], first_prev], axis=1)).astype(bf)
        pB = C["poolB"].copy()
        if h == 1:
            pB[:, 2] = pB[:, 0]
            pB[:, 3] = pB[:, 1]
        m["c_poolB"] = pB.astype(bf)
        in_maps.append(m)
    return in_maps


_NC_CACHE = {}


def kernel(**inputs):
    stage = "B1"
    if stage not in _NC_CACHE:
        _NC_CACHE[stage] = build_program(stage)
    nc = _NC_CACHE[stage]
    in_maps = make_in_maps(inputs)
    res = run_bass_kernel_spmd(nc, in_maps, core_ids=list(range(NCORES)))
    outs = [np.asarray(r["out"], np.float32) for r in res.results]
    full = np.zeros((4, 8192, D), np.float32)
    for c in range(NCORES):
        full[c // 2, (c % 2) * TOK:(c % 2 + 1) * TOK] = outs[c]
    return full
```
